# Optimizing a Trainium2 kernel written in Bass

```python
import math
import jax, jax.numpy as jnp
from jax import lax
import numpy as np

D_MODEL = 1024
BATCH = 8
SEQ = 4096
DEPTH = 2

CTX_LEN = 256
GRID_W = 64
HEAD_DIM = 64
HY_WIDTH = 256
GA_HEADS = 6
GA_KV_HEADS = 2
WA_HEADS = 6
WA_KV_HEADS = 2
MIX_WIDTH = HY_WIDTH + (GA_HEADS + WA_HEADS) * HEAD_DIM
Q_BLOCK = 128
WINDOW = 128
ROPE_THETA = 10000.0
ATTN_SCALE = HEAD_DIM ** -0.5
HY_SHORT = 3
HY_EMB = 33
HY_BANDS = (HY_EMB - 1) // 2
HY_FILTER_WIDTH = 64
HY_FAST_DECAY = 0.3
HY_SLOW_DECAY = 1.5
HY_TARGET = 1e-2
N_EXPERTS = 32
TOP_K = 4
D_EXPERT = D_MODEL
SWIGLU_ALPHA = 1.702
SWIGLU_LIMIT = 7.0
MOE_BLOCK = 256
EPS = 1e-6
NEG_INF = -1e30
IN_SEGMENTS = (('hy', 3 * HY_WIDTH), ('ga_q', GA_HEADS * HEAD_DIM), ('ga_k', GA_KV_HEADS * HEAD_DIM), ('ga_v', GA_KV_HEADS * HEAD_DIM), ('wa_q', WA_HEADS * HEAD_DIM), ('wa_k', WA_KV_HEADS * HEAD_DIM), ('wa_v', WA_KV_HEADS * HEAD_DIM))
IN_WIDTH = 3 * HY_WIDTH + (GA_HEADS + 2 * GA_KV_HEADS + WA_HEADS + 2 * WA_KV_HEADS) * HEAD_DIM

kernel_name = 'hybrid_hyena_gqa_swa_moe_dit'


def _col_range(name):
    lo = 0
    for seg, width in IN_SEGMENTS:
        if seg == name:
            return lo, lo + width
        lo += width
    raise KeyError(name)


def rms_norm(x, w):
    xf = x.astype(jnp.float32)
    y = xf * lax.rsqrt(jnp.mean(xf * xf, axis=-1, keepdims=True) + EPS)
    return (y * w.astype(jnp.float32)).astype(x.dtype)


def modulate(x, shift, scale):
    return x * (1 + scale) + shift


def _heads(t, n_heads):
    return t.reshape(t.shape[:-1] + (n_heads, HEAD_DIM))


def _groups(t, n_kv):
    return t.reshape(t.shape[:-2] + (n_kv, t.shape[-2] // n_kv, HEAD_DIM))


def axial_rope_tables(n):
    rows = n // GRID_W
    row = jnp.repeat(jnp.arange(rows, dtype=jnp.float32), GRID_W)
    col = jnp.tile(jnp.arange(GRID_W, dtype=jnp.float32), rows)
    pos = jnp.stack([row, col], axis=-1)
    n_freq = HEAD_DIM // 4
    inv_freq = ROPE_THETA ** (-jnp.arange(n_freq, dtype=jnp.float32) / n_freq)
    ang = pos[:, :, None] * inv_freq
    return jnp.cos(ang), jnp.sin(ang)


def apply_axial_rope(x, cos, sin):
    shp = x.shape
    xr = x.reshape(shp[:-1] + (2, 2, HEAD_DIM // 4))
    x1, x2 = xr[..., 0, :], xr[..., 1, :]
    c = cos[:, None].astype(x.dtype)
    s = sin[:, None].astype(x.dtype)
    out = jnp.stack([x1 * c - x2 * s, x2 * c + x1 * s], axis=-2)
    return out.reshape(shp)


def short_conv(u, w, b):
    ch = u.shape[-1]
    pad = HY_SHORT // 2
    y = lax.conv_general_dilated(u, w[:, None, :].astype(u.dtype), window_strides=(1,), padding=((pad, pad),), dimension_numbers=('NWC', 'WIO', 'NWC'), feature_group_count=ch)
    return y + b


def hyena_filters(n, filt):
    w1, b1, w2, b2, w3, b3, freq, w_out = filt
    f32 = jnp.float32
    t = jnp.linspace(0.0, 1.0, n, dtype=f32)[:, None]
    wpos = (2.0 * math.pi / n) * jnp.arange(n, dtype=f32)[:, None]
    bands = jnp.linspace(1e-4, HY_BANDS - 1, HY_BANDS, dtype=f32)
    z = jnp.concatenate([t, jnp.cos(wpos * bands), -jnp.sin(wpos * bands)], axis=-1)
    fr = freq.astype(f32)
    hdn = jnp.sin(fr * (z @ w1.astype(f32) + b1.astype(f32)))
    hdn = jnp.sin(fr * (hdn @ w2.astype(f32) + b2.astype(f32)))
    hdn = jnp.sin(fr * (hdn @ w3.astype(f32) + b3.astype(f32)))
    k = (hdn @ w_out.astype(f32)).reshape(n, 2, HY_WIDTH)
    max_decay = math.log(HY_TARGET) / HY_FAST_DECAY
    min_decay = math.log(HY_TARGET) / HY_SLOW_DECAY
    deltas = jnp.linspace(min_decay, max_decay, HY_WIDTH, dtype=f32)
    decay = jnp.exp(-t * jnp.abs(deltas))
    k = k * decay[:, None, :]
    return k[:, 0], k[:, 1]


def hyena_mix(u, conv_w, conv_b, filt, skip):
    n = u.shape[1]
    uc = short_conv(u, conv_w, conv_b)
    x0, x1, v = jnp.split(uc, 3, axis=-1)
    k_fwd, k_bwd = hyena_filters(n, filt)
    k_full = jnp.concatenate([k_fwd, k_bwd[::-1]], axis=0)
    z = (v * x1).astype(jnp.float32)
    y = jnp.fft.irfft(jnp.fft.rfft(z, n=2 * n, axis=1) * jnp.fft.rfft(k_full, axis=0)[None], n=2 * n, axis=1)[:, :n]
    y = y + z * skip.astype(jnp.float32)
    return x0 * y.astype(u.dtype)


def dense_gqa(q, k, v, sink=None):
    s = jnp.einsum('bqkgd,bskd->bkgqs', q, k).astype(jnp.float32) * ATTN_SCALE
    if sink is not None:
        sk = jnp.broadcast_to(sink.astype(jnp.float32)[None, :, :, None, None], s.shape[:-1] + (1,))
        s = jnp.concatenate([s, sk], axis=-1)
    p = jax.nn.softmax(s, axis=-1)
    if sink is not None:
        p = p[..., :-1]
    return jnp.einsum('bkgqs,bskd->bqkgd', p.astype(v.dtype), v)


def global_attention(q, k, v, k_ctx, v_ctx):
    bsz, n = q.shape[:2]
    k_all = jnp.concatenate([k, k_ctx], axis=1)
    v_all = jnp.concatenate([v, v_ctx], axis=1)
    qb = jnp.moveaxis(q.reshape((bsz, n // Q_BLOCK, Q_BLOCK) + q.shape[2:]), 1, 0)
    ob = lax.map(lambda qblk: dense_gqa(qblk, k_all, v_all), qb)
    return jnp.moveaxis(ob, 0, 1).reshape(bsz, n, -1)


def window_attention(q, k, v, k_ctx, v_ctx, sink):
    bsz, n, kv, g, dh = q.shape
    nb = n // Q_BLOCK
    band_len = 3 * Q_BLOCK

    def band(t):
        tp = jnp.pad(t, ((0, 0), (Q_BLOCK, Q_BLOCK), (0, 0), (0, 0))).reshape(bsz, nb + 2, Q_BLOCK, kv, dh)
        return jnp.concatenate([tp[:, :-2], tp[:, 1:-1], tp[:, 2:]], axis=2)

    kb, vb = band(k), band(v)
    qb = q.reshape(bsz, nb, Q_BLOCK, kv, g, dh)
    s_loc = jnp.einsum('bnqkgd,bnskd->bnkgqs', qb, kb).astype(jnp.float32) * ATTN_SCALE
    blk = jnp.arange(nb, dtype=jnp.int32)[:, None, None] * Q_BLOCK
    qpos = blk + jnp.arange(Q_BLOCK, dtype=jnp.int32)[None, :, None]
    kpos = blk - Q_BLOCK + jnp.arange(band_len, dtype=jnp.int32)[None, None, :]
    valid = (jnp.abs(kpos - qpos) <= WINDOW) & (kpos >= 0) & (kpos < n)
    s_loc = jnp.where(valid[None, :, None, None], s_loc, NEG_INF)
    s_ctx = jnp.einsum('bnqkgd,bckd->bnkgqc', qb, k_ctx).astype(jnp.float32) * ATTN_SCALE
    s_sink = jnp.broadcast_to(sink.astype(jnp.float32)[None, None, :, :, None, None], s_ctx.shape[:-1] + (1,))
    p = jax.nn.softmax(jnp.concatenate([s_loc, s_ctx, s_sink], axis=-1), axis=-1).astype(v.dtype)
    n_ctx = k_ctx.shape[1]
    o = jnp.einsum('bnkgqs,bnskd->bnqkgd', p[..., :band_len], vb) + jnp.einsum('bnkgqc,bckd->bnqkgd', p[..., band_len:band_len + n_ctx], v_ctx)
    return o.reshape(bsz, n, kv * g * dh)


def token_mixer(h, hc, rope_cos, rope_sin, w_in, b_in, conv_w, conv_b, filt, skip, q_norm, k_norm, sink, branch_norm, w_out, b_out, with_ctx_out):
    p = h @ w_in + b_in

    def lat(name):
        lo, hi = _col_range(name)
        return p[..., lo:hi]

    def cproj(name):
        lo, hi = _col_range(name)
        return hc @ w_in[:, lo:hi] + b_in[lo:hi]

    def rope(t):
        return apply_axial_rope(t, rope_cos, rope_sin)

    def merge(y_hy, y_ga, y_wa):
        g0 = HY_WIDTH
        g1 = HY_WIDTH + GA_HEADS * HEAD_DIM
        y = jnp.concatenate([rms_norm(y_hy, branch_norm[:g0]), rms_norm(y_ga, branch_norm[g0:g1]), rms_norm(y_wa, branch_norm[g1:])], axis=-1)
        return y @ w_out + b_out

    wa_sink = sink.reshape(WA_KV_HEADS, WA_HEADS // WA_KV_HEADS)
    y_hy = hyena_mix(lat('hy'), conv_w, conv_b, filt, skip)
    gq = rope(rms_norm(_heads(lat('ga_q'), GA_HEADS), q_norm))
    gk = rope(rms_norm(_heads(lat('ga_k'), GA_KV_HEADS), k_norm))
    gv = _heads(lat('ga_v'), GA_KV_HEADS)
    gkc = rms_norm(_heads(cproj('ga_k'), GA_KV_HEADS), k_norm)
    gvc = _heads(cproj('ga_v'), GA_KV_HEADS)
    y_ga = global_attention(_groups(gq, GA_KV_HEADS), gk, gv, gkc, gvc)
    wq = rope(_heads(lat('wa_q'), WA_HEADS))
    wk = rope(_heads(lat('wa_k'), WA_KV_HEADS))
    wv = _heads(lat('wa_v'), WA_KV_HEADS)
    wkc = _heads(cproj('wa_k'), WA_KV_HEADS)
    wvc = _heads(cproj('wa_v'), WA_KV_HEADS)
    y_wa = window_attention(_groups(wq, WA_KV_HEADS), wk, wv, wkc, wvc, wa_sink)
    y = merge(y_hy, y_ga, y_wa)
    if not with_ctx_out:
        return y, None
    yc_hy = hyena_mix(cproj('hy'), conv_w, conv_b, filt, skip)
    gqc = rms_norm(_heads(cproj('ga_q'), GA_HEADS), q_norm)
    yc_ga = dense_gqa(_groups(gqc, GA_KV_HEADS), gkc, gvc)
    yc_ga = yc_ga.reshape(yc_ga.shape[:2] + (-1,))
    wqc = _heads(cproj('wa_q'), WA_HEADS)
    yc_wa = dense_gqa(_groups(wqc, WA_KV_HEADS), wkc, wvc, wa_sink)
    yc_wa = yc_wa.reshape(yc_wa.shape[:2] + (-1,))
    return y, merge(yc_hy, yc_ga, yc_wa)


def moe_ffn(h, w_router, b_router, w1, b1, w2, b2):
    n_tok, d = h.shape
    f = w2.shape[1]
    logits = (h @ w_router + b_router).astype(jnp.float32)
    top_val, top_idx = lax.top_k(logits, TOP_K)
    gates = jax.nn.softmax(top_val, axis=-1)
    n_assign = n_tok * TOP_K
    n_blocks = -(-n_assign // MOE_BLOCK) + N_EXPERTS
    e_flat = top_idx.reshape(n_assign)
    order = jnp.argsort(e_flat)
    e_sorted = e_flat[order]
    tok_sorted = (order // TOP_K).astype(jnp.int32)
    g_sorted = gates.reshape(n_assign)[order]
    counts = jnp.bincount(e_flat, length=N_EXPERTS)
    starts = jnp.cumsum(counts) - counts
    padded = (counts + MOE_BLOCK - 1) // MOE_BLOCK * MOE_BLOCK
    ends = jnp.cumsum(padded)
    slot = (ends - padded)[e_sorted] + jnp.arange(n_assign, dtype=jnp.int32) - starts[e_sorted]
    slot_tok = jnp.full((n_blocks * MOE_BLOCK,), n_tok, jnp.int32).at[slot].set(tok_sorted)
    slot_gate = jnp.zeros((n_blocks * MOE_BLOCK,), jnp.float32).at[slot].set(g_sorted)
    block_expert = jnp.minimum(jnp.searchsorted(ends, jnp.arange(n_blocks, dtype=jnp.int32) * MOE_BLOCK, side='right'), N_EXPERTS - 1)
    h_pad = jnp.concatenate([h, jnp.zeros((1, d), h.dtype)], axis=0)

    def expert_block(acc, blk):
        tok, gate, e = blk
        a = h_pad[tok] @ w1[e] + b1[e]
        glu = jnp.minimum(a[:, :f], SWIGLU_LIMIT)
        lin = jnp.clip(a[:, f:], -SWIGLU_LIMIT, SWIGLU_LIMIT)
        out = (glu * jax.nn.sigmoid(SWIGLU_ALPHA * glu) * (lin + 1)) @ w2[e] + b2[e]
        return acc.at[tok].add(out.astype(jnp.float32) * gate[:, None]), None

    acc, _ = lax.scan(expert_block, jnp.zeros((n_tok + 1, d), jnp.float32), (slot_tok.reshape(n_blocks, MOE_BLOCK), slot_gate.reshape(n_blocks, MOE_BLOCK), block_expert))
    return acc[:n_tok].astype(h.dtype)


def setup_inputs(seed: int = 0) -> dict:
    key = jax.random.key(seed)
    keys = list(jax.random.split(key, 40))
    counter = [0]

    def nk():
        counter[0] += 1
        return keys[counter[0] - 1]

    def nrm(shape, std):
        return std * jax.random.normal(nk(), shape, jnp.float32)

    def gain(shape):
        return 1.0 + 0.05 * jax.random.normal(nk(), shape, jnp.float32)

    L, D, E, F = DEPTH, D_MODEL, N_EXPERTS, D_EXPERT
    return {
        'x': nrm((BATCH, SEQ, D), 1.0),
        'c': nrm((BATCH, D), 1.0),
        'ctx': nrm((BATCH, CTX_LEN, D), 1.0),
        'c_ctx': nrm((D,), 1.0),
        'w_ada': nrm((L, D, 6 * D), 0.02),
        'b_ada': nrm((L, 6 * D), 0.01),
        'norm_mix': gain((L, D)),
        'norm_ffn': gain((L, D)),
        'w_in': nrm((L, D, IN_WIDTH), D ** -0.5),
        'b_in': nrm((L, IN_WIDTH), 0.01),
        'hy_conv_w': nrm((L, HY_SHORT, 3 * HY_WIDTH), HY_SHORT ** -0.5),
        'hy_conv_b': nrm((L, 3 * HY_WIDTH), 0.01),
        'hy_filt_w1': nrm((L, HY_EMB, HY_FILTER_WIDTH), HY_EMB ** -0.5),
        'hy_filt_b1': nrm((L, HY_FILTER_WIDTH), 0.1),
        'hy_filt_w2': nrm((L, HY_FILTER_WIDTH, HY_FILTER_WIDTH), HY_FILTER_WIDTH ** -0.5),
        'hy_filt_b2': nrm((L, HY_FILTER_WIDTH), 0.1),
        'hy_filt_w3': nrm((L, HY_FILTER_WIDTH, HY_FILTER_WIDTH), HY_FILTER_WIDTH ** -0.5),
        'hy_filt_b3': nrm((L, HY_FILTER_WIDTH), 0.1),
        'hy_filt_freq': gain((L, HY_FILTER_WIDTH)),
        'hy_filt_out': nrm((L, HY_FILTER_WIDTH, 2 * HY_WIDTH), 0.005),
        'hy_skip': nrm((L, HY_WIDTH), 1.0),
        'ga_q_norm': gain((L, HEAD_DIM)),
        'ga_k_norm': gain((L, HEAD_DIM)),
        'wa_sink': nrm((L, WA_HEADS), 0.5),
        'branch_norm': gain((L, MIX_WIDTH)),
        'w_out': nrm((L, MIX_WIDTH, D), MIX_WIDTH ** -0.5),
        'b_out': nrm((L, D), 0.01),
        'w_router': nrm((L, D, E), D ** -0.5),
        'b_router': nrm((L, E), 0.01),
        'w_mlp1': nrm((L, E, D, 2 * F), D ** -0.5),
        'b_mlp1': nrm((L, E, 2 * F), 0.01),
        'w_mlp2': nrm((L, E, F, D), F ** -0.5),
        'b_mlp2': nrm((L, E, D), 0.01),
        'norm_final': gain((D,)),
    }


def reference(x, c, ctx, c_ctx, w_ada, b_ada, norm_mix, norm_ffn, w_in, b_in, hy_conv_w, hy_conv_b, hy_filt_w1, hy_filt_b1, hy_filt_w2, hy_filt_b2, hy_filt_w3, hy_filt_b3, hy_filt_freq, hy_filt_out, hy_skip, ga_q_norm, ga_k_norm, wa_sink, branch_norm, w_out, b_out, w_router, b_router, w_mlp1, b_mlp1, w_mlp2, b_mlp2, norm_final):
    bsz, n, d = x.shape
    rope_cos, rope_sin = axial_rope_tables(n)
    s_c = jax.nn.silu(c)
    s_cc = jax.nn.silu(c_ctx)
    xc = ctx
    for l in range(DEPTH):
        last = l == DEPTH - 1
        mod = (s_c @ w_ada[l] + b_ada[l])[:, None, :]
        mod_c = s_cc @ w_ada[l] + b_ada[l]
        sh1, sc1, g1, sh2, sc2, g2 = jnp.split(mod, 6, axis=-1)
        csh1, csc1, cg1, csh2, csc2, cg2 = jnp.split(mod_c, 6, axis=-1)
        h = modulate(rms_norm(x, norm_mix[l]), sh1, sc1)
        hc = modulate(rms_norm(xc, norm_mix[l]), csh1, csc1)
        filt = (hy_filt_w1[l], hy_filt_b1[l], hy_filt_w2[l], hy_filt_b2[l], hy_filt_w3[l], hy_filt_b3[l], hy_filt_freq[l], hy_filt_out[l])
        o, oc = token_mixer(h, hc, rope_cos, rope_sin, w_in[l], b_in[l], hy_conv_w[l], hy_conv_b[l], filt, hy_skip[l], ga_q_norm[l], ga_k_norm[l], wa_sink[l], branch_norm[l], w_out[l], b_out[l], not last)
        x = x + g1 * o
        h = modulate(rms_norm(x, norm_ffn[l]), sh2, sc2).reshape(bsz * n, d)
        moe_w = (w_router[l], b_router[l], w_mlp1[l], b_mlp1[l], w_mlp2[l], b_mlp2[l])
        if last:
            x = x + g2 * moe_ffn(h, *moe_w).reshape(bsz, n, d)
        else:
            xc = xc + cg1 * oc
            hc = modulate(rms_norm(xc, norm_ffn[l]), csh2, csc2).reshape(-1, d)
            y = moe_ffn(jnp.concatenate([h, hc], axis=0), *moe_w)
            x = x + g2 * y[:bsz * n].reshape(bsz, n, d)
            xc = xc + cg2 * y[bsz * n:].reshape(xc.shape)
    return rms_norm(x, norm_final)
```

```python
import math
import numpy as np
import ml_dtypes
import concourse.bass as bass
import concourse.mybir as mybir
from concourse.bass_utils import run_bass_kernel_spmd

F32 = mybir.dt.float32
BF16 = mybir.dt.bfloat16
ALU = mybir.AluOpType
AF = mybir.ActivationFunctionType
AX = mybir.AxisListType

SEM_LIMIT = 30000
DMA_POOL = 6

D = 1024
NLAT = 4096
NCTX = 256
T = NLAT + NCTX
NT = T // 128
NLT = NLAT // 128
EPS = 1e-6
NE = 32


class Op:
    __slots__ = ("eng", "fn", "sem", "val", "deps", "is_dma", "pre")

    def __init__(self, eng, fn, is_dma):
        self.eng = eng
        self.fn = fn
        self.is_dma = is_dma
        self.sem = None
        self.val = 0
        self.deps = []
        self.pre = None


class Prog:
    ENGS = ("pe", "act", "dve", "pool", "sp")

    def __init__(self, nc):
        self.nc = nc
        self.ops = []
        self.last_w = {}
        self.readers = {}
        self.cnt = {e: 0 for e in self.ENGS}
        self.cur_sem = {e: None for e in self.ENGS}
        self.dma_cnt = {e: 0 for e in self.ENGS}
        self.dma_sems = {e: [] for e in self.ENGS}
        self.dma_hist = {e: [] for e in self.ENGS}
        self._semctx = []
        self.bykey = {}

    def _new_sem(self, name):
        ctx = self.nc.semaphore(name)
        s = ctx.__enter__()
        self._semctx.append(ctx)
        return s

    @staticmethod
    def base(k):
        return k[0] if isinstance(k, tuple) else k

    def hazards_of(self, basename):
        ops = []
        for k in self.bykey.get(basename, ()):
            w = self.last_w.get(k)
            if w is not None:
                ops.append(w)
            ops.extend(self.readers.get(k, ()))
        return ops

    def inherit(self, newbase, ops):
        self.readers.setdefault(("#inh", newbase), []).extend(ops)

    def op(self, eng, fn, reads=(), writes=(), dma=False):
        o = Op(eng, fn, dma)
        deps = set()
        for r in reads:
            w = self.last_w.get(r)
            if w is not None:
                deps.add(w)
        for w in writes:
            lw = self.last_w.get(w)
            if lw is not None:
                deps.add(lw)
            for rd in self.readers.get(w, ()):
                deps.add(rd)
            inh = self.readers.get(("#inh", self.base(w)))
            if inh:
                deps.update(inh)
        for w in writes:
            self.last_w[w] = o
            self.readers[w] = []
            self.bykey.setdefault(self.base(w), set()).add(w)
        for r in reads:
            if r in writes:
                continue
            self.readers.setdefault(r, []).append(o)
            self.bykey.setdefault(self.base(r), set()).add(r)
        deps.discard(o)
        o.deps = list(deps)
        if dma:
            j = self.dma_cnt[eng]
            self.dma_cnt[eng] += 1
            if j < DMA_POOL:
                self.dma_sems[eng].append(self._new_sem(f"d_{eng}_{j}"))
            o.sem = self.dma_sems[eng][j % DMA_POOL]
            o.val = 16 * (j // DMA_POOL + 1)
            if j >= DMA_POOL:
                o.pre = self.dma_hist[eng][j - DMA_POOL]
            self.dma_hist[eng].append(o)
        else:
            if self.cur_sem[eng] is None or self.cnt[eng] >= SEM_LIMIT:
                self.cur_sem[eng] = self._new_sem(f"c_{eng}_{len(self._semctx)}")
                self.cnt[eng] = 0
            self.cnt[eng] += 1
            o.sem = self.cur_sem[eng]
            o.val = self.cnt[eng]
        self.ops.append(o)
        return o

    def dma(self, out, in_, reads=(), writes=(), eng="sp", **kw):
        return self.op(eng, lambda e: e.dma_start(out=out, in_=in_, **kw), reads, writes, dma=True)

    def emit(self):
        nc = self.nc
        per = {e: [] for e in self.ENGS}
        for o in self.ops:
            per[o.eng].append(o)
        engmap = {"pe": "tensor", "act": "scalar", "dve": "vector", "pool": "gpsimd", "sp": "sync"}

        def run(eng_name):
            def body(e):
                seen = {}
                for o in per[eng_name]:
                    waits = {}
                    dl = list(o.deps)
                    if o.pre is not None:
                        dl.append(o.pre)
                    for d in dl:
                        if d.eng == eng_name and eng_name == "pe" and not d.is_dma:
                            continue
                        k = id(d.sem)
                        if seen.get(k, 0) >= d.val:
                            continue
                        if k not in waits or waits[k][1] < d.val:
                            waits[k] = (d.sem, d.val)
                    for k, (s, v) in waits.items():
                        e.wait_ge(s, v)
                        seen[k] = v
                    ins = o.fn(e)
                    ins.then_inc(o.sem, 16 if o.is_dma else 1)
                tail = {}
                for o in per[eng_name]:
                    if o.is_dma:
                        k = id(o.sem)
                        if k not in tail or tail[k][1] < o.val:
                            tail[k] = (o.sem, o.val)
                for k, (s, v) in tail.items():
                    if seen.get(k, 0) < v:
                        e.wait_ge(s, v)
            return body

        with nc.Block() as block:
            for en in self.ENGS:
                if per[en]:
                    getattr(block, engmap[en])(run(en))

    def close(self):
        for c in reversed(self._semctx):
            c.__exit__(None, None, None)
        self._semctx = []


class Arena:
    def __init__(self, P, tensor, nwords):
        self.P = P
        self.t = tensor
        self.n = nwords
        self.live = {}
        self.dead = []

    def alloc(self, name, shape, dtype=F32):
        per = 1
        for s in shape[1:]:
            per *= s
        words = per if dtype == F32 else (per + 1) // 2
        segs = sorted(self.live.values())
        lo = 0
        pos = None
        for (a, b) in segs + [(self.n, self.n)]:
            if a - lo >= words:
                pos = lo
                break
            lo = max(lo, b)
        if pos is None:
            raise RuntimeError(f"arena full allocating {name} {words} words; live={self.live}")
        hi = pos + words
        self.live[name] = (pos, hi)
        ops = []
        nd = []
        for (a, b, o) in self.dead:
            if a < hi and pos < b:
                ops.extend(o)
            nd.append((a, b, o))
        self.dead = nd
        if ops:
            self.P.inherit(name, ops)
        v = self.t[:shape[0], pos:hi]
        if dtype != F32:
            v = v.bitcast(dtype)[:, 0:per]
        if len(shape) > 2:
            names = " ".join(f"d{i}" for i in range(len(shape) - 1))
            kw = {f"d{i}": shape[i + 1] for i in range(len(shape) - 1)}
            v = v.rearrange(f"p ({names}) -> p {names}", **kw)
        return v

    def free(self, *names):
        for name in names:
            lo, hi = self.live.pop(name)
            self.dead.append((lo, hi, self.P.hazards_of(name)))


def _tables():
    tb = {}
    rows = NLAT // 64
    row = np.repeat(np.arange(rows, dtype=np.float32), 64)
    col = np.tile(np.arange(64, dtype=np.float32), rows)
    pos = np.stack([row, col], -1)
    inv = (10000.0 ** (-np.arange(16, dtype=np.float32) / 16)).astype(np.float32)
    ang = pos[:, :, None] * inv
    cos = np.ones((T, 2, 16), np.float32)
    sin = np.zeros((T, 2, 16), np.float32)
    cos[:NLAT] = np.cos(ang)
    sin[:NLAT] = np.sin(ang)
    tb["ropeC"] = np.ascontiguousarray(cos.reshape(NT, 128, 32).transpose(1, 0, 2))
    tb["ropeS"] = np.ascontiguousarray(sin.reshape(NT, 128, 32).transpose(1, 0, 2))
    tb["ident"] = np.eye(128, dtype=np.float32)
    a = np.arange(128)
    m = np.zeros((128, 2, 128), np.float32)
    m[:, 0, :] = (a[None, :] <= a[:, None])
    m[:, 1, :] = (a[:, None] <= a[None, :])
    tb["wmask"] = m.astype(ml_dtypes.bfloat16)
    for n, tag in ((NLAT, "L"), (NCTX, "C")):
        nch = n // 128
        t = np.linspace(0.0, 1.0, n, dtype=np.float32)[:, None]
        wpos = (2.0 * math.pi / n) * np.arange(n, dtype=np.float32)[:, None]
        bands = np.linspace(1e-4, 15, 16, dtype=np.float32)
        z = np.concatenate([t, np.cos(wpos * bands), -np.sin(wpos * bands)], -1).astype(np.float32)
        zz = np.concatenate([z, z[::-1]], 0)
        tb["hyz" + tag] = np.ascontiguousarray(zz.T)
        deltas = np.linspace(math.log(1e-2) / 1.5, math.log(1e-2) / 0.3, 256, dtype=np.float32)
        decay = np.exp(-t * np.abs(deltas)).astype(np.float32)
        dd = np.stack([decay, decay[::-1]], 1)
        tb["hydec" + tag] = np.ascontiguousarray(dd.reshape(nch, 128, 512).transpose(1, 0, 2))
        N2 = 2 * n
        s = np.arange(n, dtype=np.int64)
        ph = (np.outer(s, s) % N2).astype(np.float64) * (2 * math.pi / N2)
        Fc = np.cos(ph)
        Fs = -np.sin(ph)
        Gs = Fs.copy()
        Fs[:, 0] = (-1.0) ** s
        Gs[0, :] = (-1.0) ** s
        def fwd_layout(M):
            return np.ascontiguousarray(M.reshape(nch, 128, nch, 128).transpose(2, 1, 0, 3).reshape(nch, 128, nch * 128)).astype(ml_dtypes.bfloat16)
        TC = 256
        ntc = n // TC
        def inv_layout(M):
            return np.ascontiguousarray(M.reshape(nch, 128, ntc, TC).transpose(2, 1, 0, 3).reshape(ntc, 128, nch * TC)).astype(ml_dtypes.bfloat16)
        tb["Fc" + tag] = fwd_layout(Fc)
        tb["Fs" + tag] = fwd_layout(Fs)
        tb["Gc" + tag] = inv_layout(Fc)
        tb["Gs" + tag] = inv_layout(Gs)
        sc = np.full((128, nch), 2.0 / N2, np.float32)
        sc[0, 0] = 1.0 / N2
        sg = np.where(np.arange(128) % 2 == 0, 1.0, -1.0).astype(np.float32)[:, None]
        tb["hysc" + tag] = np.ascontiguousarray(np.concatenate([sc, sg], 1))
    return tb


_TB = None


def build(nlayers=2, stop=None, dbg=()):
    nc = bass.Bass("TRN2", target_bir_lowering=False)
    ins = {}

    class Lazy:
        def __init__(self, name, shape, dt=F32):
            self.name, self.shape, self.dt, self.ap = name, list(shape), dt, None

        def __call__(self):
            if self.ap is None:
                self.ap = nc.dram_tensor(self.name, self.shape, self.dt, kind="ExternalInput").ap()
                ins[self.name] = self.ap
            return self.ap

    def din(name, shape, dt=F32):
        return Lazy(name, shape, dt)

    def dscr(name, shape, dt=F32):
        kind = "ExternalOutput" if name in dbg else "Internal"
        return nc.dram_tensor(name, list(shape), dt, kind=kind).ap()

    x_in = din("x", [NLAT, D]); ctx_in = din("ctx", [NCTX, D]); c2T = din("c2T", [128, 2, 8])
    w_ada = din("w_ada", [2, D, 6 * D]); b_ada = din("b_ada", [2, 6 * D])
    norm_mix = din("norm_mix", [2, D]); norm_ffn = din("norm_ffn", [2, D])
    w_in = din("w_in", [2, D, 2048]); b_in = din("b_in", [2, 2048]); b_inT = din("b_inT", [128, 2, 6])
    convw = din("convw", [128, 2, 3, 6]); convb = din("convb", [128, 2, 6])
    hyw1 = din("hyw1", [2, 33, 64]); hyw2 = din("hyw2", [2, 64, 64]); hyw3 = din("hyw3", [2, 64, 64])
    hyvec = din("hyvec", [64, 2, 4]); hyout = din("hyout", [2, 64, 512]); hyskip = din("hyskip", [128, 2, 2])
    qkg = din("qkg", [2, 512]); sink = din("sink", [2, 6]); bnorm = din("bnorm", [2, D]); bnT = din("bnT", [128, 2, 2])
    w_out = din("w_out", [2, D, D]); b_out = din("b_out", [2, D])
    w_router = din("w_router", [2, D, NE]); b_router = din("b_router", [2, NE])
    w_mlp1 = din("w_mlp1", [2, NE, D, 2 * D]); b1T = din("b1T", [128, 2, NE, 16])
    w_mlp2 = din("w_mlp2", [2, NE, D, D]); b_mlp2 = din("b_mlp2", [2, NE, D])
    norm_final = din("norm_final", [D])
    ropeC = din("ropeC", [128, NT, 32]); ropeS = din("ropeS", [128, NT, 32])
    ident_in = din("ident", [128, 128]); wmask_in = din("wmask", [128, 2, 128], BF16)
    HN = {"L": NLAT, "C": NCTX}
    hyz = {g: din("hyz" + g, [33, 2 * HN[g]]) for g in "LC"}
    hydec = {g: din("hydec" + g, [128, HN[g] // 128, 512]) for g in "LC"}
    hysc = {g: din("hysc" + g, [128, HN[g] // 128 + 1]) for g in "LC"}
    Fc = {g: din("Fc" + g, [HN[g] // 128, 128, HN[g]], BF16) for g in "LC"}
    Fs = {g: din("Fs" + g, [HN[g] // 128, 128, HN[g]], BF16) for g in "LC"}
    Gc = {g: din("Gc" + g, [HN[g] // 256, 128, HN[g] * 2], BF16) for g in "LC"}
    Gs = {g: din("Gs" + g, [HN[g] // 256, 128, HN[g] * 2], BF16) for g in "LC"}

    out = nc.dram_tensor("out", [NLAT, D], F32, kind="ExternalOutput").ap()
    MODD = dscr("MODD", [2, 6, 128, D])
    XR = dscr("XR", [T, D]); X1 = dscr("X1", [T, D])
    QKD = dscr("QKD", [10, 128, T], BF16)
    VD = dscr("VD", [NT, 128, 260], BF16)
    YNT = dscr("YNT", [8, 128, T], BF16)
    GATD = dscr("GATD", [NE, T])
    HYD = dscr("HYD", [3, 128, 2 * T], BF16)
    DBG = {k: None for k in ("DBGH", "DBGU", "DBGK", "DBGY", "DBGM")}
    if "DBGH" in dbg: DBG["DBGH"] = dscr("DBGH", [T, D], BF16)
    if "DBGK" in dbg: DBG["DBGK"] = dscr("DBGK", [NLAT, 512], BF16)
    if "DBGM" in dbg: DBG["DBGM"] = dscr("DBGM", [D, T])

    from contextlib import ExitStack
    es = ExitStack()
    SBW = 49152 - 1024
    Abig = es.enter_context(nc.sbuf_tensor("arena", [128, SBW], F32))
    PSb = es.enter_context(nc.psum_tensor("psum", [128, 8, 512], F32))
    P = Prog(nc)
    A = Arena(P, Abig, SBW)

    def psbf(bank, nb=1):
        return PSb[:, bank:bank + nb, :].rearrange("p b n -> p (b n)").bitcast(BF16)

    def TT(eng, out_, a, b, op, r, w):
        P.op(eng, lambda e: e.tensor_tensor(out=out_, in0=a, in1=b, op=op), r, w)

    def TS(eng, out_, a, s1, s2, op0, op1, r, w):
        if s2 is None:
            P.op(eng, lambda e: e.tensor_scalar(out=out_, in0=a, scalar1=s1, scalar2=None, op0=op0), r, w)
        else:
            P.op(eng, lambda e: e.tensor_scalar(out=out_, in0=a, scalar1=s1, scalar2=s2, op0=op0, op1=op1), r, w)

    def STT(eng, out_, a, s, b, op0, op1, r, w):
        P.op(eng, lambda e: e.scalar_tensor_tensor(out=out_, in0=a, scalar=s, in1=b, op0=op0, op1=op1), r, w)

    def ACT(out_, a, func, r, w, **kw):
        P.op("act", lambda e: e.activation(out=out_, in_=a, func=func, **kw), r, w)

    def CP(eng, out_, a, r, w):
        if eng == "act":
            P.op("act", lambda e: e.activation(out=out_, in_=a, func=AF.Copy), r, w)
        else:
            P.op(eng, lambda e: e.tensor_copy(out=out_, in_=a), r, w)

    def MM(out_, pairs, r, w):
        def f(e):
            n = len(pairs)
            for i, (l_, rh) in enumerate(pairs):
                ins_ = e.matmul(out_, lhsT=l_, rhs=rh, start=(i == 0), stop=(i == n - 1))
            return ins_
        P.op("pe", f, r, w)

    def MM1(out_, l_, rh, start, stop, r, w):
        P.op("pe", lambda e: e.matmul(out_, lhsT=l_, rhs=rh, start=start, stop=stop), r, w)

    def TR(outs_ins, ident, r, w):
        def f(e):
            for (o_, i_) in outs_ins:
                ins_ = e.transpose(out=o_, in_=i_, identity=ident)
            return ins_
        P.op("pe", f, r, w)

    def MS(eng, ap, val, w):
        P.op(eng, lambda e: e.memset(ap, val), (), w)

    def RECIP(ap, k):
        P.op("dve", lambda e: e.reciprocal(out=ap, in_=ap), k, k)

    def rstd_from_ssq(ssq, rs, scale, keyr, keyw):
        ACT(rs, ssq, AF.Sqrt, keyr, keyw, scale=scale, bias=EPS)
        RECIP(rs, keyw)

    identf = A.alloc("identf", [128, 128]); identb = A.alloc("identb", [128, 128], BF16)
    P.dma(identf, ident_in(), writes=["identf"])
    CP("dve", identb, identf, ["identf"], ["identb"])
    onesf = A.alloc("onesf", [128, 128])
    MS("pool", onesf, 1.0, ["onesf"])
    sil = A.alloc("sil", [128, 2, 8])
    P.dma(sil, c2T(), writes=["sil"])
    ACT(sil, sil, AF.Silu, ["sil"], ["sil"])
    silB = A.alloc("silB", [128, 2, 8, 128])
    for v in range(2):
        for k in range(8):
            CP("dve", silB[:, v, k, :], sil[:, v, k:k + 1].to_broadcast([128, 128]), ["sil"], [("silB", v, k)])

    stages = []

    def done(name):
        stages.append(name)
        return stop == name

    def modd_load(dst, v, j, key):
        P.dma(dst, MODD[v, j], reads=[("MODD", v, j, 0), ("MODD", v, j, 1)], writes=[key])

    def xsrc(l, t):
        if l == 0:
            return (x_in()[t * 128:(t + 1) * 128, :] if t < NLT else ctx_in()[(t - NLT) * 128:(t - NLT + 1) * 128, :]), []
        return XR[t * 128:(t + 1) * 128, :], [("XR", t)]

    def hyena(l, tag, tok0, X0T, ZT, ZTM):
        n = HN[tag]; nch = n // 128; TC = 256; ntc = n // TC
        KF = A.alloc("KF", [128, nch, 512], BF16)
        w1 = A.alloc("hw1", [33, 64]); w2 = A.alloc("hw2", [64, 64]); w3 = A.alloc("hw3", [64, 64])
        wo = A.alloc("hwo", [64, 512]); hv = A.alloc("hv", [64, 8])
        P.dma(w1, hyw1()[l], writes=["hw1"]); P.dma(w2, hyw2()[l], writes=["hw2"]); P.dma(w3, hyw3()[l], writes=["hw3"])
        P.dma(wo, hyout()[l], writes=["hwo"]); P.dma(hv[:, 0:4], hyvec()[:, l, :], writes=["hv"])
        for j in range(3):
            TT("dve", hv[:, 4 + j:5 + j], hv[:, j:j + 1], hv[:, 3:4], ALU.mult, ["hv"], ["hv"])
        CW = min(512, n)
        hz = [A.alloc(f"hz{i}", [33, CW]) for i in range(2)]
        hh = [A.alloc(f"hh{i}", [64, CW]) for i in range(3)]
        hm = A.alloc("hm", [64, CW])
        dec = [A.alloc(f"dec{i}", [128, CW // 128, 256]) for i in range(2)]
        ci = 0
        for half in range(2):
            for c0 in range(0, n, CW):
                zb = hz[ci % 2]; zk = f"hz{ci % 2}"
                P.dma(zb, hyz[tag]()[:, half * n + c0: half * n + c0 + CW], writes=[zk])
                db = dec[ci % 2]; dk = f"dec{ci % 2}"
                P.dma(db, hydec[tag]()[:, c0 // 128:(c0 + CW) // 128, half * 256:(half + 1) * 256], writes=[dk])
                src = zb; srck = zk; ws = [w1, w2, w3]; wks = ["hw1", "hw2", "hw3"]
                for j in range(3):
                    bk = ("ps", 4 + (j % 2))
                    pso = PSb[0:64, 4 + (j % 2), 0:CW]
                    MM(pso, [(ws[j], src)], [wks[j], srck], [bk])
                    hk = f"hh{j}"
                    TS("dve", hh[j], pso, hv[:, 3:4], hv[:, 4 + j:5 + j], ALU.mult, ALU.add, [bk, "hv"], [hk])
                    TS("pool", hm, hh[j], math.pi, 2.0 * math.pi, ALU.is_gt, ALU.mult, [hk], ["hm"])
                    TT("dve", hh[j], hh[j], hm, ALU.subtract, [hk, "hm"], [hk])
                    TS("pool", hm, hh[j], -math.pi, 2.0 * math.pi, ALU.is_lt, ALU.mult, [hk], ["hm"])
                    TT("dve", hh[j], hh[j], hm, ALU.add, [hk, "hm"], [hk])
                    ACT(hh[j], hh[j], AF.Sin, [hk], [hk])
                    src = hh[j]; srck = hk
                for tt_ in range(CW // 128):
                    bk = ("ps", 6 + (tt_ % 2))
                    pso = PSb[:, 6 + (tt_ % 2), 0:256]
                    MM(pso, [(hh[2][:, tt_ * 128:(tt_ + 1) * 128], wo[:, half * 256:(half + 1) * 256])], ["hh2", "hwo"], [bk])
                    TT("dve", KF[:, c0 // 128 + tt_, half * 256:(half + 1) * 256], pso, db[:, tt_, :], ALU.mult, [bk, dk], [("KF", c0 // 128 + tt_, half)])
                ci += 1
        A.free("hw1", "hw2", "hw3", "hwo", "hv", "hz0", "hz1", "hh0", "hh1", "hh2", "hm", "dec0", "dec1")
        if DBG["DBGK"] is not None and tag == "L" and l == 0:
            P.dma(DBG["DBGK"].rearrange("(c p) n -> p c n", p=128), KF, reads=[("KF", c, h) for c in range(nch) for h in range(2)], eng="pool")
        YRE = A.alloc("YRE", [128, nch, 256], BF16); YIM = A.alloc("YIM", [128, nch, 256], BF16)
        hs = A.alloc("hsc", [128, nch + 1])
        P.dma(hs, hysc[tag](), writes=["hsc"])
        ftab = [(A.alloc(f"fct{i}", [128, nch, 128], BF16), A.alloc(f"fst{i}", [128, nch, 128], BF16)) for i in range(2)]
        KA = A.alloc("KA", [128, 2, 512])
        KK = A.alloc("KK", [128, 4, 256])
        TMP = A.alloc("hTMP", [128, 4, 256])
        kfk = [("KF", c, h) for c in range(nch) for h in range(2)]
        ztk = [("ZTM", tok0 // 128 + c) for c in range(nch)]
        for fc in range(nch):
            fct, fst = ftab[fc % 2]; fk = f"fct{fc % 2}"; sk = f"fst{fc % 2}"
            P.dma(fct, Fc[tag]()[fc].rearrange("p (s j) -> p s j", j=128), writes=[fk])
            P.dma(fst, Fs[tag]()[fc].rearrange("p (s j) -> p s j", j=128), writes=[sk])
            MM(PSb[:, 0, :], [(fct[:, s_, :], KF[:, s_, :]) for s_ in range(nch)], [fk] + kfk, [("ps", 0)])
            MM(PSb[:, 1, 0:256], [(fct[:, s_, :], ZTM[:, tok0 // 128 + s_, :]) for s_ in range(nch)], [fk] + ztk, [("ps", 1)])
            MM(PSb[:, 2, :], [(fst[:, s_, :], KF[:, s_, :]) for s_ in range(nch)], [sk] + kfk, [("ps", 2)])
            MM(PSb[:, 3, 0:256], [(fst[:, s_, :], ZTM[:, tok0 // 128 + s_, :]) for s_ in range(nch)], [sk] + ztk, [("ps", 3)])
            CP("act", KA[:, 0, :], PSb[:, 0, :], [("ps", 0)], [("KA", 0)])
            CP("act", KA[:, 1, :], PSb[:, 2, :], [("ps", 2)], [("KA", 1)])
            sg = hs[:, nch:nch + 1]; scl = hs[:, fc:fc + 1]
            for ri in range(2):
                dst = KK[:, 0 if ri == 0 else 1, :]
                STT("dve", dst, KA[:, ri, 256:512], sg, KA[:, ri, 0:256], ALU.mult, ALU.add, [("KA", ri), "hsc"], [("KK", ri)])
                TS("pool", dst, dst, scl, None, ALU.mult, None, [("KK", ri), "hsc"], [("KK", ri)])
            CP("pool", KK[:, 2, :], KK[:, 1, :], [("KK", 1)], [("KK", 2)])
            CP("pool", KK[:, 3, :], KK[:, 0, :], [("KK", 0)], [("KK", 3)])
            if fc == 0:
                CP("pool", KK[0:1, 3, :], KK[0:1, 1, :], [("KK", 1), ("KK", 3)], [("KK", 3)])
                MS("pool", KK[0:1, 2, :], 0.0, [("KK", 2)])
                P.op("pool", lambda e: e.memset(KK[0:1, 1, :], 0.0), [("KK", 2), ("KK", 3)], [("KK", 1)])
            zre = PSb[:, 1, 0:256]; zim = PSb[:, 3, 0:256]
            TT("dve", TMP[:, 0, :], zre, KK[:, 0, :], ALU.mult, [("ps", 1), ("KK", 0)], [("hTMP", 0)])
            TT("dve", TMP[:, 1, :], zim, KK[:, 1, :], ALU.mult, [("ps", 3), ("KK", 1)], [("hTMP", 1)])
            TT("dve", TMP[:, 2, :], zre, KK[:, 2, :], ALU.mult, [("ps", 1), ("KK", 2)], [("hTMP", 2)])
            TT("dve", TMP[:, 3, :], zim, KK[:, 3, :], ALU.mult, [("ps", 3), ("KK", 3)], [("hTMP", 3)])
            TT("pool", YRE[:, fc, :], TMP[:, 0, :], TMP[:, 1, :], ALU.subtract, [("hTMP", 0), ("hTMP", 1)], [("YRE", fc)])
            TT("pool", YIM[:, fc, :], TMP[:, 2, :], TMP[:, 3, :], ALU.add, [("hTMP", 2), ("hTMP", 3)], [("YIM", fc)])
        A.free("KF", "hsc", "fct0", "fst0", "fct1", "fst1", "KA", "KK", "hTMP")
        gtab = [(A.alloc(f"gct{i}", [128, nch, TC], BF16), A.alloc(f"gst{i}", [128, nch, TC], BF16)) for i in range(2)]
        YH = A.alloc("YH", [128, 2, TC]); SQ = A.alloc("hSQ", [128, 2, TC]); RS = A.alloc("hRS", [128, TC])
        YO = A.alloc("hYO", [128, 2, TC], BF16)
        sk2 = A.alloc("hskip", [128, 4])
        P.dma(sk2[:, 0:2], hyskip()[:, l, :], writes=["hskip"])
        P.dma(sk2[:, 2:4], bnT()[:, l, :], writes=["hskip"])
        yk = [("YRE", c) for c in range(nch)] + [("YIM", c) for c in range(nch)]
        for tc in range(ntc):
            gct, gst = gtab[tc % 2]; gk = f"gct{tc % 2}"; gsk = f"gst{tc % 2}"
            P.dma(gct, Gc[tag]()[tc].rearrange("p (s j) -> p s j", j=TC), writes=[gk])
            P.dma(gst, Gs[tag]()[tc].rearrange("p (s j) -> p s j", j=TC), writes=[gsk])
            tsl = slice(tok0 + tc * TC, tok0 + (tc + 1) * TC)
            for cc in range(2):
                pso = PSb[:, 4 + cc, 0:TC]
                MM(pso, [(YRE[:, s_, cc * 128:(cc + 1) * 128], gct[:, s_, :]) for s_ in range(nch)] +
                        [(YIM[:, s_, cc * 128:(cc + 1) * 128], gst[:, s_, :]) for s_ in range(nch)], yk + [gk, gsk], [("ps", 4 + cc)])
                STT("dve", YH[:, cc, :], ZT[:, cc, tsl], sk2[:, cc:cc + 1], pso, ALU.mult, ALU.add, [("ZT", cc), "hskip", ("ps", 4 + cc)], [("YH", cc)])
                TT("pool", YH[:, cc, :], YH[:, cc, :], X0T[:, cc, tsl], ALU.mult, [("YH", cc), ("X0T", cc)], [("YH", cc)])
                ACT(SQ[:, cc, :], YH[:, cc, :], AF.Square, [("YH", cc)], [("hSQ", cc)])
            MM(PSb[:, 6, 0:TC], [(onesf, SQ[:, 0, :]), (onesf, SQ[:, 1, :])], ["onesf", ("hSQ", 0), ("hSQ", 1)], [("ps", 6)])
            ACT(RS, PSb[:, 6, 0:TC], AF.Sqrt, [("ps", 6)], ["hRS"], scale=1.0 / 256, bias=EPS)
            RECIP(RS, ["hRS"])
            for cc in range(2):
                STT("dve", YO[:, cc, :], YH[:, cc, :], sk2[:, 2 + cc:3 + cc], RS, ALU.mult, ALU.mult, [("YH", cc), "hskip", "hRS"], [("hYO", cc)])
            P.dma(YNT[0:2, :, tsl].rearrange("c p n -> p c n"), YO, reads=[("hYO", 0), ("hYO", 1)], writes=[("YNT", 0, tag, tc)], eng="pool")
        A.free("YRE", "YIM", "gct0", "gst0", "gct1", "gst1", "YH", "hSQ", "hRS", "hYO", "hskip")

    def attention(l, br, last):
        qi0 = 0 if br == "ga" else 5
        vi0 = 0 if br == "ga" else 2
        QT = A.alloc("QT", [128, 3, T], BF16); KT = A.alloc("KT", [128, 2, T], BF16); VA = A.alloc("VA", [128, NT, 4, 65], BF16)
        qkr = [("QKD", g_) for g_ in range(9)]
        P.dma(QT, QKD[qi0:qi0 + 3].rearrange("i p n -> p i n"), reads=qkr, writes=["QT"])
        P.dma(KT, QKD[qi0 + 3:qi0 + 5].rearrange("i p n -> p i n"), reads=qkr, writes=["KT"])
        P.dma(VA.rearrange("p t a b -> p t (a b)"), VD.rearrange("t p c -> p t c"), reads=[("VD", t) for t in range(NT)], writes=["VA"])
        BN = A.alloc("BNb", [128, 384])
        boff = 256 if br == "ga" else 640
        P.dma(BN, bnorm()[l][boff:boff + 384].partition_broadcast(128), writes=["BNb"])
        SKE = A.alloc("SKE", [128, 6])
        if br == "wa":
            P.dma(SKE, sink()[l].partition_broadcast(128), writes=["SKE"])
            ACT(SKE, SKE, AF.Exp, ["SKE"], ["SKE"])
            WM = A.alloc("WM", [128, 2, 128], BF16)
            P.dma(WM, wmask_in(), writes=["WM"])
        Yb = [A.alloc(f"Yb{i}", [128, 4, 384]) for i in range(2)]
        Pb = [A.alloc(f"Pb{i}", [128, 512], BF16) for i in range(2)]
        YNb = A.alloc("YNb", [128, 384], BF16)
        YS = [A.alloc(f"YS{i}", [128, 3, 512], BF16) for i in range(2)]
        st = A.alloc("ast", [128, 8])
        junk = A.alloc("ajunk", [128, 384])
        den = A.alloc("aden", [128, 8])
        chunks = [(c * 4, 4) for c in range(8)] + ([] if last else [(32, 2)])
        pcnt = 0
        for ci, (t0, ntl) in enumerate(chunks):
            nq = ntl * 128
            Y = Yb[ci % 2]; ykey = f"Yb{ci % 2}"
            isctx = t0 >= NLT
            for h in range(6):
                g = h // 3; pair = h // 2; b = 64 * (h % 2)
                order = 0 if b == 64 * g else 1
                if br == "ga":
                    units = [(list(range(ntl)), ([32, 33] if isctx else list(range(NT))))]
                else:
                    units = []
                    for qs in range(ntl):
                        i = t0 + qs
                        if isctx:
                            units.append(([qs], [32, 33]))
                        else:
                            units.append(([qs], [j for j in (i - 1, i, i + 1) if 0 <= j < NLT] + [32, 33]))
                for (qsl, keys) in units:
                    q0 = (t0 + qsl[0]) * 128; nqq = len(qsl) * 128
                    for ji, j in enumerate(keys):
                        sb_ = pcnt % 2; pcnt += 1
                        stp = PSb[:, sb_, 0:nqq]
                        MM(stp, [(KT[b:b + 64, order, j * 128:(j + 1) * 128], QT[b:b + 64, pair, q0:q0 + nqq])], ["KT", "QT"], [("ps", sb_)])
                        pb_ = Pb[sb_][:, 0:nqq]; pk = f"Pb{sb_}"
                        ACT(pb_, stp, AF.Exp, [("ps", sb_)], [pk], scale=0.125)
                        if br == "wa" and not isctx and j < NLT and j != t0 + qsl[0]:
                            mi = 0 if j < t0 + qsl[0] else 1
                            TT("pool", pb_, pb_, WM[:, mi, :], ALU.mult, [pk, "WM"], [pk])
                        for qq, qs in enumerate(qsl):
                            MM1(PSb[:, 2 + qs, 0:65], pb_[:, qq * 128:(qq + 1) * 128], VA[:, j, vi0 + g, :], ji == 0, ji == len(keys) - 1, [pk, "VA"], [("ps", 2 + qs)])
                    for qs in qsl:
                        dk = ("aden", qs)
                        if br == "wa":
                            TT("dve", den[:, qs:qs + 1], PSb[:, 2 + qs, 64:65], SKE[:, h:h + 1], ALU.add, [("ps", 2 + qs), "SKE"], [dk])
                        else:
                            CP("dve", den[:, qs:qs + 1], PSb[:, 2 + qs, 64:65], [("ps", 2 + qs)], [dk])
                        RECIP(den[:, qs:qs + 1], [dk])
                        TS("dve", Y[:, qs, h * 64:(h + 1) * 64], PSb[:, 2 + qs, 0:64], den[:, qs:qs + 1], None, ALU.mult, None, [("ps", 2 + qs), dk], [(ykey, qs)])
            ys = YS[ci % 2]; ysk = f"YS{ci % 2}"
            for qs in range(ntl):
                sk_ = ("ast", qs % 2)
                sq = st[:, (qs % 2) * 2:(qs % 2) * 2 + 1]; rs = st[:, (qs % 2) * 2 + 1:(qs % 2) * 2 + 2]
                ACT(junk, Y[:, qs, :], AF.Square, [(ykey, qs)], ["ajunk", sk_], accum_out=sq)
                rstd_from_ssq(sq, rs, 1.0 / 384, [sk_], [sk_])
                STT("dve", YNb, Y[:, qs, :], rs, BN, ALU.mult, ALU.mult, [(ykey, qs), sk_, "BNb"], ["YNb"])
                pb = psbf(6 + qs % 2)
                TR([(pb[:, k * 128:(k + 1) * 128], YNb[:, k * 128:(k + 1) * 128]) for k in range(3)], identb, ["YNb", "identb"], [("ps", 6 + qs % 2)])
                CP("act", ys[:, :, qs * 128:(qs + 1) * 128], pb[:, 0:384].rearrange("p (k c) -> p k c", k=3), [("ps", 6 + qs % 2)], [(ysk, qs)])
            c0 = 2 if br == "ga" else 5
            P.dma(YNT[c0:c0 + 3, :, t0 * 128:t0 * 128 + nq].rearrange("c p n -> p c n"), ys[:, :, 0:nq], reads=[(ysk, qs) for qs in range(ntl)], writes=[("YNT", br, ci)], eng="pool")
        A.free("QT", "KT", "VA", "BNb", "SKE", "Yb0", "Yb1", "Pb0", "Pb1", "YNb", "YS0", "YS1", "ast", "ajunk", "aden")
        if br == "wa":
            A.free("WM")

    for l in range(nlayers):
        last = (l == 1)
        ntl_l = NLT if last else NT
        wst = [A.alloc(f"wada{i}", [128, 8, 512]) for i in range(2)]
        bb = A.alloc("badab", [128, 512]); nb = A.alloc("normb", [128, 2, D])
        P.dma(nb[:, 0, :], norm_mix()[l].partition_broadcast(128), writes=[("normb", 0)])
        P.dma(nb[:, 1, :], norm_ffn()[l].partition_broadcast(128), writes=[("normb", 1)])
        mo = [A.alloc(f"modo{i}", [128, 512]) for i in range(2)]
        for n_ in range(12):
            ws = wst[n_ % 2]; wk = f"wada{n_ % 2}"
            P.dma(ws, w_ada()[l][:, n_ * 512:(n_ + 1) * 512].rearrange("(k p) n -> p k n", p=128), writes=[wk])
            P.dma(bb, b_ada()[l][n_ * 512:(n_ + 1) * 512].partition_broadcast(128), writes=["badab"])
            j = n_ // 2; hf = n_ % 2
            for v in range(2):
                MM(PSb[:, v, :], [(silB[:, v, k, :], ws[:, k, :]) for k in range(8)], [wk] + [("silB", v, k) for k in range(8)], [("ps", v)])
                m_ = mo[v]; mk = f"modo{v}"
                TT("dve", m_, PSb[:, v, :], bb, ALU.add, [("ps", v), "badab"], [mk])
                if j in (1, 4):
                    nsel = 0 if j == 1 else 1
                    STT("dve", m_, m_, 1.0, nb[:, nsel, hf * 512:(hf + 1) * 512], ALU.add, ALU.mult, [mk, ("normb", nsel)], [mk])
                P.dma(MODD[v, j, :, hf * 512:(hf + 1) * 512], m_, reads=[mk], writes=[("MODD", v, j, hf)], eng="pool")
        A.free("wada0", "wada1", "badab", "normb", "modo0", "modo1")
        if done(f"mod{l}"): break

        HT = A.alloc("HT", [128, 8, T], BF16)
        a1 = A.alloc("a1", [128, 2, D]); s1 = A.alloc("s1", [128, 2, D])
        for v in range(2):
            modd_load(a1[:, v, :], v, 1, ("a1", v)); modd_load(s1[:, v, :], v, 0, ("s1", v))
        xts = [A.alloc(f"xt{i}", [128, D]) for i in range(2)]
        junk = A.alloc("junk", [128, D])
        hb = [A.alloc(f"hb{i}", [128, D], BF16) for i in range(2)]
        st = A.alloc("stat", [128, 4])
        for t in range(NT):
            v = 0 if t < NLT else 1
            xt = xts[t % 2]; xk = f"xt{t % 2}"
            src, rk = xsrc(l, t)
            P.dma(xt, src, reads=rk, writes=[xk])
            sq = st[:, (t % 2) * 2:(t % 2) * 2 + 1]; rs = st[:, (t % 2) * 2 + 1:(t % 2) * 2 + 2]; sk = ("stat", t % 2)
            ACT(junk, xt, AF.Square, [xk], ["junk", sk], accum_out=sq)
            rstd_from_ssq(sq, rs, 1.0 / D, [sk], [sk])
            STT("dve", xt, xt, rs, a1[:, v, :], ALU.mult, ALU.mult, [xk, sk, ("a1", v)], [xk])
            h_ = hb[t % 2]; hk = f"hb{t % 2}"
            TT("pool", h_, xt, s1[:, v, :], ALU.add, [xk, ("s1", v)], [hk])
            if DBG["DBGH"] is not None and l == 0:
                P.dma(DBG["DBGH"][t * 128:(t + 1) * 128, :], h_, reads=[hk], eng="pool")
            pb = psbf(t % 2).rearrange("p (k c) -> p k c", k=8)
            TR([(pb[:, k, :], h_[:, k * 128:(k + 1) * 128]) for k in range(8)], identb, [hk, "identb"], [("ps", t % 2)])
            CP("act", HT[:, :, t * 128:(t + 1) * 128], pb, [("ps", t % 2)], [("HT", t)])
        A.free("a1", "s1", "xt0", "xt1", "junk", "hb0", "hb1", "stat")
        if done(f"A{l}"): break

        WIN = A.alloc("WIN", [128, 8, 2048], BF16)
        wstg = [A.alloc(f"wstg{i}", [128, 2048]) for i in range(2)]
        for k in range(8):
            P.dma(wstg[k % 2], w_in()[l][k * 128:(k + 1) * 128, :], writes=[f"wstg{k % 2}"])
            CP("act" if k % 2 == 0 else "pool", WIN[:, k, :], wstg[k % 2], [f"wstg{k % 2}"], [("WIN", k)])
        A.free("wstg0", "wstg1")
        htk = [("HT", t) for t in range(NT)]
        wink = [("WIN", k) for k in range(8)]

        X0T = A.alloc("X0T", [128, 2, T], BF16); ZT = A.alloc("ZT", [128, 2, T], BF16)
        bT = A.alloc("bT", [128, 6]); cw = A.alloc("cw", [128, 3, 6]); cb = A.alloc("cb", [128, 6])
        P.dma(bT, b_inT()[:, l, :], writes=["bT"]); P.dma(cw, convw()[:, l], writes=["cw"]); P.dma(cb, convb()[:, l], writes=["cb"])
        U = A.alloc("U", [128, T + 4]); UC1 = A.alloc("UC1", [128, T], BF16); UC = A.alloc("UC", [128, T])
        MS("pool", U, 0.0, ["U"])
        segs = [(0, NLAT, 1), (NLAT, NCTX, NLAT + 3)]
        for m in (0, 1, 2, 4, 3, 5):
            ck = 0
            for (tk0, n_, uo) in segs:
                for c0 in range(0, n_, 512):
                    w_ = min(512, n_ - c0); bkk = ("ps", ck % 2)
                    MM(PSb[:, ck % 2, 0:w_], [(WIN[:, k, m * 128:(m + 1) * 128], HT[:, k, tk0 + c0:tk0 + c0 + w_]) for k in range(8)], wink + htk, [bkk])
                    ACT(U[:, uo + c0:uo + c0 + w_], PSb[:, ck % 2, 0:w_], AF.Identity, [bkk, "bT"], ["U"], bias=bT[:, m:m + 1])
                    ck += 1
            dst = UC1 if m in (2, 3) else UC; dk = "UC1" if m in (2, 3) else "UC"
            for (tk0, n_, uo) in segs:
                d_ = dst[:, tk0:tk0 + n_]
                TS("dve", d_, U[:, uo:uo + n_], cw[:, 1, m:m + 1], cb[:, m:m + 1], ALU.mult, ALU.add, ["U", "cw", "cb"], [dk])
                STT("dve", d_, U[:, uo - 1:uo - 1 + n_], cw[:, 0, m:m + 1], d_, ALU.mult, ALU.add, ["U", "cw", dk], [dk])
                STT("dve", d_, U[:, uo + 1:uo + 1 + n_], cw[:, 2, m:m + 1], d_, ALU.mult, ALU.add, ["U", "cw", dk], [dk])
            if m < 2:
                CP("pool", X0T[:, m, :], UC, ["UC"], [("X0T", m)])
            elif m >= 4:
                TT("pool", ZT[:, m - 4, :], UC1, UC, ALU.mult, ["UC", "UC1"], [("ZT", m - 4)])
        A.free("bT", "cw", "cb", "U", "UC1", "UC")
        ZTM = A.alloc("ZTM", [128, NT, 256], BF16)
        for t0 in range(0, NT, 4):
            nt_ = min(4, NT - t0); bnk = (t0 // 4) % 2
            pb = psbf(bnk).rearrange("p (t c) -> p t c", t=4)
            TR([(pb[:, tt_, cc * 128:(cc + 1) * 128], ZT[:, cc, (t0 + tt_) * 128:(t0 + tt_ + 1) * 128]) for tt_ in range(nt_) for cc in range(2)], identb,
               [("ZT", 0), ("ZT", 1), "identb"], [("ps", bnk)])
            for tt_ in range(nt_):
                CP("act", ZTM[:, t0 + tt_, :], pb[:, tt_, :], [("ps", bnk)], [("ZTM", t0 + tt_)])
        for i_, (nm_, tl_) in enumerate((("X0T", X0T), ("ZT", ZT), ("ZTM", ZTM))):
            P.dma(HYD[i_], tl_.rearrange("p a b -> p (a b)"), reads=[k_ for k_ in P.bykey.get(nm_, ())], writes=[("HYD", i_)], eng="pool")
        A.free("X0T", "ZT", "ZTM")
        if done(f"B1{l}"): break

        PTs = [A.alloc(f"PT{i}", [128, 1280]) for i in range(2)]
        RQs = [A.alloc(f"RQ{i}", [128, 1536], BF16) for i in range(2)]
        SQ = A.alloc("SQ", [128, 512]); st8 = A.alloc("st8", [128, 2, 8])
        QKG = A.alloc("QKG", [128, 512]); BINB = A.alloc("BINB", [128, 1280])
        P.dma(QKG, qkg()[l].partition_broadcast(128), writes=["QKG"])
        P.dma(BINB, b_in()[l][768:2048].partition_broadcast(128), writes=["BINB"])
        RC = A.alloc("RC", [128, NT, 32]); RSn = A.alloc("RSn", [128, NT, 32])
        P.dma(RC, ropeC(), writes=["RC"]); P.dma(RSn, ropeS(), writes=["RSn"])
        VAUG = [A.alloc(f"VAUG{i}", [128, 4, 65], BF16) for i in range(2)]
        for i in range(2):
            MS("pool", VAUG[i], 1.0, [f"VAUG{i}"])
        QKT = [A.alloc(f"QKT{i}", [128, 10, 512], BF16) for i in range(2)]
        TMPS = [A.alloc(f"rt{i}", [128, 8, 2, 16]) for i in range(4)]
        tcols = [(0, 128), (128, 256), (256, 384), (384, 512), (1280, 1408), (640, 768), (768, 896), (896, 1024), (1024, 1152), (1408, 1536)]
        for t in range(NT):
            PT = PTs[t % 2]; pk = f"PT{t % 2}"; RQ = RQs[t % 2]; rqk = f"RQ{t % 2}"
            b0 = 3 * (t % 2)
            for pi, (c0, c1) in enumerate(((768, 1280), (1280, 1792), (1792, 2048))):
                MM(PSb[:, b0 + pi, 0:c1 - c0], [(HT[:, k, t * 128:(t + 1) * 128], WIN[:, k, c0:c1]) for k in range(8)], wink + [("HT", t)], [("ps", b0 + pi)])
                TT("dve", PT[:, c0 - 768:c1 - 768], PSb[:, b0 + pi, 0:c1 - c0], BINB[:, c0 - 768:c1 - 768], ALU.add, [("ps", b0 + pi), "BINB"], [(pk, pi)])
            pkall = [(pk, 0), (pk, 1), (pk, 2)]
            sk = ("st8", t % 2)
            ACT(SQ, PT[:, 0:512], AF.Square, [(pk, 0)], ["SQ"])
            P.op("dve", lambda e, o_=st8[:, t % 2, :], i_=SQ.rearrange("p (h d) -> p h d", d=64): e.tensor_reduce(out=o_, in_=i_, axis=AX.X, op=ALU.add), ["SQ"], [sk])
            ACT(st8[:, t % 2, :], st8[:, t % 2, :], AF.Sqrt, [sk], [sk], scale=1.0 / 64, bias=EPS)
            RECIP(st8[:, t % 2, :], [sk])
            pv = PT[:, 0:512].rearrange("p (h d) -> p h d", d=64)
            TT("pool", pv, pv, st8[:, t % 2, :].unsqueeze(2).to_broadcast([128, 8, 64]), ALU.mult, [(pk, 0), sk], [(pk, 0)])
            TT("pool", PT[:, 0:512], PT[:, 0:512], QKG, ALU.mult, [(pk, 0), "QKG"], [(pk, 0)])
            Cb = RC[:, t, :].rearrange("p (a f) -> p a f", a=2).unsqueeze(1).to_broadcast([128, 8, 2, 16])
            Sb = RSn[:, t, :].rearrange("p (a f) -> p a f", a=2).unsqueeze(1).to_broadcast([128, 8, 2, 16])
            for ri, base in enumerate((0, 640)):
                xv = PT[:, base:base + 512].rearrange("p (h a s f) -> p h a s f", h=8, a=2, s=2)
                ov = RQ[:, base:base + 512].rearrange("p (h a s f) -> p h a s f", h=8, a=2, s=2)
                x1 = xv[:, :, :, 0, :]; x2 = xv[:, :, :, 1, :]
                rk = pkall
                tk = [(f"rt{i}", ri) for i in range(4)]
                TT("dve", TMPS[0], x1, Cb, ALU.mult, rk + ["RC"], [tk[0]])
                TT("pool", TMPS[1], x2, Sb, ALU.mult, rk + ["RSn"], [tk[1]])
                TT("dve", ov[:, :, :, 0, :], TMPS[0], TMPS[1], ALU.subtract, [tk[0], tk[1]], [(rqk, ri, 0)])
                TT("pool", TMPS[2], x2, Cb, ALU.mult, rk + ["RC"], [tk[2]])
                TT("dve", TMPS[3], x1, Sb, ALU.mult, rk + ["RSn"], [tk[3]])
                TT("pool", ov[:, :, :, 1, :], TMPS[2], TMPS[3], ALU.add, [tk[2], tk[3]], [(rqk, ri, 1)])
            rqall = [(rqk, 0, 0), (rqk, 0, 1), (rqk, 1, 0), (rqk, 1, 1)]
            for (dst0, s0) in ((1280, 448), (1344, 384), (1408, 1088), (1472, 1024)):
                CP("pool", RQ[:, dst0:dst0 + 64], RQ[:, s0:s0 + 64], rqall, [(rqk, "sw", dst0)])
            rqall = rqall + [(rqk, "sw", d_) for d_ in (1280, 1344, 1408, 1472)]
            va = VAUG[t % 2]; vk = f"VAUG{t % 2}"
            CP("act", va[:, 0:2, 0:64], PT[:, 512:640].rearrange("p (h d) -> p h d", h=2), [(pk, 1)], [vk])
            CP("act", va[:, 2:4, 0:64], PT[:, 1152:1280].rearrange("p (h d) -> p h d", h=2), [(pk, 2)], [vk])
            P.dma(VD[t], va.rearrange("p a b -> p (a b)"), reads=[vk], writes=[("VD", t)], eng="pool")
            pb = psbf(6, 2)
            TR([(pb[:, i * 128:(i + 1) * 128], RQ[:, c0:c1]) for i, (c0, c1) in enumerate(tcols)], identb, rqall + ["identb"], [("ps", 6)])
            g4 = t // 4; qt = QKT[g4 % 2]; qtk = f"QKT{g4 % 2}"
            CP("act", qt[:, :, (t % 4) * 128:(t % 4 + 1) * 128], pb[:, 0:1280].rearrange("p (i c) -> p i c", i=10), [("ps", 6)], [(qtk, t % 4)])
            if t % 4 == 3 or t == NT - 1:
                t0 = (t // 4) * 4; nn = (t - t0 + 1) * 128
                P.dma(QKD[:, :, t0 * 128:t0 * 128 + nn].rearrange("i p n -> p i n"), qt[:, :, 0:nn], reads=[(qtk, i) for i in range(t - t0 + 1)], writes=[("QKD", t // 4)], eng="pool")
        A.free("PT0", "PT1", "RQ0", "RQ1", "SQ", "st8", "QKG", "BINB", "RC", "RSn", "VAUG0", "VAUG1", "QKT0", "QKT1", "rt0", "rt1", "rt2", "rt3")
        A.free("HT", "WIN")
        if done(f"B2{l}"): break

        X0T = A.alloc("X0T", [128, 2, T], BF16); ZT = A.alloc("ZT", [128, 2, T], BF16); ZTM = A.alloc("ZTM", [128, NT, 256], BF16)
        P.dma(X0T.rearrange("p a b -> p (a b)"), HYD[0], reads=[("HYD", 0)], writes=[("X0T", 0), ("X0T", 1)])
        P.dma(ZT.rearrange("p a b -> p (a b)"), HYD[1], reads=[("HYD", 1)], writes=[("ZT", 0), ("ZT", 1)])
        P.dma(ZTM.rearrange("p a b -> p (a b)"), HYD[2], reads=[("HYD", 2)], writes=[("ZTM", t_) for t_ in range(NT)])
        hyena(l, "L", 0, X0T, ZT, ZTM)
        if not last:
            hyena(l, "C", NLAT, X0T, ZT, ZTM)
        A.free("X0T", "ZT", "ZTM")
        if done(f"C{l}"): break

        attention(l, "ga", last)
        if done(f"Dga{l}"): break
        attention(l, "wa", last)
        if done(f"D{l}"): break

        YN = A.alloc("YN", [128, 8, T], BF16)
        ynr = [("YNT", 0, "L", tc) for tc in range(NLAT // 256)] + ([] if last else [("YNT", 0, "C", 0)]) + \
              [("YNT", br, ci) for br in ("ga", "wa") for ci in range(8 if last else 9)]
        P.dma(YN, YNT.rearrange("c p n -> p c n"), reads=ynr, writes=["YN"])
        WO = A.alloc("WO", [128, 8, D], BF16)
        wstg = [A.alloc(f"wstg{i}", [128, D]) for i in range(2)]
        for k in range(8):
            P.dma(wstg[k % 2], w_out()[l][k * 128:(k + 1) * 128, :], writes=[f"wstg{k % 2}"])
            CP("act" if k % 2 == 0 else "pool", WO[:, k, :], wstg[k % 2], [f"wstg{k % 2}"], [("WO", k)])
        A.free("wstg0", "wstg1")
        wok = [("WO", k) for k in range(8)]
        BO = A.alloc("BO", [128, D]); G1 = A.alloc("G1", [128, 2, D])
        P.dma(BO, b_out()[l].partition_broadcast(128), writes=["BO"])
        for v in range(2):
            modd_load(G1[:, v, :], v, 2, ("G1", v))
        xts = [A.alloc(f"xt{i}", [128, D]) for i in range(2)]
        tm = [A.alloc(f"tm{i}", [128, D]) for i in range(2)]
        for t in range(ntl_l):
            v = 0 if t < NLT else 1
            xt = xts[t % 2]; xk = f"xt{t % 2}"; tmp = tm[t % 2]; tk = f"tm{t % 2}"
            src, rk = xsrc(l, t)
            P.dma(xt, src, reads=rk, writes=[xk])
            for hf in range(2):
                bk = ("ps", (t % 2) * 2 + hf)
                MM(PSb[:, (t % 2) * 2 + hf, :], [(YN[:, k, t * 128:(t + 1) * 128], WO[:, k, hf * 512:(hf + 1) * 512]) for k in range(8)], ["YN"] + wok, [bk])
                TT("dve", tmp[:, hf * 512:(hf + 1) * 512], PSb[:, (t % 2) * 2 + hf, :], BO[:, hf * 512:(hf + 1) * 512], ALU.add, [bk, "BO"], [(tk, hf)])
            TT("pool", tmp, tmp, G1[:, v, :], ALU.mult, [(tk, 0), (tk, 1), ("G1", v)], [(tk, 0), (tk, 1)])
            TT("pool", xt, xt, tmp, ALU.add, [xk, (tk, 0), (tk, 1)], [xk])
            P.dma(X1[t * 128:(t + 1) * 128, :], xt, reads=[xk], writes=[("X1", t)], eng="pool")
        A.free("YN", "WO", "BO", "G1", "xt0", "xt1", "tm0", "tm1")
        if done(f"E{l}"): break

        GT = A.alloc("GT", [32, T])
        a2 = A.alloc("a2", [128, 2, D]); s2 = A.alloc("s2", [128, 2, D])
        for v in range(2):
            modd_load(a2[:, v, :], v, 4, ("a2", v)); modd_load(s2[:, v, :], v, 3, ("s2", v))
        WR = A.alloc("WR", [128, 8, NE]); BR = A.alloc("BR", [128, NE])
        P.dma(WR, w_router()[l].rearrange("(k p) e -> p k e", p=128), writes=["WR"])
        P.dma(BR, b_router()[l].partition_broadcast(128), writes=["BR"])
        H2D = dscr(f"H2D{l}", [8, 128, T], BF16)
        xts = [A.alloc(f"xt{i}", [128, D]) for i in range(2)]
        junk = A.alloc("junk", [128, D]); h2f = A.alloc("h2f", [128, 8, 128])
        h2b = [A.alloc(f"h2b{i}", [128, 8, 128], BF16) for i in range(2)]
        st = A.alloc("stat", [128, 4]); lg = A.alloc("lg", [128, NE]); m8 = A.alloc("m8", [128, 8]); msk = A.alloc("msk", [128, NE])
        gs = A.alloc("gs", [128, 4])
        for t in range(ntl_l):
            v = 0 if t < NLT else 1
            xt = xts[t % 2]; xk = f"xt{t % 2}"
            P.dma(xt, X1[t * 128:(t + 1) * 128, :], reads=[("X1", t)], writes=[xk])
            sq = st[:, (t % 2) * 2:(t % 2) * 2 + 1]; rs = st[:, (t % 2) * 2 + 1:(t % 2) * 2 + 2]; sk = ("stat", t % 2)
            ACT(junk, xt, AF.Square, [xk], ["junk", sk], accum_out=sq)
            rstd_from_ssq(sq, rs, 1.0 / D, [sk], [sk])
            STT("dve", xt, xt, rs, a2[:, v, :], ALU.mult, ALU.mult, [xk, sk, ("a2", v)], [xk])
            TT("pool", xt, xt, s2[:, v, :], ALU.add, [xk, ("s2", v)], [xk])
            pf = PSb[:, 0:2, :].rearrange("p b n -> p (b n)").rearrange("p (k c) -> p k c", k=8)
            TR([(pf[:, k, :], xt[:, k * 128:(k + 1) * 128]) for k in range(8)], identf, [xk, "identf"], [("ps", 0), ("ps", 1)])
            CP("act", h2f, pf, [("ps", 0), ("ps", 1)], ["h2f"])
            hb_ = h2b[t % 2]; hbk = f"h2b{t % 2}"
            CP("pool", hb_, h2f, ["h2f"], [hbk])
            P.dma(H2D[:, :, t * 128:(t + 1) * 128].rearrange("k p n -> p k n"), hb_, reads=[hbk], writes=[("H2D", t)], eng="pool")
            MM(PSb[:, 2, 0:NE], [(h2f[:, k, :], WR[:, k, :]) for k in range(8)], ["h2f", "WR"], [("ps", 2)])
            TT("dve", lg, PSb[:, 2, 0:NE], BR, ALU.add, [("ps", 2), "BR"], ["lg"])
            P.op("dve", lambda e: e.max(out=m8, in_=lg), ["lg"], ["m8"])
            TS("dve", msk, lg, m8[:, 3:4], None, ALU.is_ge, None, ["lg", "m8"], ["msk"])
            TS("dve", gs[:, 0:1], m8[:, 0:1], -1.0, None, ALU.mult, None, ["m8"], ["gs"])
            ACT(lg, lg, AF.Exp, ["lg", "gs"], ["lg"], bias=gs[:, 0:1])
            TT("dve", lg, lg, msk, ALU.mult, ["lg", "msk"], ["lg"])
            P.op("dve", lambda e: e.tensor_reduce(out=gs[:, 1:2], in_=lg, axis=AX.X, op=ALU.add), ["lg"], ["gs"])
            RECIP(gs[:, 1:2], ["gs"])
            TS("dve", lg, lg, gs[:, 1:2], None, ALU.mult, None, ["lg", "gs"], ["lg"])
            TR([(PSb[0:32, 3, 0:128], lg)], identf, ["lg", "identf"], [("ps", 3)])
            CP("act", GT[:, t * 128:(t + 1) * 128], PSb[0:32, 3, 0:128], [("ps", 3)], [("GT", t)])
        gtk = [("GT", t) for t in range(ntl_l)]
        ntok = ntl_l * 128
        P.dma(GATD[:, 0:ntok], GT[:, 0:ntok], reads=gtk, writes=["GATD"], eng="pool")
        A.free("a2", "s2", "WR", "BR", "xt0", "xt1", "junk", "h2f", "h2b0", "h2b1", "stat", "lg", "m8", "msk", "gs")
        if done(f"F1{l}"): break

        B2f = A.alloc("B2f", [32, D])
        P.dma(B2f, b_mlp2()[l], writes=["B2f"])
        B1 = A.alloc("B1", [128, NE, 16])
        P.dma(B1, b1T()[:, l], writes=["B1"])
        G2 = A.alloc("G2", [128, 2, D])
        for v in range(2):
            modd_load(G2[:, v, :], v, 5, ("G2", v))
        NFb = A.alloc("NFb", [128, D])
        if last:
            P.dma(NFb, norm_final().partition_broadcast(128), writes=["NFb"])
        SCT = 9
        W1 = A.alloc("W1", [128, 8, 2048], BF16); W2 = A.alloc("W2", [128, 8, D], BF16)
        stg = [A.alloc(f"mstg{i}", [128, 2048]) for i in range(2)]
        ACC = A.alloc("ACC", [128, 8, SCT * 128]); H2T = A.alloc("H2T", [128, 8, SCT * 128], BF16)
        GB = A.alloc("GB", [128, SCT * 128])
        Gt = A.alloc("mG", [128, 512]); St = A.alloc("mS", [128, 512]); Lt = A.alloc("mL", [128, 512])
        Ub = A.alloc("mU", [128, 8, 512], BF16)
        w1k = [("W1", k) for k in range(8)]; w2k = [("W2", k) for k in range(8)]
        sgi = 0
        for sc0 in range(0, ntl_l, SCT):
            nts = min(SCT, ntl_l - sc0); tk0 = sc0 * 128; ntk = nts * 128
            P.dma(H2T[:, :, 0:ntk], H2D[:, :, tk0:tk0 + ntk].rearrange("k p n -> p k n"), reads=[("H2D", t) for t in range(sc0, sc0 + nts)], writes=["H2T"])
            cks = [(c0, min(512, ntk - c0)) for c0 in range(0, ntk, 512)]
            for (c0, w_) in cks:
                for dc in range(8):
                    bk = ("ps", 6 + dc % 2)
                    MM(PSb[:, 6 + dc % 2, 0:w_], [(B2f[:, dc * 128:(dc + 1) * 128], GT[:, tk0 + c0:tk0 + c0 + w_])], ["B2f"] + gtk, [bk])
                    CP("act", ACC[:, dc, c0:c0 + w_], PSb[:, 6 + dc % 2, 0:w_], [bk], [("ACC", dc, c0)])
            for e_ in range(NE):
                for k in range(8):
                    sb_ = stg[sgi % 2]; sbk = f"mstg{sgi % 2}"; sgi += 1
                    P.dma(sb_, w_mlp1()[l, e_, k * 128:(k + 1) * 128, :], writes=[sbk])
                    CP("act" if k % 2 == 0 else "pool", W1[:, k, :], sb_, [sbk], [("W1", k)])
                for k in range(0, 8, 2):
                    sb_ = stg[sgi % 2]; sbk = f"mstg{sgi % 2}"; sgi += 1
                    P.dma(sb_.rearrange("p (a n) -> p a n", a=2), w_mlp2()[l, e_, k * 128:(k + 2) * 128, :].rearrange("(a p) n -> p a n", p=128), writes=[sbk])
                    CP("act" if (k // 2) % 2 == 0 else "pool", W2[:, k:k + 2, :], sb_.rearrange("p (a n) -> p a n", a=2), [sbk], [("W2", k), ("W2", k + 1)])
                P.dma(GB[:, 0:ntk], GATD[e_, tk0:tk0 + ntk].partition_broadcast(128), reads=["GATD"], writes=["GB"])
                for (c0, w_) in cks:
                    for jf in range(8):
                        bg = jf % 2; bl = 2 + jf % 2
                        MM(PSb[:, bg, 0:w_], [(W1[:, k, jf * 128:(jf + 1) * 128], H2T[:, k, c0:c0 + w_]) for k in range(8)], w1k + ["H2T"], [("ps", bg)])
                        MM(PSb[:, bl, 0:w_], [(W1[:, k, 1024 + jf * 128:1024 + (jf + 1) * 128], H2T[:, k, c0:c0 + w_]) for k in range(8)], w1k + ["H2T"], [("ps", bl)])
                        G_ = Gt[:, 0:w_]; S_ = St[:, 0:w_]; L_ = Lt[:, 0:w_]
                        TS("dve", G_, PSb[:, bg, 0:w_], B1[:, e_, jf:jf + 1], 7.0, ALU.add, ALU.min, [("ps", bg), "B1"], ["mG"])
                        ACT(S_, G_, AF.Sigmoid, ["mG"], ["mS"], scale=1.702)
                        TS("dve", L_, PSb[:, bl, 0:w_], B1[:, e_, 8 + jf:9 + jf], 7.0, ALU.add, ALU.min, [("ps", bl), "B1"], ["mL"])
                        TS("pool", L_, L_, -7.0, 1.0, ALU.max, ALU.add, ["mL"], ["mL"])
                        TT("pool", S_, S_, G_, ALU.mult, ["mS", "mG"], ["mS"])
                        TT("pool", S_, S_, L_, ALU.mult, ["mS", "mL"], ["mS"])
                        TT("dve", Ub[:, jf, 0:w_], S_, GB[:, c0:c0 + w_], ALU.mult, ["mS", "GB"], [("mU", jf)])
                    for dc in range(8):
                        bk = ("ps", 4 + dc % 2)
                        MM(PSb[:, 4 + dc % 2, 0:w_], [(W2[:, jf, dc * 128:(dc + 1) * 128], Ub[:, jf, 0:w_]) for jf in range(8)], w2k + [("mU", jf) for jf in range(8)], [bk])
                        TT("dve", ACC[:, dc, c0:c0 + w_], ACC[:, dc, c0:c0 + w_], PSb[:, 4 + dc % 2, 0:w_], ALU.add, [bk, ("ACC", dc, c0)], [("ACC", dc, c0)])
            acck = [("ACC", dc, c0) for dc in range(8) for (c0, _) in cks]
            for tt_ in range(nts):
                t = sc0 + tt_; v = 0 if t < NLT else 1
                xt = stg[tt_ % 2][:, 0:D]; xk = f"mstg{tt_ % 2}"
                P.dma(xt, X1[t * 128:(t + 1) * 128, :], reads=[("X1", t)], writes=[xk])
                pf = PSb[:, 6:8, :].rearrange("p b n -> p (b n)").rearrange("p (k c) -> p k c", k=8)
                TR([(pf[:, k, :], ACC[:, k, tt_ * 128:(tt_ + 1) * 128]) for k in range(8)], identf, acck + ["identf"], [("ps", 6), ("ps", 7)])
                y_ = stg[tt_ % 2][:, D:2 * D]
                TT("dve", y_, PSb[:, 6:8, :].rearrange("p b n -> p (b n)"), G2[:, v, :], ALU.mult, [("ps", 6), ("ps", 7), ("G2", v)], [xk])
                TT("pool", xt, xt, y_, ALU.add, [xk], [xk])
                if DBG["DBGM"] is not None and l == 0:
                    pass
                if not last:
                    P.dma(XR[t * 128:(t + 1) * 128, :], xt, reads=[xk], writes=[("XR", t)], eng="pool")
                else:
                    sq = Gt[:, 0:1]; rs = Gt[:, 1:2]
                    ACT(y_, xt, AF.Square, [xk], [xk, "mG"], accum_out=sq)
                    rstd_from_ssq(sq, rs, 1.0 / D, ["mG"], ["mG"])
                    STT("dve", xt, xt, rs, NFb, ALU.mult, ALU.mult, [xk, "mG", "NFb"], [xk])
                    P.dma(out[t * 128:(t + 1) * 128, :], xt, reads=[xk], writes=[("out", t)], eng="pool")
        A.free("GT", "B2f", "B1", "G2", "NFb", "W1", "W2", "mstg0", "mstg1", "ACC", "H2T", "GB", "mG", "mS", "mL", "mU")
        if done(f"F{l}"): break

    P.emit()
    P.close()
    es.close()
    return nc, stages, ins


def make_inputs(inputs, b, names=None):
    global _TB
    if _TB is None:
        _TB = _tables()
    f = lambda a: np.ascontiguousarray(a, dtype=np.float32)
    d = {}
    lay = {
        "x": lambda: f(inputs["x"][b]),
        "ctx": lambda: f(inputs["ctx"][b]),
        "c2T": lambda: f(np.stack([inputs["c"][b], inputs["c_ctx"]], 0).reshape(2, 8, 128).transpose(2, 0, 1)),
        "b_inT": lambda: f(inputs["b_in"][:, :768].reshape(2, 6, 128).transpose(2, 0, 1)),
        "convw": lambda: f(inputs["hy_conv_w"].reshape(2, 3, 6, 128).transpose(3, 0, 1, 2)),
        "convb": lambda: f(inputs["hy_conv_b"].reshape(2, 6, 128).transpose(2, 0, 1)),
        "hyw1": lambda: f(inputs["hy_filt_w1"]), "hyw2": lambda: f(inputs["hy_filt_w2"]), "hyw3": lambda: f(inputs["hy_filt_w3"]),
        "hyvec": lambda: f(np.stack([inputs["hy_filt_b1"], inputs["hy_filt_b2"], inputs["hy_filt_b3"], inputs["hy_filt_freq"]], -1).transpose(1, 0, 2)),
        "hyout": lambda: f(inputs["hy_filt_out"]),
        "hyskip": lambda: f(inputs["hy_skip"].reshape(2, 2, 128).transpose(2, 0, 1)),
        "qkg": lambda: f(np.concatenate([np.tile(inputs["ga_q_norm"], (1, 6)), np.tile(inputs["ga_k_norm"], (1, 2))], 1)),
        "sink": lambda: f(inputs["wa_sink"]),
        "bnorm": lambda: f(inputs["branch_norm"]),
        "bnT": lambda: f(inputs["branch_norm"][:, :256].reshape(2, 2, 128).transpose(2, 0, 1)),
        "b1T": lambda: f(inputs["b_mlp1"].reshape(2, NE, 16, 128).transpose(3, 0, 1, 2)),
    }
    for k in ["w_ada", "b_ada", "norm_mix", "norm_ffn", "w_in", "b_in", "w_out", "b_out", "w_router", "b_router",
              "w_mlp1", "w_mlp2", "b_mlp2", "norm_final"]:
        lay[k] = (lambda k=k: f(inputs[k]))
    for k in (names if names is not None else list(lay.keys()) + list(_TB.keys())):
        d[k] = _TB[k] if k in _TB else lay[k]()
    return d


_SHARED = {}


def kernel(**inputs):
    nc, _, ins = build()
    names = list(ins.keys())
    shared = {}
    in_maps = []
    for b in range(8):
        m = make_inputs(inputs, b, [n for n in names if n in ("x", "ctx", "c2T")])
        if not shared:
            shared = make_inputs(inputs, 0, [n for n in names if n not in ("x", "ctx", "c2T")])
        m.update(shared)
        in_maps.append(m)
    res = run_bass_kernel_spmd(nc, in_maps, core_ids=list(range(8)))
    return np.stack([np.asarray(r["out"], dtype=np.float32) for r in res.results], 0)
```

```python
import math
import numpy as np
import ml_dtypes
import concourse.bass as bass
import concourse.mybir as mybir
from concourse.bass_utils import run_bass_kernel_spmd

F32 = mybir.dt.float32
BF16 = mybir.dt.bfloat16
ALU = mybir.AluOpType
AF = mybir.ActivationFunctionType
AX = mybir.AxisListType

SEM_LIMIT = 30000
DMA_POOL = 6

D = 1024
NLAT = 4096
NCTX = 256
T = NLAT + NCTX
NT = T // 128
NLT = NLAT // 128
EPS = 1e-6
NE = 32


class Op:
    __slots__ = ("eng", "fn", "sem", "val", "deps", "is_dma", "pre")

    def __init__(self, eng, fn, is_dma):
        self.eng = eng
        self.fn = fn
        self.is_dma = is_dma
        self.sem = None
        self.val = 0
        self.deps = []
        self.pre = None


class Prog:
    ENGS = ("pe", "act", "dve", "pool", "sp")

    def __init__(self, nc):
        self.nc = nc
        self.ops = []
        self.last_w = {}
        self.readers = {}
        self.cnt = {e: 0 for e in self.ENGS}
        self.cur_sem = {e: None for e in self.ENGS}
        self.dma_cnt = {e: 0 for e in self.ENGS}
        self.dma_sems = {e: [] for e in self.ENGS}
        self.dma_hist = {e: [] for e in self.ENGS}
        self._semctx = []
        self.bykey = {}

    def _new_sem(self, name):
        ctx = self.nc.semaphore(name)
        s = ctx.__enter__()
        self._semctx.append(ctx)
        return s

    @staticmethod
    def base(k):
        return k[0] if isinstance(k, tuple) else k

    def hazards_of(self, basename):
        ops = []
        for k in self.bykey.get(basename, ()):
            w = self.last_w.get(k)
            if w is not None:
                ops.append(w)
            ops.extend(self.readers.get(k, ()))
        return ops

    def inherit(self, newbase, ops):
        self.readers.setdefault(("#inh", newbase), []).extend(ops)

    def op(self, eng, fn, reads=(), writes=(), dma=False):
        o = Op(eng, fn, dma)
        deps = set()
        for r in reads:
            w = self.last_w.get(r)
            if w is not None:
                deps.add(w)
        for w in writes:
            lw = self.last_w.get(w)
            if lw is not None:
                deps.add(lw)
            for rd in self.readers.get(w, ()):
                deps.add(rd)
            inh = self.readers.get(("#inh", self.base(w)))
            if inh:
                deps.update(inh)
        for w in writes:
            self.last_w[w] = o
            self.readers[w] = []
            self.bykey.setdefault(self.base(w), set()).add(w)
        for r in reads:
            if r in writes:
                continue
            self.readers.setdefault(r, []).append(o)
            self.bykey.setdefault(self.base(r), set()).add(r)
        deps.discard(o)
        o.deps = list(deps)
        if dma:
            j = self.dma_cnt[eng]
            self.dma_cnt[eng] += 1
            if j < DMA_POOL:
                self.dma_sems[eng].append(self._new_sem(f"d_{eng}_{j}"))
            o.sem = self.dma_sems[eng][j % DMA_POOL]
            o.val = 16 * (j // DMA_POOL + 1)
            if j >= DMA_POOL:
                o.pre = self.dma_hist[eng][j - DMA_POOL]
            self.dma_hist[eng].append(o)
        else:
            if self.cur_sem[eng] is None or self.cnt[eng] >= SEM_LIMIT:
                self.cur_sem[eng] = self._new_sem(f"c_{eng}_{len(self._semctx)}")
                self.cnt[eng] = 0
            self.cnt[eng] += 1
            o.sem = self.cur_sem[eng]
            o.val = self.cnt[eng]
        self.ops.append(o)
        return o

    def dma(self, out, in_, reads=(), writes=(), eng="sp", **kw):
        return self.op(eng, lambda e: e.dma_start(out=out, in_=in_, **kw), reads, writes, dma=True)

    def emit(self):
        nc = self.nc
        per = {e: [] for e in self.ENGS}
        for o in self.ops:
            per[o.eng].append(o)
        engmap = {"pe": "tensor", "act": "scalar", "dve": "vector", "pool": "gpsimd", "sp": "sync"}

        def run(eng_name):
            def body(e):
                seen = {}
                for o in per[eng_name]:
                    waits = {}
                    dl = list(o.deps)
                    if o.pre is not None:
                        dl.append(o.pre)
                    for d in dl:
                        if d.eng == eng_name and eng_name == "pe" and not d.is_dma:
                            continue
                        k = id(d.sem)
                        if seen.get(k, 0) >= d.val:
                            continue
                        if k not in waits or waits[k][1] < d.val:
                            waits[k] = (d.sem, d.val)
                    for k, (s, v) in waits.items():
                        e.wait_ge(s, v)
                        seen[k] = v
                    ins = o.fn(e)
                    ins.then_inc(o.sem, 16 if o.is_dma else 1)
                tail = {}
                for o in per[eng_name]:
                    if o.is_dma:
                        k = id(o.sem)
                        if k not in tail or tail[k][1] < o.val:
                            tail[k] = (o.sem, o.val)
                for k, (s, v) in tail.items():
                    if seen.get(k, 0) < v:
                        e.wait_ge(s, v)
            return body

        with nc.Block() as block:
            for en in self.ENGS:
                if per[en]:
                    getattr(block, engmap[en])(run(en))

    def close(self):
        for c in reversed(self._semctx):
            c.__exit__(None, None, None)
        self._semctx = []


class Arena:
    def __init__(self, P, tensor, nwords):
        self.P = P
        self.t = tensor
        self.n = nwords
        self.live = {}
        self.dead = []

    def alloc(self, name, shape, dtype=F32):
        per = 1
        for s in shape[1:]:
            per *= s
        words = per if dtype == F32 else (per + 1) // 2
        segs = sorted(self.live.values())
        lo = 0
        pos = None
        for (a, b) in segs + [(self.n, self.n)]:
            if a - lo >= words:
                pos = lo
                break
            lo = max(lo, b)
        if pos is None:
            raise RuntimeError(f"arena full allocating {name} {words} words; live={self.live}")
        hi = pos + words
        self.live[name] = (pos, hi)
        ops = []
        nd = []
        for (a, b, o) in self.dead:
            if a < hi and pos < b:
                ops.extend(o)
            nd.append((a, b, o))
        self.dead = nd
        if ops:
            self.P.inherit(name, ops)
        v = self.t[:shape[0], pos:hi]
        if dtype != F32:
            v = v.bitcast(dtype)[:, 0:per]
        if len(shape) > 2:
            names = " ".join(f"d{i}" for i in range(len(shape) - 1))
            kw = {f"d{i}": shape[i + 1] for i in range(len(shape) - 1)}
            v = v.rearrange(f"p ({names}) -> p {names}", **kw)
        return v

    def free(self, *names):
        for name in names:
            lo, hi = self.live.pop(name)
            self.dead.append((lo, hi, self.P.hazards_of(name)))


def _tables():
    tb = {}
    rows = NLAT // 64
    row = np.repeat(np.arange(rows, dtype=np.float32), 64)
    col = np.tile(np.arange(64, dtype=np.float32), rows)
    pos = np.stack([row, col], -1)
    inv = (10000.0 ** (-np.arange(16, dtype=np.float32) / 16)).astype(np.float32)
    ang = pos[:, :, None] * inv
    cos = np.ones((T, 2, 16), np.float32)
    sin = np.zeros((T, 2, 16), np.float32)
    cos[:NLAT] = np.cos(ang)
    sin[:NLAT] = np.sin(ang)
    tb["ropeC"] = np.ascontiguousarray(cos.reshape(NT, 128, 32).transpose(1, 0, 2))
    tb["ropeS"] = np.ascontiguousarray(sin.reshape(NT, 128, 32).transpose(1, 0, 2))
    tb["ident"] = np.eye(128, dtype=np.float32)
    a = np.arange(128)
    m = np.zeros((128, 2, 128), np.float32)
    m[:, 0, :] = (a[None, :] <= a[:, None])
    m[:, 1, :] = (a[:, None] <= a[None, :])
    tb["wmask"] = m.astype(ml_dtypes.bfloat16)
    for n, tag in ((NLAT, "L"), (NCTX, "C")):
        nch = n // 128
        t = np.linspace(0.0, 1.0, n, dtype=np.float32)[:, None]
        wpos = (2.0 * math.pi / n) * np.arange(n, dtype=np.float32)[:, None]
        bands = np.linspace(1e-4, 15, 16, dtype=np.float32)
        z = np.concatenate([t, np.cos(wpos * bands), -np.sin(wpos * bands)], -1).astype(np.float32)
        zz = np.concatenate([z, z[::-1]], 0)
        tb["hyz" + tag] = np.ascontiguousarray(zz.T)
        deltas = np.linspace(math.log(1e-2) / 1.5, math.log(1e-2) / 0.3, 256, dtype=np.float32)
        decay = np.exp(-t * np.abs(deltas)).astype(np.float32)
        dd = np.stack([decay, decay[::-1]], 1)
        tb["hydec" + tag] = np.ascontiguousarray(dd.reshape(nch, 128, 512).transpose(1, 0, 2))
        N2 = 2 * n
        s = np.arange(n, dtype=np.int64)
        ph = (np.outer(s, s) % N2).astype(np.float64) * (2 * math.pi / N2)
        Fc = np.cos(ph)
        Fs = -np.sin(ph)
        Gs = Fs.copy()
        Fs[:, 0] = (-1.0) ** s
        Gs[0, :] = (-1.0) ** s
        def fwd_layout(M):
            return np.ascontiguousarray(M.reshape(nch, 128, nch, 128).transpose(2, 1, 0, 3).reshape(nch, 128, nch * 128)).astype(ml_dtypes.bfloat16)
        TC = 256
        ntc = n // TC
        def inv_layout(M):
            return np.ascontiguousarray(M.reshape(nch, 128, ntc, TC).transpose(2, 1, 0, 3).reshape(ntc, 128, nch * TC)).astype(ml_dtypes.bfloat16)
        tb["Fc" + tag] = fwd_layout(Fc)
        tb["Fs" + tag] = fwd_layout(Fs)
        tb["Gc" + tag] = inv_layout(Fc)
        tb["Gs" + tag] = inv_layout(Gs)
        sc = np.full((128, nch), 2.0 / N2, np.float32)
        sc[0, 0] = 1.0 / N2
        sg = np.where(np.arange(128) % 2 == 0, 1.0, -1.0).astype(np.float32)[:, None]
        tb["hysc" + tag] = np.ascontiguousarray(np.concatenate([sc, sg], 1))
    return tb


_TB = None


def build(nlayers=2, stop=None, dbg=()):
    nc = bass.Bass("TRN2", target_bir_lowering=False)
    ins = {}

    class Lazy:
        def __init__(self, name, shape, dt=F32):
            self.name, self.shape, self.dt, self.ap = name, list(shape), dt, None

        def __call__(self):
            if self.ap is None:
                self.ap = nc.dram_tensor(self.name, self.shape, self.dt, kind="ExternalInput").ap()
                ins[self.name] = self.ap
            return self.ap

    def din(name, shape, dt=F32):
        return Lazy(name, shape, dt)

    def dscr(name, shape, dt=F32):
        kind = "ExternalOutput" if name in dbg else "Internal"
        return nc.dram_tensor(name, list(shape), dt, kind=kind).ap()

    x_in = din("x", [NLAT, D]); ctx_in = din("ctx", [NCTX, D]); c2T = din("c2T", [128, 2, 8])
    w_ada = din("w_ada", [2, D, 6 * D]); b_ada = din("b_ada", [2, 6 * D])
    norm_mix = din("norm_mix", [2, D]); norm_ffn = din("norm_ffn", [2, D])
    w_in = din("w_in", [2, D, 2048]); b_in = din("b_in", [2, 2048]); b_inT = din("b_inT", [128, 2, 6])
    convw = din("convw", [128, 2, 3, 6]); convb = din("convb", [128, 2, 6])
    hyw1 = din("hyw1", [2, 33, 64]); hyw2 = din("hyw2", [2, 64, 64]); hyw3 = din("hyw3", [2, 64, 64])
    hyvec = din("hyvec", [64, 2, 4]); hyout = din("hyout", [2, 64, 512]); hyskip = din("hyskip", [128, 2, 2])
    qkg = din("qkg", [2, 512]); sink = din("sink", [2, 6]); bnorm = din("bnorm", [2, D]); bnT = din("bnT", [128, 2, 2])
    w_out = din("w_out", [2, D, D]); b_out = din("b_out", [2, D])
    w_router = din("w_router", [2, D, NE]); b_router = din("b_router", [2, NE])
    w_mlp1 = din("w_mlp1", [2, NE, D, 2 * D]); b1T = din("b1T", [128, 2, NE, 16])
    w_mlp2 = din("w_mlp2", [2, NE, D, D]); b_mlp2 = din("b_mlp2", [2, NE, D])
    norm_final = din("norm_final", [D])
    ropeC = din("ropeC", [128, NT, 32]); ropeS = din("ropeS", [128, NT, 32])
    ident_in = din("ident", [128, 128]); wmask_in = din("wmask", [128, 2, 128], BF16)
    HN = {"L": NLAT, "C": NCTX}
    hyz = {g: din("hyz" + g, [33, 2 * HN[g]]) for g in "LC"}
    hydec = {g: din("hydec" + g, [128, HN[g] // 128, 512]) for g in "LC"}
    hysc = {g: din("hysc" + g, [128, HN[g] // 128 + 1]) for g in "LC"}
    Fc = {g: din("Fc" + g, [HN[g] // 128, 128, HN[g]], BF16) for g in "LC"}
    Fs = {g: din("Fs" + g, [HN[g] // 128, 128, HN[g]], BF16) for g in "LC"}
    Gc = {g: din("Gc" + g, [HN[g] // 256, 128, HN[g] * 2], BF16) for g in "LC"}
    Gs = {g: din("Gs" + g, [HN[g] // 256, 128, HN[g] * 2], BF16) for g in "LC"}

    out = nc.dram_tensor("out", [NLAT, D], F32, kind="ExternalOutput").ap()
    MODD = dscr("MODD", [2, 6, 128, D])
    XR = dscr("XR", [T, D]); X1 = dscr("X1", [T, D])
    QKD = dscr("QKD", [10, 128, T], BF16)
    VD = dscr("VD", [NT, 128, 260], BF16)
    YNT = dscr("YNT", [8, 128, T], BF16)
    GATD = dscr("GATD", [NE, T])
    HYD = dscr("HYD", [3, 128, 2 * T], BF16)
    DBG = {k: None for k in ("DBGH", "DBGU", "DBGK", "DBGY", "DBGM")}
    if "DBGH" in dbg: DBG["DBGH"] = dscr("DBGH", [T, D], BF16)
    if "DBGK" in dbg: DBG["DBGK"] = dscr("DBGK", [NLAT, 512], BF16)
    if "DBGM" in dbg: DBG["DBGM"] = dscr("DBGM", [D, T])

    from contextlib import ExitStack
    es = ExitStack()
    SBW = 49152 - 1024
    Abig = es.enter_context(nc.sbuf_tensor("arena", [128, SBW], F32))
    PSb = es.enter_context(nc.psum_tensor("psum", [128, 8, 512], F32))
    P = Prog(nc)
    A = Arena(P, Abig, SBW)

    def psbf(bank, nb=1):
        return PSb[:, bank:bank + nb, :].rearrange("p b n -> p (b n)").bitcast(BF16)

    def TT(eng, out_, a, b, op, r, w):
        P.op(eng, lambda e: e.tensor_tensor(out=out_, in0=a, in1=b, op=op), r, w)

    def TS(eng, out_, a, s1, s2, op0, op1, r, w):
        if s2 is None:
            P.op(eng, lambda e: e.tensor_scalar(out=out_, in0=a, scalar1=s1, scalar2=None, op0=op0), r, w)
        else:
            P.op(eng, lambda e: e.tensor_scalar(out=out_, in0=a, scalar1=s1, scalar2=s2, op0=op0, op1=op1), r, w)

    def STT(eng, out_, a, s, b, op0, op1, r, w):
        P.op(eng, lambda e: e.scalar_tensor_tensor(out=out_, in0=a, scalar=s, in1=b, op0=op0, op1=op1), r, w)

    def ACT(out_, a, func, r, w, **kw):
        P.op("act", lambda e: e.activation(out=out_, in_=a, func=func, **kw), r, w)

    def CP(eng, out_, a, r, w):
        if eng == "act":
            P.op("act", lambda e: e.activation(out=out_, in_=a, func=AF.Copy), r, w)
        else:
            P.op(eng, lambda e: e.tensor_copy(out=out_, in_=a), r, w)

    def MM(out_, pairs, r, w):
        def f(e):
            n = len(pairs)
            for i, (l_, rh) in enumerate(pairs):
                ins_ = e.matmul(out_, lhsT=l_, rhs=rh, start=(i == 0), stop=(i == n - 1))
            return ins_
        P.op("pe", f, r, w)

    def MM1(out_, l_, rh, start, stop, r, w):
        P.op("pe", lambda e: e.matmul(out_, lhsT=l_, rhs=rh, start=start, stop=stop), r, w)

    def TR(outs_ins, ident, r, w):
        def f(e):
            for (o_, i_) in outs_ins:
                ins_ = e.transpose(out=o_, in_=i_, identity=ident)
            return ins_
        P.op("pe", f, r, w)

    def MS(eng, ap, val, w):
        P.op(eng, lambda e: e.memset(ap, val), (), w)

    def RECIP(ap, k):
        P.op("dve", lambda e: e.reciprocal(out=ap, in_=ap), k, k)

    def rstd_from_ssq(ssq, rs, scale, keyr, keyw):
        ACT(rs, ssq, AF.Sqrt, keyr, keyw, scale=scale, bias=EPS)
        RECIP(rs, keyw)

    identf = A.alloc("identf", [128, 128]); identb = A.alloc("identb", [128, 128], BF16)
    P.dma(identf, ident_in(), writes=["identf"])
    CP("dve", identb, identf, ["identf"], ["identb"])
    onesf = A.alloc("onesf", [128, 128])
    MS("pool", onesf, 1.0, ["onesf"])
    sil = A.alloc("sil", [128, 2, 8])
    P.dma(sil, c2T(), writes=["sil"])
    ACT(sil, sil, AF.Silu, ["sil"], ["sil"])

    stages = []

    def done(name):
        stages.append(name)
        return stop == name

    def modd_load(dst, v, j, key):
        P.dma(dst, MODD[v, j], reads=[("MODD", v, j, 0), ("MODD", v, j, 1)], writes=[key])

    def xsrc(l, t):
        if l == 0:
            return (x_in()[t * 128:(t + 1) * 128, :] if t < NLT else ctx_in()[(t - NLT) * 128:(t - NLT + 1) * 128, :]), []
        return XR[t * 128:(t + 1) * 128, :], [("XR", t)]

    def hyena(l, tag, tok0, X0T, ZT, ZTM):
        n = HN[tag]; nch = n // 128; TC = 256; ntc = n // TC
        KF = A.alloc("KF", [128, nch, 512], BF16)
        w1 = A.alloc("hw1", [33, 64]); w2 = A.alloc("hw2", [64, 64]); w3 = A.alloc("hw3", [64, 64])
        wo = A.alloc("hwo", [64, 512]); hv = A.alloc("hv", [64, 8])
        P.dma(w1, hyw1()[l], writes=["hw1"]); P.dma(w2, hyw2()[l], writes=["hw2"]); P.dma(w3, hyw3()[l], writes=["hw3"])
        P.dma(wo, hyout()[l], writes=["hwo"]); P.dma(hv[:, 0:4], hyvec()[:, l, :], writes=["hv"])
        for j in range(3):
            TT("dve", hv[:, 4 + j:5 + j], hv[:, j:j + 1], hv[:, 3:4], ALU.mult, ["hv"], ["hv"])
        CW = min(512, n)
        hz = [A.alloc(f"hz{i}", [33, CW]) for i in range(2)]
        hh = [A.alloc(f"hh{i}", [64, CW]) for i in range(3)]
        hm = A.alloc("hm", [64, CW])
        dec = [A.alloc(f"dec{i}", [128, CW // 128, 256]) for i in range(2)]
        ci = 0
        for half in range(2):
            for c0 in range(0, n, CW):
                zb = hz[ci % 2]; zk = f"hz{ci % 2}"
                P.dma(zb, hyz[tag]()[:, half * n + c0: half * n + c0 + CW], writes=[zk])
                db = dec[ci % 2]; dk = f"dec{ci % 2}"
                P.dma(db, hydec[tag]()[:, c0 // 128:(c0 + CW) // 128, half * 256:(half + 1) * 256], writes=[dk])
                src = zb; srck = zk; ws = [w1, w2, w3]; wks = ["hw1", "hw2", "hw3"]
                for j in range(3):
                    bk = ("ps", 4 + (j % 2))
                    pso = PSb[0:64, 4 + (j % 2), 0:CW]
                    MM(pso, [(ws[j], src)], [wks[j], srck], [bk])
                    hk = f"hh{j}"
                    TS("dve", hh[j], pso, hv[:, 3:4], hv[:, 4 + j:5 + j], ALU.mult, ALU.add, [bk, "hv"], [hk])
                    TS("pool", hm, hh[j], math.pi, 2.0 * math.pi, ALU.is_gt, ALU.mult, [hk], ["hm"])
                    TT("dve", hh[j], hh[j], hm, ALU.subtract, [hk, "hm"], [hk])
                    TS("pool", hm, hh[j], -math.pi, 2.0 * math.pi, ALU.is_lt, ALU.mult, [hk], ["hm"])
                    TT("dve", hh[j], hh[j], hm, ALU.add, [hk, "hm"], [hk])
                    ACT(hh[j], hh[j], AF.Sin, [hk], [hk])
                    src = hh[j]; srck = hk
                for tt_ in range(CW // 128):
                    bk = ("ps", 6 + (tt_ % 2))
                    pso = PSb[:, 6 + (tt_ % 2), 0:256]
                    MM(pso, [(hh[2][:, tt_ * 128:(tt_ + 1) * 128], wo[:, half * 256:(half + 1) * 256])], ["hh2", "hwo"], [bk])
                    TT("dve", KF[:, c0 // 128 + tt_, half * 256:(half + 1) * 256], pso, db[:, tt_, :], ALU.mult, [bk, dk], [("KF", c0 // 128 + tt_, half)])
                ci += 1
        A.free("hw1", "hw2", "hw3", "hwo", "hv", "hz0", "hz1", "hh0", "hh1", "hh2", "hm", "dec0", "dec1")
        if DBG["DBGK"] is not None and tag == "L" and l == 0:
            P.dma(DBG["DBGK"].rearrange("(c p) n -> p c n", p=128), KF, reads=[("KF", c, h) for c in range(nch) for h in range(2)], eng="pool")
        YRE = A.alloc("YRE", [128, nch, 256], BF16); YIM = A.alloc("YIM", [128, nch, 256], BF16)
        hs = A.alloc("hsc", [128, nch + 1])
        P.dma(hs, hysc[tag](), writes=["hsc"])
        ftab = [(A.alloc(f"fct{i}", [128, nch, 128], BF16), A.alloc(f"fst{i}", [128, nch, 128], BF16)) for i in range(2)]
        KA = A.alloc("KA", [128, 2, 512])
        KK = A.alloc("KK", [128, 4, 256])
        TMP = A.alloc("hTMP", [128, 4, 256])
        kfk = [("KF", c, h) for c in range(nch) for h in range(2)]
        ztk = [("ZTM", tok0 // 128 + c) for c in range(nch)]
        for fc in range(nch):
            fct, fst = ftab[fc % 2]; fk = f"fct{fc % 2}"; sk = f"fst{fc % 2}"
            P.dma(fct, Fc[tag]()[fc].rearrange("p (s j) -> p s j", j=128), writes=[fk])
            P.dma(fst, Fs[tag]()[fc].rearrange("p (s j) -> p s j", j=128), writes=[sk])
            MM(PSb[:, 0, :], [(fct[:, s_, :], KF[:, s_, :]) for s_ in range(nch)], [fk] + kfk, [("ps", 0)])
            MM(PSb[:, 1, 0:256], [(fct[:, s_, :], ZTM[:, tok0 // 128 + s_, :]) for s_ in range(nch)], [fk] + ztk, [("ps", 1)])
            MM(PSb[:, 2, :], [(fst[:, s_, :], KF[:, s_, :]) for s_ in range(nch)], [sk] + kfk, [("ps", 2)])
            MM(PSb[:, 3, 0:256], [(fst[:, s_, :], ZTM[:, tok0 // 128 + s_, :]) for s_ in range(nch)], [sk] + ztk, [("ps", 3)])
            CP("act", KA[:, 0, :], PSb[:, 0, :], [("ps", 0)], [("KA", 0)])
            CP("act", KA[:, 1, :], PSb[:, 2, :], [("ps", 2)], [("KA", 1)])
            sg = hs[:, nch:nch + 1]; scl = hs[:, fc:fc + 1]
            for ri in range(2):
                dst = KK[:, 0 if ri == 0 else 1, :]
                STT("dve", dst, KA[:, ri, 256:512], sg, KA[:, ri, 0:256], ALU.mult, ALU.add, [("KA", ri), "hsc"], [("KK", ri)])
                TS("pool", dst, dst, scl, None, ALU.mult, None, [("KK", ri), "hsc"], [("KK", ri)])
            CP("pool", KK[:, 2, :], KK[:, 1, :], [("KK", 1)], [("KK", 2)])
            CP("pool", KK[:, 3, :], KK[:, 0, :], [("KK", 0)], [("KK", 3)])
            if fc == 0:
                CP("pool", KK[0:1, 3, :], KK[0:1, 1, :], [("KK", 1), ("KK", 3)], [("KK", 3)])
                MS("pool", KK[0:1, 2, :], 0.0, [("KK", 2)])
                P.op("pool", lambda e: e.memset(KK[0:1, 1, :], 0.0), [("KK", 2), ("KK", 3)], [("KK", 1)])
            zre = PSb[:, 1, 0:256]; zim = PSb[:, 3, 0:256]
            TT("dve", TMP[:, 0, :], zre, KK[:, 0, :], ALU.mult, [("ps", 1), ("KK", 0)], [("hTMP", 0)])
            TT("dve", TMP[:, 1, :], zim, KK[:, 1, :], ALU.mult, [("ps", 3), ("KK", 1)], [("hTMP", 1)])
            TT("dve", TMP[:, 2, :], zre, KK[:, 2, :], ALU.mult, [("ps", 1), ("KK", 2)], [("hTMP", 2)])
            TT("dve", TMP[:, 3, :], zim, KK[:, 3, :], ALU.mult, [("ps", 3), ("KK", 3)], [("hTMP", 3)])
            TT("pool", YRE[:, fc, :], TMP[:, 0, :], TMP[:, 1, :], ALU.subtract, [("hTMP", 0), ("hTMP", 1)], [("YRE", fc)])
            TT("pool", YIM[:, fc, :], TMP[:, 2, :], TMP[:, 3, :], ALU.add, [("hTMP", 2), ("hTMP", 3)], [("YIM", fc)])
        A.free("KF", "hsc", "fct0", "fst0", "fct1", "fst1", "KA", "KK", "hTMP")
        gtab = [(A.alloc(f"gct{i}", [128, nch, TC], BF16), A.alloc(f"gst{i}", [128, nch, TC], BF16)) for i in range(2)]
        YH = A.alloc("YH", [128, 2, TC]); SQ = A.alloc("hSQ", [128, 2, TC]); RS = A.alloc("hRS", [128, TC])
        YO = A.alloc("hYO", [128, 2, TC], BF16)
        sk2 = A.alloc("hskip", [128, 4])
        P.dma(sk2[:, 0:2], hyskip()[:, l, :], writes=["hskip"])
        P.dma(sk2[:, 2:4], bnT()[:, l, :], writes=["hskip"])
        yk = [("YRE", c) for c in range(nch)] + [("YIM", c) for c in range(nch)]
        for tc in range(ntc):
            gct, gst = gtab[tc % 2]; gk = f"gct{tc % 2}"; gsk = f"gst{tc % 2}"
            P.dma(gct, Gc[tag]()[tc].rearrange("p (s j) -> p s j", j=TC), writes=[gk])
            P.dma(gst, Gs[tag]()[tc].rearrange("p (s j) -> p s j", j=TC), writes=[gsk])
            tsl = slice(tok0 + tc * TC, tok0 + (tc + 1) * TC)
            for cc in range(2):
                pso = PSb[:, 4 + cc, 0:TC]
                MM(pso, [(YRE[:, s_, cc * 128:(cc + 1) * 128], gct[:, s_, :]) for s_ in range(nch)] +
                        [(YIM[:, s_, cc * 128:(cc + 1) * 128], gst[:, s_, :]) for s_ in range(nch)], yk + [gk, gsk], [("ps", 4 + cc)])
                STT("dve", YH[:, cc, :], ZT[:, cc, tsl], sk2[:, cc:cc + 1], pso, ALU.mult, ALU.add, [("ZT", cc), "hskip", ("ps", 4 + cc)], [("YH", cc)])
                TT("pool", YH[:, cc, :], YH[:, cc, :], X0T[:, cc, tsl], ALU.mult, [("YH", cc), ("X0T", cc)], [("YH", cc)])
                ACT(SQ[:, cc, :], YH[:, cc, :], AF.Square, [("YH", cc)], [("hSQ", cc)])
            MM(PSb[:, 6, 0:TC], [(onesf, SQ[:, 0, :]), (onesf, SQ[:, 1, :])], ["onesf", ("hSQ", 0), ("hSQ", 1)], [("ps", 6)])
            ACT(RS, PSb[:, 6, 0:TC], AF.Sqrt, [("ps", 6)], ["hRS"], scale=1.0 / 256, bias=EPS)
            RECIP(RS, ["hRS"])
            for cc in range(2):
                STT("dve", YO[:, cc, :], YH[:, cc, :], sk2[:, 2 + cc:3 + cc], RS, ALU.mult, ALU.mult, [("YH", cc), "hskip", "hRS"], [("hYO", cc)])
            P.dma(YNT[0:2, :, tsl].rearrange("c p n -> p c n"), YO, reads=[("hYO", 0), ("hYO", 1)], writes=[("YNT", 0, tag, tc)], eng="pool")
        A.free("YRE", "YIM", "gct0", "gst0", "gct1", "gst1", "YH", "hSQ", "hRS", "hYO", "hskip")

    def attention(l, br, last):
        qi0 = 0 if br == "ga" else 5
        vi0 = 0 if br == "ga" else 2
        QT = A.alloc("QT", [128, 3, T], BF16); KT = A.alloc("KT", [128, 2, T], BF16); VA = A.alloc("VA", [128, NT, 4, 65], BF16)
        qkr = [("QKD", g_) for g_ in range(9)]
        P.dma(QT, QKD[qi0:qi0 + 3].rearrange("i p n -> p i n"), reads=qkr, writes=["QT"])
        P.dma(KT, QKD[qi0 + 3:qi0 + 5].rearrange("i p n -> p i n"), reads=qkr, writes=["KT"])
        P.dma(VA.rearrange("p t a b -> p t (a b)"), VD.rearrange("t p c -> p t c"), reads=[("VD", t) for t in range(NT)], writes=["VA"])
        BN = A.alloc("BNb", [128, 384])
        boff = 256 if br == "ga" else 640
        P.dma(BN, bnorm()[l][boff:boff + 384].partition_broadcast(128), writes=["BNb"])
        SKE = A.alloc("SKE", [128, 6])
        if br == "wa":
            P.dma(SKE, sink()[l].partition_broadcast(128), writes=["SKE"])
            ACT(SKE, SKE, AF.Exp, ["SKE"], ["SKE"])
            WM = A.alloc("WM", [128, 2, 128], BF16)
            P.dma(WM, wmask_in(), writes=["WM"])
        Yb = [A.alloc(f"Yb{i}", [128, 4, 384]) for i in range(2)]
        Pb = [A.alloc(f"Pb{i}", [128, 512], BF16) for i in range(2)]
        YNb = A.alloc("YNb", [128, 384], BF16)
        YS = [A.alloc(f"YS{i}", [128, 3, 512], BF16) for i in range(2)]
        st = A.alloc("ast", [128, 8])
        junk = A.alloc("ajunk", [128, 384])
        den = A.alloc("aden", [128, 8])
        chunks = [(c * 4, 4) for c in range(8)] + ([] if last else [(32, 2)])
        pcnt = 0
        for ci, (t0, ntl) in enumerate(chunks):
            nq = ntl * 128
            Y = Yb[ci % 2]; ykey = f"Yb{ci % 2}"
            isctx = t0 >= NLT
            for h in range(6):
                g = h // 3; pair = h // 2; b = 64 * (h % 2)
                order = 0 if b == 64 * g else 1
                if br == "ga":
                    units = [(list(range(ntl)), ([32, 33] if isctx else list(range(NT))))]
                else:
                    units = []
                    for qs in range(ntl):
                        i = t0 + qs
                        if isctx:
                            units.append(([qs], [32, 33]))
                        else:
                            units.append(([qs], [j for j in (i - 1, i, i + 1) if 0 <= j < NLT] + [32, 33]))
                for (qsl, keys) in units:
                    q0 = (t0 + qsl[0]) * 128; nqq = len(qsl) * 128
                    pend = None

                    def pv_emit(pd):
                        ji_, j_, pb__, pk__ = pd
                        for qq, qs in enumerate(qsl):
                            MM1(PSb[:, 2 + qs, 0:65], pb__[:, qq * 128:(qq + 1) * 128], VA[:, j_, vi0 + g, :], ji_ == 0, ji_ == len(keys) - 1, [pk__, "VA"], [("ps", 2 + qs)])
                    for ji, j in enumerate(keys):
                        sb_ = pcnt % 2; pcnt += 1
                        stp = PSb[:, sb_, 0:nqq]
                        MM(stp, [(KT[b:b + 64, order, j * 128:(j + 1) * 128], QT[b:b + 64, pair, q0:q0 + nqq])], ["KT", "QT"], [("ps", sb_)])
                        pb_ = Pb[sb_][:, 0:nqq]; pk = f"Pb{sb_}"
                        ACT(pb_, stp, AF.Exp, [("ps", sb_)], [pk], scale=0.125)
                        if br == "wa" and not isctx and j < NLT and j != t0 + qsl[0]:
                            mi = 0 if j < t0 + qsl[0] else 1
                            TT("pool", pb_, pb_, WM[:, mi, :], ALU.mult, [pk, "WM"], [pk])
                        if pend is not None:
                            pv_emit(pend)
                        pend = (ji, j, pb_, pk)
                    pv_emit(pend)
                    for qs in qsl:
                        dk = ("aden", qs)
                        if br == "wa":
                            TT("dve", den[:, qs:qs + 1], PSb[:, 2 + qs, 64:65], SKE[:, h:h + 1], ALU.add, [("ps", 2 + qs), "SKE"], [dk])
                        else:
                            CP("dve", den[:, qs:qs + 1], PSb[:, 2 + qs, 64:65], [("ps", 2 + qs)], [dk])
                        RECIP(den[:, qs:qs + 1], [dk])
                        TS("dve", Y[:, qs, h * 64:(h + 1) * 64], PSb[:, 2 + qs, 0:64], den[:, qs:qs + 1], None, ALU.mult, None, [("ps", 2 + qs), dk], [(ykey, qs)])
            ys = YS[ci % 2]; ysk = f"YS{ci % 2}"
            for qs in range(ntl):
                sk_ = ("ast", qs % 2)
                sq = st[:, (qs % 2) * 2:(qs % 2) * 2 + 1]; rs = st[:, (qs % 2) * 2 + 1:(qs % 2) * 2 + 2]
                ACT(junk, Y[:, qs, :], AF.Square, [(ykey, qs)], ["ajunk", sk_], accum_out=sq)
                rstd_from_ssq(sq, rs, 1.0 / 384, [sk_], [sk_])
                STT("dve", YNb, Y[:, qs, :], rs, BN, ALU.mult, ALU.mult, [(ykey, qs), sk_, "BNb"], ["YNb"])
                pb = psbf(6 + qs % 2)
                TR([(pb[:, k * 128:(k + 1) * 128], YNb[:, k * 128:(k + 1) * 128]) for k in range(3)], identb, ["YNb", "identb"], [("ps", 6 + qs % 2)])
                CP("act", ys[:, :, qs * 128:(qs + 1) * 128], pb[:, 0:384].rearrange("p (k c) -> p k c", k=3), [("ps", 6 + qs % 2)], [(ysk, qs)])
            c0 = 2 if br == "ga" else 5
            P.dma(YNT[c0:c0 + 3, :, t0 * 128:t0 * 128 + nq].rearrange("c p n -> p c n"), ys[:, :, 0:nq], reads=[(ysk, qs) for qs in range(ntl)], writes=[("YNT", br, ci)], eng="pool")
        A.free("QT", "KT", "VA", "BNb", "SKE", "Yb0", "Yb1", "Pb0", "Pb1", "YNb", "YS0", "YS1", "ast", "ajunk", "aden")
        if br == "wa":
            A.free("WM")

    for l in range(nlayers):
        last = (l == 1)
        ntl_l = NLT if last else NT
        silB = A.alloc("silB", [128, 2, 8, 128])
        for v in range(2):
            for k in range(8):
                CP("dve", silB[:, v, k, :], sil[:, v, k:k + 1].to_broadcast([128, 128]), ["sil"], [("silB", v, k)])
        wst = [A.alloc(f"wada{i}", [128, 8, 512]) for i in range(2)]
        bb = A.alloc("badab", [128, 512]); nb = A.alloc("normb", [128, 2, D])
        P.dma(nb[:, 0, :], norm_mix()[l].partition_broadcast(128), writes=[("normb", 0)])
        P.dma(nb[:, 1, :], norm_ffn()[l].partition_broadcast(128), writes=[("normb", 1)])
        mo = [A.alloc(f"modo{i}", [128, 512]) for i in range(2)]
        for n_ in range(12):
            ws = wst[n_ % 2]; wk = f"wada{n_ % 2}"
            P.dma(ws, w_ada()[l][:, n_ * 512:(n_ + 1) * 512].rearrange("(k p) n -> p k n", p=128), writes=[wk])
            P.dma(bb, b_ada()[l][n_ * 512:(n_ + 1) * 512].partition_broadcast(128), writes=["badab"])
            j = n_ // 2; hf = n_ % 2
            for v in range(2):
                MM(PSb[:, v, :], [(silB[:, v, k, :], ws[:, k, :]) for k in range(8)], [wk] + [("silB", v, k) for k in range(8)], [("ps", v)])
                m_ = mo[v]; mk = f"modo{v}"
                TT("dve", m_, PSb[:, v, :], bb, ALU.add, [("ps", v), "badab"], [mk])
                if j in (1, 4):
                    nsel = 0 if j == 1 else 1
                    STT("dve", m_, m_, 1.0, nb[:, nsel, hf * 512:(hf + 1) * 512], ALU.add, ALU.mult, [mk, ("normb", nsel)], [mk])
                P.dma(MODD[v, j, :, hf * 512:(hf + 1) * 512], m_, reads=[mk], writes=[("MODD", v, j, hf)], eng="pool")
        A.free("wada0", "wada1", "badab", "normb", "modo0", "modo1", "silB")
        if done(f"mod{l}"): break

        HT = A.alloc("HT", [128, 8, T], BF16)
        a1 = A.alloc("a1", [128, 2, D]); s1 = A.alloc("s1", [128, 2, D])
        for v in range(2):
            modd_load(a1[:, v, :], v, 1, ("a1", v)); modd_load(s1[:, v, :], v, 0, ("s1", v))
        xts = [A.alloc(f"xt{i}", [128, D]) for i in range(2)]
        junk = A.alloc("junk", [128, D])
        hb = [A.alloc(f"hb{i}", [128, D], BF16) for i in range(2)]
        st = A.alloc("stat", [128, 4])
        for t in range(NT):
            v = 0 if t < NLT else 1
            xt = xts[t % 2]; xk = f"xt{t % 2}"
            src, rk = xsrc(l, t)
            P.dma(xt, src, reads=rk, writes=[xk])
            sq = st[:, (t % 2) * 2:(t % 2) * 2 + 1]; rs = st[:, (t % 2) * 2 + 1:(t % 2) * 2 + 2]; sk = ("stat", t % 2)
            ACT(junk, xt, AF.Square, [xk], ["junk", sk], accum_out=sq)
            rstd_from_ssq(sq, rs, 1.0 / D, [sk], [sk])
            STT("dve", xt, xt, rs, a1[:, v, :], ALU.mult, ALU.mult, [xk, sk, ("a1", v)], [xk])
            h_ = hb[t % 2]; hk = f"hb{t % 2}"
            TT("pool", h_, xt, s1[:, v, :], ALU.add, [xk, ("s1", v)], [hk])
            if DBG["DBGH"] is not None and l == 0:
                P.dma(DBG["DBGH"][t * 128:(t + 1) * 128, :], h_, reads=[hk], eng="pool")
            pb = psbf(t % 2).rearrange("p (k c) -> p k c", k=8)
            TR([(pb[:, k, :], h_[:, k * 128:(k + 1) * 128]) for k in range(8)], identb, [hk, "identb"], [("ps", t % 2)])
            CP("act", HT[:, :, t * 128:(t + 1) * 128], pb, [("ps", t % 2)], [("HT", t)])
        A.free("a1", "s1", "xt0", "xt1", "junk", "hb0", "hb1", "stat")
        if done(f"A{l}"): break

        WIN = A.alloc("WIN", [128, 8, 2048], BF16)
        wstg = [A.alloc(f"wstg{i}", [128, 2048]) for i in range(2)]
        for k in range(8):
            P.dma(wstg[k % 2], w_in()[l][k * 128:(k + 1) * 128, :], writes=[f"wstg{k % 2}"])
            CP("act" if k % 2 == 0 else "pool", WIN[:, k, :], wstg[k % 2], [f"wstg{k % 2}"], [("WIN", k)])
        A.free("wstg0", "wstg1")
        htk = [("HT", t) for t in range(NT)]
        wink = [("WIN", k) for k in range(8)]

        X0T = A.alloc("X0T", [128, 2, T], BF16); ZT = A.alloc("ZT", [128, 2, T], BF16)
        bT = A.alloc("bT", [128, 6]); cw = A.alloc("cw", [128, 3, 6]); cb = A.alloc("cb", [128, 6])
        P.dma(bT, b_inT()[:, l, :], writes=["bT"]); P.dma(cw, convw()[:, l], writes=["cw"]); P.dma(cb, convb()[:, l], writes=["cb"])
        U = A.alloc("U", [128, T + 4]); UC1 = A.alloc("UC1", [128, T], BF16); UC = A.alloc("UC", [128, T])
        MS("pool", U, 0.0, ["U"])
        segs = [(0, NLAT, 1), (NLAT, NCTX, NLAT + 3)]
        for m in (0, 1, 2, 4, 3, 5):
            ck = 0
            for (tk0, n_, uo) in segs:
                for c0 in range(0, n_, 512):
                    w_ = min(512, n_ - c0); bkk = ("ps", ck % 2)
                    MM(PSb[:, ck % 2, 0:w_], [(WIN[:, k, m * 128:(m + 1) * 128], HT[:, k, tk0 + c0:tk0 + c0 + w_]) for k in range(8)], wink + htk, [bkk])
                    ACT(U[:, uo + c0:uo + c0 + w_], PSb[:, ck % 2, 0:w_], AF.Identity, [bkk, "bT"], ["U"], bias=bT[:, m:m + 1])
                    ck += 1
            dst = UC1 if m in (2, 3) else UC; dk = "UC1" if m in (2, 3) else "UC"
            for (tk0, n_, uo) in segs:
                d_ = dst[:, tk0:tk0 + n_]
                TS("dve", d_, U[:, uo:uo + n_], cw[:, 1, m:m + 1], cb[:, m:m + 1], ALU.mult, ALU.add, ["U", "cw", "cb"], [dk])
                STT("dve", d_, U[:, uo - 1:uo - 1 + n_], cw[:, 0, m:m + 1], d_, ALU.mult, ALU.add, ["U", "cw", dk], [dk])
                STT("dve", d_, U[:, uo + 1:uo + 1 + n_], cw[:, 2, m:m + 1], d_, ALU.mult, ALU.add, ["U", "cw", dk], [dk])
            if m < 2:
                CP("pool", X0T[:, m, :], UC, ["UC"], [("X0T", m)])
            elif m >= 4:
                TT("pool", ZT[:, m - 4, :], UC1, UC, ALU.mult, ["UC", "UC1"], [("ZT", m - 4)])
        A.free("bT", "cw", "cb", "U", "UC1", "UC")
        ZTM = A.alloc("ZTM", [128, NT, 256], BF16)
        for t0 in range(0, NT, 4):
            nt_ = min(4, NT - t0); bnk = (t0 // 4) % 2
            pb = psbf(bnk).rearrange("p (t c) -> p t c", t=4)
            TR([(pb[:, tt_, cc * 128:(cc + 1) * 128], ZT[:, cc, (t0 + tt_) * 128:(t0 + tt_ + 1) * 128]) for tt_ in range(nt_) for cc in range(2)], identb,
               [("ZT", 0), ("ZT", 1), "identb"], [("ps", bnk)])
            for tt_ in range(nt_):
                CP("act", ZTM[:, t0 + tt_, :], pb[:, tt_, :], [("ps", bnk)], [("ZTM", t0 + tt_)])
        for i_, (nm_, tl_) in enumerate((("X0T", X0T), ("ZT", ZT), ("ZTM", ZTM))):
            P.dma(HYD[i_], tl_.rearrange("p a b -> p (a b)"), reads=[k_ for k_ in P.bykey.get(nm_, ())], writes=[("HYD", i_)], eng="pool")
        A.free("X0T", "ZT", "ZTM")
        if done(f"B1{l}"): break

        PTs = [A.alloc(f"PT{i}", [128, 1280]) for i in range(2)]
        RQs = [A.alloc(f"RQ{i}", [128, 1536], BF16) for i in range(2)]
        SQ = A.alloc("SQ", [128, 512]); st8 = A.alloc("st8", [128, 2, 8])
        QKG = A.alloc("QKG", [128, 512]); BINB = A.alloc("BINB", [128, 1280])
        P.dma(QKG, qkg()[l].partition_broadcast(128), writes=["QKG"])
        P.dma(BINB, b_in()[l][768:2048].partition_broadcast(128), writes=["BINB"])
        RC = A.alloc("RC", [128, NT, 32]); RSn = A.alloc("RSn", [128, NT, 32])
        P.dma(RC, ropeC(), writes=["RC"]); P.dma(RSn, ropeS(), writes=["RSn"])
        VAUG = [A.alloc(f"VAUG{i}", [128, 4, 65], BF16) for i in range(2)]
        for i in range(2):
            MS("pool", VAUG[i], 1.0, [f"VAUG{i}"])
        QKT = [A.alloc(f"QKT{i}", [128, 10, 512], BF16) for i in range(2)]
        TMPS = [A.alloc(f"rt{i}", [128, 8, 2, 16]) for i in range(4)]
        tcols = [(0, 128), (128, 256), (256, 384), (384, 512), (1280, 1408), (640, 768), (768, 896), (896, 1024), (1024, 1152), (1408, 1536)]
        for t in range(NT):
            PT = PTs[t % 2]; pk = f"PT{t % 2}"; RQ = RQs[t % 2]; rqk = f"RQ{t % 2}"
            b0 = 3 * (t % 2)
            for pi, (c0, c1) in enumerate(((768, 1280), (1280, 1792), (1792, 2048))):
                MM(PSb[:, b0 + pi, 0:c1 - c0], [(HT[:, k, t * 128:(t + 1) * 128], WIN[:, k, c0:c1]) for k in range(8)], wink + [("HT", t)], [("ps", b0 + pi)])
                TT("dve", PT[:, c0 - 768:c1 - 768], PSb[:, b0 + pi, 0:c1 - c0], BINB[:, c0 - 768:c1 - 768], ALU.add, [("ps", b0 + pi), "BINB"], [(pk, pi)])
            pkall = [(pk, 0), (pk, 1), (pk, 2)]
            sk = ("st8", t % 2)
            ACT(SQ, PT[:, 0:512], AF.Square, [(pk, 0)], ["SQ"])
            P.op("dve", lambda e, o_=st8[:, t % 2, :], i_=SQ.rearrange("p (h d) -> p h d", d=64): e.tensor_reduce(out=o_, in_=i_, axis=AX.X, op=ALU.add), ["SQ"], [sk])
            ACT(st8[:, t % 2, :], st8[:, t % 2, :], AF.Sqrt, [sk], [sk], scale=1.0 / 64, bias=EPS)
            RECIP(st8[:, t % 2, :], [sk])
            pv = PT[:, 0:512].rearrange("p (h d) -> p h d", d=64)
            TT("dve", pv, pv, st8[:, t % 2, :].unsqueeze(2).to_broadcast([128, 8, 64]), ALU.mult, [(pk, 0), sk], [(pk, 0)])
            TT("dve", PT[:, 0:512], PT[:, 0:512], QKG, ALU.mult, [(pk, 0), "QKG"], [(pk, 0)])
            Cb = RC[:, t, :].rearrange("p (a f) -> p a f", a=2).unsqueeze(1).to_broadcast([128, 8, 2, 16])
            Sb = RSn[:, t, :].rearrange("p (a f) -> p a f", a=2).unsqueeze(1).to_broadcast([128, 8, 2, 16])
            for ri, base in enumerate((0, 640)):
                xv = PT[:, base:base + 512].rearrange("p (h a s f) -> p h a s f", h=8, a=2, s=2)
                ov = RQ[:, base:base + 512].rearrange("p (h a s f) -> p h a s f", h=8, a=2, s=2)
                x1 = xv[:, :, :, 0, :]; x2 = xv[:, :, :, 1, :]
                rk = pkall
                tk = [(f"rt{i}", ri) for i in range(4)]
                TT("dve", TMPS[0], x1, Cb, ALU.mult, rk + ["RC"], [tk[0]])
                TT("dve", TMPS[1], x2, Sb, ALU.mult, rk + ["RSn"], [tk[1]])
                TT("dve", ov[:, :, :, 0, :], TMPS[0], TMPS[1], ALU.subtract, [tk[0], tk[1]], [(rqk, ri, 0)])
                TT("pool", TMPS[2], x2, Cb, ALU.mult, rk + ["RC"], [tk[2]])
                TT("dve", TMPS[3], x1, Sb, ALU.mult, rk + ["RSn"], [tk[3]])
                TT("pool", ov[:, :, :, 1, :], TMPS[2], TMPS[3], ALU.add, [tk[2], tk[3]], [(rqk, ri, 1)])
            rqall = [(rqk, 0, 0), (rqk, 0, 1), (rqk, 1, 0), (rqk, 1, 1)]
            for (dst0, s0) in ((1280, 448), (1344, 384), (1408, 1088), (1472, 1024)):
                CP("pool", RQ[:, dst0:dst0 + 64], RQ[:, s0:s0 + 64], rqall, [(rqk, "sw", dst0)])
            rqall = rqall + [(rqk, "sw", d_) for d_ in (1280, 1344, 1408, 1472)]
            va = VAUG[t % 2]; vk = f"VAUG{t % 2}"
            CP("act", va[:, 0:2, 0:64], PT[:, 512:640].rearrange("p (h d) -> p h d", h=2), [(pk, 1)], [vk])
            CP("act", va[:, 2:4, 0:64], PT[:, 1152:1280].rearrange("p (h d) -> p h d", h=2), [(pk, 2)], [vk])
            P.dma(VD[t], va.rearrange("p a b -> p (a b)"), reads=[vk], writes=[("VD", t)], eng="pool")
            pb = psbf(6, 2)
            TR([(pb[:, i * 128:(i + 1) * 128], RQ[:, c0:c1]) for i, (c0, c1) in enumerate(tcols)], identb, rqall + ["identb"], [("ps", 6)])
            g4 = t // 4; qt = QKT[g4 % 2]; qtk = f"QKT{g4 % 2}"
            CP("act", qt[:, :, (t % 4) * 128:(t % 4 + 1) * 128], pb[:, 0:1280].rearrange("p (i c) -> p i c", i=10), [("ps", 6)], [(qtk, t % 4)])
            if t % 4 == 3 or t == NT - 1:
                t0 = (t // 4) * 4; nn = (t - t0 + 1) * 128
                P.dma(QKD[:, :, t0 * 128:t0 * 128 + nn].rearrange("i p n -> p i n"), qt[:, :, 0:nn], reads=[(qtk, i) for i in range(t - t0 + 1)], writes=[("QKD", t // 4)], eng="pool")
        A.free("PT0", "PT1", "RQ0", "RQ1", "SQ", "st8", "QKG", "BINB", "RC", "RSn", "VAUG0", "VAUG1", "QKT0", "QKT1", "rt0", "rt1", "rt2", "rt3")
        A.free("HT", "WIN")
        if done(f"B2{l}"): break

        X0T = A.alloc("X0T", [128, 2, T], BF16); ZT = A.alloc("ZT", [128, 2, T], BF16); ZTM = A.alloc("ZTM", [128, NT, 256], BF16)
        P.dma(X0T.rearrange("p a b -> p (a b)"), HYD[0], reads=[("HYD", 0)], writes=[("X0T", 0), ("X0T", 1)])
        P.dma(ZT.rearrange("p a b -> p (a b)"), HYD[1], reads=[("HYD", 1)], writes=[("ZT", 0), ("ZT", 1)])
        P.dma(ZTM.rearrange("p a b -> p (a b)"), HYD[2], reads=[("HYD", 2)], writes=[("ZTM", t_) for t_ in range(NT)])
        hyena(l, "L", 0, X0T, ZT, ZTM)
        if not last:
            hyena(l, "C", NLAT, X0T, ZT, ZTM)
        A.free("X0T", "ZT", "ZTM")
        if done(f"C{l}"): break

        attention(l, "ga", last)
        if done(f"Dga{l}"): break
        attention(l, "wa", last)
        if done(f"D{l}"): break

        YN = A.alloc("YN", [128, 8, T], BF16)
        ynr = [("YNT", 0, "L", tc) for tc in range(NLAT // 256)] + ([] if last else [("YNT", 0, "C", 0)]) + \
              [("YNT", br, ci) for br in ("ga", "wa") for ci in range(8 if last else 9)]
        P.dma(YN, YNT.rearrange("c p n -> p c n"), reads=ynr, writes=["YN"])
        WO = A.alloc("WO", [128, 8, D], BF16)
        wstg = [A.alloc(f"wstg{i}", [128, D]) for i in range(2)]
        for k in range(8):
            P.dma(wstg[k % 2], w_out()[l][k * 128:(k + 1) * 128, :], writes=[f"wstg{k % 2}"])
            CP("act" if k % 2 == 0 else "pool", WO[:, k, :], wstg[k % 2], [f"wstg{k % 2}"], [("WO", k)])
        A.free("wstg0", "wstg1")
        wok = [("WO", k) for k in range(8)]
        BO = A.alloc("BO", [128, D]); G1 = A.alloc("G1", [128, 2, D])
        P.dma(BO, b_out()[l].partition_broadcast(128), writes=["BO"])
        for v in range(2):
            modd_load(G1[:, v, :], v, 2, ("G1", v))
        xts = [A.alloc(f"xt{i}", [128, D]) for i in range(2)]
        tm = [A.alloc(f"tm{i}", [128, D]) for i in range(2)]
        for t in range(ntl_l):
            v = 0 if t < NLT else 1
            xt = xts[t % 2]; xk = f"xt{t % 2}"; tmp = tm[t % 2]; tk = f"tm{t % 2}"
            src, rk = xsrc(l, t)
            P.dma(xt, src, reads=rk, writes=[xk])
            for hf in range(2):
                bk = ("ps", (t % 2) * 2 + hf)
                MM(PSb[:, (t % 2) * 2 + hf, :], [(YN[:, k, t * 128:(t + 1) * 128], WO[:, k, hf * 512:(hf + 1) * 512]) for k in range(8)], ["YN"] + wok, [bk])
                TT("dve", tmp[:, hf * 512:(hf + 1) * 512], PSb[:, (t % 2) * 2 + hf, :], BO[:, hf * 512:(hf + 1) * 512], ALU.add, [bk, "BO"], [(tk, hf)])
            TT("pool", tmp, tmp, G1[:, v, :], ALU.mult, [(tk, 0), (tk, 1), ("G1", v)], [(tk, 0), (tk, 1)])
            TT("pool", xt, xt, tmp, ALU.add, [xk, (tk, 0), (tk, 1)], [xk])
            P.dma(X1[t * 128:(t + 1) * 128, :], xt, reads=[xk], writes=[("X1", t)], eng="pool")
        A.free("YN", "WO", "BO", "G1", "xt0", "xt1", "tm0", "tm1")
        if done(f"E{l}"): break

        GT = A.alloc("GT", [32, T])
        a2 = A.alloc("a2", [128, 2, D]); s2 = A.alloc("s2", [128, 2, D])
        for v in range(2):
            modd_load(a2[:, v, :], v, 4, ("a2", v)); modd_load(s2[:, v, :], v, 3, ("s2", v))
        WR = A.alloc("WR", [128, 8, NE]); BR = A.alloc("BR", [128, NE])
        P.dma(WR, w_router()[l].rearrange("(k p) e -> p k e", p=128), writes=["WR"])
        P.dma(BR, b_router()[l].partition_broadcast(128), writes=["BR"])
        H2D = dscr(f"H2D{l}", [8, 128, T], BF16)
        xts = [A.alloc(f"xt{i}", [128, D]) for i in range(2)]
        junk = A.alloc("junk", [128, D]); h2f = A.alloc("h2f", [128, 8, 128])
        h2b = [A.alloc(f"h2b{i}", [128, 8, 128], BF16) for i in range(2)]
        st = A.alloc("stat", [128, 4]); lg = A.alloc("lg", [128, NE]); m8 = A.alloc("m8", [128, 8]); msk = A.alloc("msk", [128, NE])
        gs = A.alloc("gs", [128, 4])
        for t in range(ntl_l):
            v = 0 if t < NLT else 1
            xt = xts[t % 2]; xk = f"xt{t % 2}"
            P.dma(xt, X1[t * 128:(t + 1) * 128, :], reads=[("X1", t)], writes=[xk])
            sq = st[:, (t % 2) * 2:(t % 2) * 2 + 1]; rs = st[:, (t % 2) * 2 + 1:(t % 2) * 2 + 2]; sk = ("stat", t % 2)
            ACT(junk, xt, AF.Square, [xk], ["junk", sk], accum_out=sq)
            rstd_from_ssq(sq, rs, 1.0 / D, [sk], [sk])
            STT("dve", xt, xt, rs, a2[:, v, :], ALU.mult, ALU.mult, [xk, sk, ("a2", v)], [xk])
            TT("pool", xt, xt, s2[:, v, :], ALU.add, [xk, ("s2", v)], [xk])
            pf = PSb[:, 0:2, :].rearrange("p b n -> p (b n)").rearrange("p (k c) -> p k c", k=8)
            TR([(pf[:, k, :], xt[:, k * 128:(k + 1) * 128]) for k in range(8)], identf, [xk, "identf"], [("ps", 0), ("ps", 1)])
            CP("act", h2f, pf, [("ps", 0), ("ps", 1)], ["h2f"])
            hb_ = h2b[t % 2]; hbk = f"h2b{t % 2}"
            CP("pool", hb_, h2f, ["h2f"], [hbk])
            P.dma(H2D[:, :, t * 128:(t + 1) * 128].rearrange("k p n -> p k n"), hb_, reads=[hbk], writes=[("H2D", t)], eng="pool")
            MM(PSb[:, 2, 0:NE], [(h2f[:, k, :], WR[:, k, :]) for k in range(8)], ["h2f", "WR"], [("ps", 2)])
            TT("dve", lg, PSb[:, 2, 0:NE], BR, ALU.add, [("ps", 2), "BR"], ["lg"])
            P.op("dve", lambda e: e.max(out=m8, in_=lg), ["lg"], ["m8"])
            TS("dve", msk, lg, m8[:, 3:4], None, ALU.is_ge, None, ["lg", "m8"], ["msk"])
            TS("dve", gs[:, 0:1], m8[:, 0:1], -1.0, None, ALU.mult, None, ["m8"], ["gs"])
            ACT(lg, lg, AF.Exp, ["lg", "gs"], ["lg"], bias=gs[:, 0:1])
            TT("dve", lg, lg, msk, ALU.mult, ["lg", "msk"], ["lg"])
            P.op("dve", lambda e: e.tensor_reduce(out=gs[:, 1:2], in_=lg, axis=AX.X, op=ALU.add), ["lg"], ["gs"])
            RECIP(gs[:, 1:2], ["gs"])
            TS("dve", lg, lg, gs[:, 1:2], None, ALU.mult, None, ["lg", "gs"], ["lg"])
            TR([(PSb[0:32, 3, 0:128], lg)], identf, ["lg", "identf"], [("ps", 3)])
            CP("act", GT[:, t * 128:(t + 1) * 128], PSb[0:32, 3, 0:128], [("ps", 3)], [("GT", t)])
        gtk = [("GT", t) for t in range(ntl_l)]
        ntok = ntl_l * 128
        P.dma(GATD[:, 0:ntok], GT[:, 0:ntok], reads=gtk, writes=["GATD"], eng="pool")
        A.free("a2", "s2", "WR", "BR", "xt0", "xt1", "junk", "h2f", "h2b0", "h2b1", "stat", "lg", "m8", "msk", "gs", "GT")
        if done(f"F1{l}"): break

        B2f = A.alloc("B2f", [32, D])
        P.dma(B2f, b_mlp2()[l], writes=["B2f"])
        B1 = A.alloc("B1", [128, NE, 16])
        P.dma(B1, b1T()[:, l], writes=["B1"])
        TS("dve", B1[:, :, 8:16], B1[:, :, 8:16], 1.0, None, ALU.add, None, ["B1"], ["B1"])
        G2 = A.alloc("G2", [128, 2, D])
        for v in range(2):
            modd_load(G2[:, v, :], v, 5, ("G2", v))
        NFb = A.alloc("NFb", [128, D])
        if last:
            P.dma(NFb, norm_final().partition_broadcast(128), writes=["NFb"])
        SCT = 9
        W1 = A.alloc("W1", [128, 8, 2048], BF16); W2 = A.alloc("W2", [128, 8, D], BF16)
        stg = [A.alloc(f"mstg{i}", [128, 2048]) for i in range(2)]
        ACC = A.alloc("ACC", [128, 8, SCT * 128]); H2T = A.alloc("H2T", [128, 8, SCT * 128], BF16)
        GB = A.alloc("GB", [128, SCT * 128]); GTs = A.alloc("GTs", [32, SCT * 128])
        Gt = [A.alloc(f"mG{i}", [128, 512]) for i in range(2)]; St = [A.alloc(f"mS{i}", [128, 512]) for i in range(2)]
        Lt = [A.alloc(f"mL{i}", [128, 512]) for i in range(2)]; Xt = [A.alloc(f"mX{i}", [128, 512]) for i in range(2)]
        Ub = A.alloc("mU", [128, 8, SCT * 128], BF16)
        w1k = [("W1", k) for k in range(8)]; w2k = [("W2", k) for k in range(8)]
        sgi = 0; wi = 0
        for sc0 in range(0, ntl_l, SCT):
            nts = min(SCT, ntl_l - sc0); tk0 = sc0 * 128; ntk = nts * 128
            P.dma(H2T[:, :, 0:ntk], H2D[:, :, tk0:tk0 + ntk].rearrange("k p n -> p k n"), reads=[("H2D", t) for t in range(sc0, sc0 + nts)], writes=["H2T"])
            P.dma(GTs[:, 0:ntk], GATD[:, tk0:tk0 + ntk], reads=["GATD"], writes=["GTs"])
            cks = [(c0, min(512, ntk - c0)) for c0 in range(0, ntk, 512)]
            for (c0, w_) in cks:
                for dc in range(8):
                    bk = ("ps", 6 + dc % 2)
                    MM(PSb[:, 6 + dc % 2, 0:w_], [(B2f[:, dc * 128:(dc + 1) * 128], GTs[:, c0:c0 + w_])], ["B2f", "GTs"], [bk])
                    CP("act", ACC[:, dc, c0:c0 + w_], PSb[:, 6 + dc % 2, 0:w_], [bk], [("ACC", dc, c0)])
            for e_ in range(NE):
                for k in range(8):
                    sb_ = stg[sgi % 2]; sbk = f"mstg{sgi % 2}"; sgi += 1
                    P.dma(sb_, w_mlp1()[l, e_, k * 128:(k + 1) * 128, :], writes=[sbk])
                    CP("act", W1[:, k, :], sb_, [sbk], [("W1", k)])
                for k in range(0, 8, 2):
                    sb_ = stg[sgi % 2]; sbk = f"mstg{sgi % 2}"; sgi += 1
                    P.dma(sb_.rearrange("p (a n) -> p a n", a=2), w_mlp2()[l, e_, k * 128:(k + 2) * 128, :].rearrange("(a p) n -> p a n", p=128), writes=[sbk])
                    CP("act", W2[:, k:k + 2, :], sb_.rearrange("p (a n) -> p a n", a=2), [sbk], [("W2", k), ("W2", k + 1)])
                P.dma(GB[:, 0:ntk], GATD[e_, tk0:tk0 + ntk].partition_broadcast(128), reads=["GATD"], writes=["GB"])
                for (c0, w_) in cks:
                    for jf in range(8):
                        bg = jf % 2; bl = 2 + jf % 2; wb = wi % 2; wi += 1
                        MM(PSb[:, bg, 0:w_], [(W1[:, k, jf * 128:(jf + 1) * 128], H2T[:, k, c0:c0 + w_]) for k in range(8)], w1k + ["H2T"], [("ps", bg)])
                        MM(PSb[:, bl, 0:w_], [(W1[:, k, 1024 + jf * 128:1024 + (jf + 1) * 128], H2T[:, k, c0:c0 + w_]) for k in range(8)], w1k + ["H2T"], [("ps", bl)])
                        G_ = Gt[wb][:, 0:w_]; S_ = St[wb][:, 0:w_]; L_ = Lt[wb][:, 0:w_]; X_ = Xt[wb][:, 0:w_]
                        gk_ = f"mG{wb}"; sk_ = f"mS{wb}"; lk_ = f"mL{wb}"; xk_ = f"mX{wb}"
                        TS("dve", G_, PSb[:, bg, 0:w_], B1[:, e_, jf:jf + 1], 7.0, ALU.add, ALU.min, [("ps", bg), "B1"], [gk_])
                        ACT(S_, G_, AF.Sigmoid, [gk_], [sk_], scale=1.702)
                        TS("dve", L_, PSb[:, bl, 0:w_], B1[:, e_, 8 + jf:9 + jf], 8.0, ALU.add, ALU.min, [("ps", bl), "B1"], [lk_])
                        TT("pool", X_, G_, S_, ALU.mult, [gk_, sk_], [xk_])
                        STT("dve", L_, L_, -6.0, X_, ALU.max, ALU.mult, [lk_, xk_], [lk_])
                        TT("dve", Ub[:, jf, c0:c0 + w_], L_, GB[:, c0:c0 + w_], ALU.mult, [lk_, "GB"], [("mU", jf, c0)])
                for (c0, w_) in cks:
                    for dc in range(8):
                        bk = ("ps", 4 + dc % 2)
                        MM(PSb[:, 4 + dc % 2, 0:w_], [(W2[:, jf, dc * 128:(dc + 1) * 128], Ub[:, jf, c0:c0 + w_]) for jf in range(8)], w2k + [("mU", jf, c0) for jf in range(8)], [bk])
                        TT("dve", ACC[:, dc, c0:c0 + w_], ACC[:, dc, c0:c0 + w_], PSb[:, 4 + dc % 2, 0:w_], ALU.add, [bk, ("ACC", dc, c0)], [("ACC", dc, c0)])
            acck = [("ACC", dc, c0) for dc in range(8) for (c0, _) in cks]
            for tt_ in range(nts):
                t = sc0 + tt_; v = 0 if t < NLT else 1
                xt = stg[tt_ % 2][:, 0:D]; xk = f"mstg{tt_ % 2}"
                P.dma(xt, X1[t * 128:(t + 1) * 128, :], reads=[("X1", t)], writes=[xk])
                pf = PSb[:, 6:8, :].rearrange("p b n -> p (b n)").rearrange("p (k c) -> p k c", k=8)
                TR([(pf[:, k, :], ACC[:, k, tt_ * 128:(tt_ + 1) * 128]) for k in range(8)], identf, acck + ["identf"], [("ps", 6), ("ps", 7)])
                y_ = stg[tt_ % 2][:, D:2 * D]
                TT("dve", y_, PSb[:, 6:8, :].rearrange("p b n -> p (b n)"), G2[:, v, :], ALU.mult, [("ps", 6), ("ps", 7), ("G2", v)], [xk])
                TT("pool", xt, xt, y_, ALU.add, [xk], [xk])
                if DBG["DBGM"] is not None and l == 0:
                    pass
                if not last:
                    P.dma(XR[t * 128:(t + 1) * 128, :], xt, reads=[xk], writes=[("XR", t)], eng="pool")
                else:
                    sq = Gt[0][:, 0:1]; rs = Gt[0][:, 1:2]
                    ACT(y_, xt, AF.Square, [xk], [xk, "mG0"], accum_out=sq)
                    rstd_from_ssq(sq, rs, 1.0 / D, ["mG0"], ["mG0"])
                    STT("dve", xt, xt, rs, NFb, ALU.mult, ALU.mult, [xk, "mG0", "NFb"], [xk])
                    P.dma(out[t * 128:(t + 1) * 128, :], xt, reads=[xk], writes=[("out", t)], eng="pool")
        A.free("B2f", "B1", "G2", "NFb", "W1", "W2", "mstg0", "mstg1", "ACC", "H2T", "GB", "GTs", "mG0", "mG1", "mS0", "mS1", "mL0", "mL1", "mX0", "mX1", "mU")
        if done(f"F{l}"): break

    P.emit()
    P.close()
    es.close()
    return nc, stages, ins


def make_inputs(inputs, b, names=None):
    global _TB
    if _TB is None:
        _TB = _tables()
    f = lambda a: np.ascontiguousarray(a, dtype=np.float32)
    d = {}
    lay = {
        "x": lambda: f(inputs["x"][b]),
        "ctx": lambda: f(inputs["ctx"][b]),
        "c2T": lambda: f(np.stack([inputs["c"][b], inputs["c_ctx"]], 0).reshape(2, 8, 128).transpose(2, 0, 1)),
        "b_inT": lambda: f(inputs["b_in"][:, :768].reshape(2, 6, 128).transpose(2, 0, 1)),
        "convw": lambda: f(inputs["hy_conv_w"].reshape(2, 3, 6, 128).transpose(3, 0, 1, 2)),
        "convb": lambda: f(inputs["hy_conv_b"].reshape(2, 6, 128).transpose(2, 0, 1)),
        "hyw1": lambda: f(inputs["hy_filt_w1"]), "hyw2": lambda: f(inputs["hy_filt_w2"]), "hyw3": lambda: f(inputs["hy_filt_w3"]),
        "hyvec": lambda: f(np.stack([inputs["hy_filt_b1"], inputs["hy_filt_b2"], inputs["hy_filt_b3"], inputs["hy_filt_freq"]], -1).transpose(1, 0, 2)),
        "hyout": lambda: f(inputs["hy_filt_out"]),
        "hyskip": lambda: f(inputs["hy_skip"].reshape(2, 2, 128).transpose(2, 0, 1)),
        "qkg": lambda: f(np.concatenate([np.tile(inputs["ga_q_norm"], (1, 6)), np.tile(inputs["ga_k_norm"], (1, 2))], 1)),
        "sink": lambda: f(inputs["wa_sink"]),
        "bnorm": lambda: f(inputs["branch_norm"]),
        "bnT": lambda: f(inputs["branch_norm"][:, :256].reshape(2, 2, 128).transpose(2, 0, 1)),
        "b1T": lambda: f(inputs["b_mlp1"].reshape(2, NE, 16, 128).transpose(3, 0, 1, 2)),
    }
    for k in ["w_ada", "b_ada", "norm_mix", "norm_ffn", "w_in", "b_in", "w_out", "b_out", "w_router", "b_router",
              "w_mlp1", "w_mlp2", "b_mlp2", "norm_final"]:
        lay[k] = (lambda k=k: f(inputs[k]))
    for k in (names if names is not None else list(lay.keys()) + list(_TB.keys())):
        d[k] = _TB[k] if k in _TB else lay[k]()
    return d


_SHARED = {}


def kernel(**inputs):
    nc, _, ins = build()
    names = list(ins.keys())
    shared = {}
    in_maps = []
    for b in range(8):
        m = make_inputs(inputs, b, [n for n in names if n in ("x", "ctx", "c2T")])
        if not shared:
            shared = make_inputs(inputs, 0, [n for n in names if n not in ("x", "ctx", "c2T")])
        m.update(shared)
        in_maps.append(m)
    res = run_bass_kernel_spmd(nc, in_maps, core_ids=list(range(8)))
    return np.stack([np.asarray(r["out"], dtype=np.float32) for r in res.results], 0)
```

```python
import math
import numpy as np
import ml_dtypes
import concourse.bass as bass
import concourse.mybir as mybir
from concourse.bass_utils import run_bass_kernel_spmd

F32 = mybir.dt.float32
BF16 = mybir.dt.bfloat16
ALU = mybir.AluOpType
AF = mybir.ActivationFunctionType
AX = mybir.AxisListType

SEM_LIMIT = 30000
DMA_POOL = 6

D = 1024
NLAT = 4096
NCTX = 256
T = NLAT + NCTX
NT = T // 128
NLT = NLAT // 128
EPS = 1e-6
NE = 32


class Op:
    __slots__ = ("eng", "fn", "sem", "val", "deps", "is_dma", "pre")

    def __init__(self, eng, fn, is_dma):
        self.eng = eng
        self.fn = fn
        self.is_dma = is_dma
        self.sem = None
        self.val = 0
        self.deps = []
        self.pre = None


class Prog:
    ENGS = ("pe", "act", "dve", "pool", "sp")

    def __init__(self, nc):
        self.nc = nc
        self.ops = []
        self.last_w = {}
        self.readers = {}
        self.cnt = {e: 0 for e in self.ENGS}
        self.cur_sem = {e: None for e in self.ENGS}
        self.dma_cnt = {e: 0 for e in self.ENGS}
        self.dma_sems = {e: [] for e in self.ENGS}
        self.dma_hist = {e: [] for e in self.ENGS}
        self._semctx = []
        self.bykey = {}

    def _new_sem(self, name):
        ctx = self.nc.semaphore(name)
        s = ctx.__enter__()
        self._semctx.append(ctx)
        return s

    @staticmethod
    def base(k):
        return k[0] if isinstance(k, tuple) else k

    def hazards_of(self, basename):
        ops = []
        for k in self.bykey.get(basename, ()):
            w = self.last_w.get(k)
            if w is not None:
                ops.append(w)
            ops.extend(self.readers.get(k, ()))
        return ops

    def inherit(self, newbase, ops):
        self.readers.setdefault(("#inh", newbase), []).extend(ops)

    def op(self, eng, fn, reads=(), writes=(), dma=False):
        o = Op(eng, fn, dma)
        deps = set()
        for r in reads:
            w = self.last_w.get(r)
            if w is not None:
                deps.add(w)
        for w in writes:
            lw = self.last_w.get(w)
            if lw is not None:
                deps.add(lw)
            for rd in self.readers.get(w, ()):
                deps.add(rd)
            inh = self.readers.get(("#inh", self.base(w)))
            if inh:
                deps.update(inh)
        for w in writes:
            self.last_w[w] = o
            self.readers[w] = []
            self.bykey.setdefault(self.base(w), set()).add(w)
        for r in reads:
            if r in writes:
                continue
            self.readers.setdefault(r, []).append(o)
            self.bykey.setdefault(self.base(r), set()).add(r)
        deps.discard(o)
        o.deps = list(deps)
        if dma:
            j = self.dma_cnt[eng]
            self.dma_cnt[eng] += 1
            if j < DMA_POOL:
                self.dma_sems[eng].append(self._new_sem(f"d_{eng}_{j}"))
            o.sem = self.dma_sems[eng][j % DMA_POOL]
            o.val = 16 * (j // DMA_POOL + 1)
            if j >= DMA_POOL:
                o.pre = self.dma_hist[eng][j - DMA_POOL]
            self.dma_hist[eng].append(o)
        else:
            if self.cur_sem[eng] is None or self.cnt[eng] >= SEM_LIMIT:
                self.cur_sem[eng] = self._new_sem(f"c_{eng}_{len(self._semctx)}")
                self.cnt[eng] = 0
            self.cnt[eng] += 1
            o.sem = self.cur_sem[eng]
            o.val = self.cnt[eng]
        self.ops.append(o)
        return o

    def dma(self, out, in_, reads=(), writes=(), eng="sp", **kw):
        return self.op(eng, lambda e: e.dma_start(out=out, in_=in_, **kw), reads, writes, dma=True)

    def emit(self):
        nc = self.nc
        per = {e: [] for e in self.ENGS}
        for o in self.ops:
            per[o.eng].append(o)
        engmap = {"pe": "tensor", "act": "scalar", "dve": "vector", "pool": "gpsimd", "sp": "sync"}

        def run(eng_name):
            def body(e):
                seen = {}
                for o in per[eng_name]:
                    waits = {}
                    dl = list(o.deps)
                    if o.pre is not None:
                        dl.append(o.pre)
                    for d in dl:
                        if d.eng == eng_name and eng_name == "pe" and not d.is_dma:
                            continue
                        k = id(d.sem)
                        if seen.get(k, 0) >= d.val:
                            continue
                        if k not in waits or waits[k][1] < d.val:
                            waits[k] = (d.sem, d.val)
                    for k, (s, v) in waits.items():
                        e.wait_ge(s, v)
                        seen[k] = v
                    ins = o.fn(e)
                    ins.then_inc(o.sem, 16 if o.is_dma else 1)
                tail = {}
                for o in per[eng_name]:
                    if o.is_dma:
                        k = id(o.sem)
                        if k not in tail or tail[k][1] < o.val:
                            tail[k] = (o.sem, o.val)
                for k, (s, v) in tail.items():
                    if seen.get(k, 0) < v:
                        e.wait_ge(s, v)
            return body

        with nc.Block() as block:
            for en in self.ENGS:
                if per[en]:
                    getattr(block, engmap[en])(run(en))

    def close(self):
        for c in reversed(self._semctx):
            c.__exit__(None, None, None)
        self._semctx = []


class Arena:
    def __init__(self, P, tensor, nwords):
        self.P = P
        self.t = tensor
        self.n = nwords
        self.live = {}
        self.dead = []

    def alloc(self, name, shape, dtype=F32):
        per = 1
        for s in shape[1:]:
            per *= s
        words = per if dtype == F32 else (per + 1) // 2
        segs = sorted(self.live.values())
        lo = 0
        pos = None
        for (a, b) in segs + [(self.n, self.n)]:
            if a - lo >= words:
                pos = lo
                break
            lo = max(lo, b)
        if pos is None:
            raise RuntimeError(f"arena full allocating {name} {words} words; live={self.live}")
        hi = pos + words
        self.live[name] = (pos, hi)
        ops = []
        nd = []
        for (a, b, o) in self.dead:
            if a < hi and pos < b:
                ops.extend(o)
            nd.append((a, b, o))
        self.dead = nd
        if ops:
            self.P.inherit(name, ops)
        v = self.t[:shape[0], pos:hi]
        if dtype != F32:
            v = v.bitcast(dtype)[:, 0:per]
        if len(shape) > 2:
            names = " ".join(f"d{i}" for i in range(len(shape) - 1))
            kw = {f"d{i}": shape[i + 1] for i in range(len(shape) - 1)}
            v = v.rearrange(f"p ({names}) -> p {names}", **kw)
        return v

    def free(self, *names):
        for name in names:
            lo, hi = self.live.pop(name)
            self.dead.append((lo, hi, self.P.hazards_of(name)))


def _tables():
    tb = {}
    rows = NLAT // 64
    row = np.repeat(np.arange(rows, dtype=np.float32), 64)
    col = np.tile(np.arange(64, dtype=np.float32), rows)
    pos = np.stack([row, col], -1)
    inv = (10000.0 ** (-np.arange(16, dtype=np.float32) / 16)).astype(np.float32)
    ang = pos[:, :, None] * inv
    cos = np.ones((T, 2, 16), np.float32)
    sin = np.zeros((T, 2, 16), np.float32)
    cos[:NLAT] = np.cos(ang)
    sin[:NLAT] = np.sin(ang)
    tb["ropeC"] = np.ascontiguousarray(cos.reshape(NT, 128, 32).transpose(1, 0, 2))
    tb["ropeS"] = np.ascontiguousarray(sin.reshape(NT, 128, 32).transpose(1, 0, 2))
    tb["ident"] = np.eye(128, dtype=np.float32)
    a = np.arange(128)
    m = np.zeros((128, 2, 128), np.float32)
    m[:, 0, :] = (a[None, :] <= a[:, None])
    m[:, 1, :] = (a[:, None] <= a[None, :])
    tb["wmask"] = m.astype(ml_dtypes.bfloat16)
    for n, tag in ((NLAT, "L"), (NCTX, "C")):
        nch = n // 128
        t = np.linspace(0.0, 1.0, n, dtype=np.float32)[:, None]
        wpos = (2.0 * math.pi / n) * np.arange(n, dtype=np.float32)[:, None]
        bands = np.linspace(1e-4, 15, 16, dtype=np.float32)
        z = np.concatenate([t, np.cos(wpos * bands), -np.sin(wpos * bands)], -1).astype(np.float32)
        zz = np.concatenate([z, z[::-1]], 0)
        tb["hyz" + tag] = np.ascontiguousarray(zz.T)
        deltas = np.linspace(math.log(1e-2) / 1.5, math.log(1e-2) / 0.3, 256, dtype=np.float32)
        decay = np.exp(-t * np.abs(deltas)).astype(np.float32)
        dd = np.stack([decay, decay[::-1]], 1)
        tb["hydec" + tag] = np.ascontiguousarray(dd.reshape(nch, 128, 512).transpose(1, 0, 2))
        N2 = 2 * n
        s = np.arange(n, dtype=np.int64)
        ph = (np.outer(s, s) % N2).astype(np.float64) * (2 * math.pi / N2)
        Fc = np.cos(ph)
        Fs = -np.sin(ph)
        Gs = Fs.copy()
        Fs[:, 0] = (-1.0) ** s
        Gs[0, :] = (-1.0) ** s
        def fwd_layout(M):
            return np.ascontiguousarray(M.reshape(nch, 128, nch, 128).transpose(2, 1, 0, 3).reshape(nch, 128, nch * 128)).astype(ml_dtypes.bfloat16)
        TC = 256
        ntc = n // TC
        def inv_layout(M):
            return np.ascontiguousarray(M.reshape(nch, 128, ntc, TC).transpose(2, 1, 0, 3).reshape(ntc, 128, nch * TC)).astype(ml_dtypes.bfloat16)
        tb["Fc" + tag] = fwd_layout(Fc)
        tb["Fs" + tag] = fwd_layout(Fs)
        tb["Gc" + tag] = inv_layout(Fc)
        tb["Gs" + tag] = inv_layout(Gs)
        sc = np.full((128, nch), 2.0 / N2, np.float32)
        sc[0, 0] = 1.0 / N2
        sg = np.where(np.arange(128) % 2 == 0, 1.0, -1.0).astype(np.float32)[:, None]
        tb["hysc" + tag] = np.ascontiguousarray(np.concatenate([sc, sg], 1))
    return tb


_TB = None


def build(nlayers=2, stop=None, dbg=()):
    nc = bass.Bass("TRN2", target_bir_lowering=False)
    ins = {}

    class Lazy:
        def __init__(self, name, shape, dt=F32):
            self.name, self.shape, self.dt, self.ap = name, list(shape), dt, None

        def __call__(self):
            if self.ap is None:
                self.ap = nc.dram_tensor(self.name, self.shape, self.dt, kind="ExternalInput").ap()
                ins[self.name] = self.ap
            return self.ap

    def din(name, shape, dt=F32):
        return Lazy(name, shape, dt)

    def dscr(name, shape, dt=F32):
        kind = "ExternalOutput" if name in dbg else "Internal"
        return nc.dram_tensor(name, list(shape), dt, kind=kind).ap()

    x_in = din("x", [NLAT, D]); ctx_in = din("ctx", [NCTX, D]); c2T = din("c2T", [128, 2, 8])
    w_ada = din("w_ada", [2, D, 6 * D]); b_ada = din("b_ada", [2, 6 * D])
    norm_mix = din("norm_mix", [2, D]); norm_ffn = din("norm_ffn", [2, D])
    w_in = din("w_in", [2, D, 2048]); b_in = din("b_in", [2, 2048]); b_inT = din("b_inT", [128, 2, 6])
    convw = din("convw", [128, 2, 3, 6]); convb = din("convb", [128, 2, 6])
    hyw1 = din("hyw1", [2, 33, 64]); hyw2 = din("hyw2", [2, 64, 64]); hyw3 = din("hyw3", [2, 64, 64])
    hyvec = din("hyvec", [64, 2, 4]); hyout = din("hyout", [2, 64, 512]); hyskip = din("hyskip", [128, 2, 2])
    qkg = din("qkg", [2, 512]); sink = din("sink", [2, 6]); bnorm = din("bnorm", [2, D]); bnT = din("bnT", [128, 2, 2])
    w_out = din("w_out", [2, D, D]); b_out = din("b_out", [2, D])
    w_router = din("w_router", [2, D, NE]); b_router = din("b_router", [2, NE])
    w_mlp1 = din("w_mlp1", [2, NE, D, 2 * D]); b1T = din("b1T", [128, 2, NE, 16])
    w_mlp2 = din("w_mlp2", [2, NE, D, D]); b_mlp2 = din("b_mlp2", [2, NE, D])
    norm_final = din("norm_final", [D])
    ropeC = din("ropeC", [128, NT, 32]); ropeS = din("ropeS", [128, NT, 32])
    ident_in = din("ident", [128, 128]); wmask_in = din("wmask", [128, 2, 128], BF16)
    HN = {"L": NLAT, "C": NCTX}
    hyz = {g: din("hyz" + g, [33, 2 * HN[g]]) for g in "LC"}
    hydec = {g: din("hydec" + g, [128, HN[g] // 128, 512]) for g in "LC"}
    hysc = {g: din("hysc" + g, [128, HN[g] // 128 + 1]) for g in "LC"}
    Fc = {g: din("Fc" + g, [HN[g] // 128, 128, HN[g]], BF16) for g in "LC"}
    Fs = {g: din("Fs" + g, [HN[g] // 128, 128, HN[g]], BF16) for g in "LC"}
    Gc = {g: din("Gc" + g, [HN[g] // 256, 128, HN[g] * 2], BF16) for g in "LC"}
    Gs = {g: din("Gs" + g, [HN[g] // 256, 128, HN[g] * 2], BF16) for g in "LC"}

    out = nc.dram_tensor("out", [NLAT, D], F32, kind="ExternalOutput").ap()
    MODD = dscr("MODD", [2, 6, 128, D])
    XR = dscr("XR", [T, D]); X1 = dscr("X1", [T, D])
    QKD = dscr("QKD", [10, 128, T], BF16)
    VD = dscr("VD", [NT, 128, 260], BF16)
    YNT = dscr("YNT", [8, 128, T], BF16)
    GATD = dscr("GATD", [NE, T])
    HYD = dscr("HYD", [3, 128, 2 * T], BF16)
    DBG = {k: None for k in ("DBGH", "DBGU", "DBGK", "DBGY", "DBGM")}
    if "DBGH" in dbg: DBG["DBGH"] = dscr("DBGH", [T, D], BF16)
    if "DBGK" in dbg: DBG["DBGK"] = dscr("DBGK", [NLAT, 512], BF16)
    if "DBGM" in dbg: DBG["DBGM"] = dscr("DBGM", [D, T])

    from contextlib import ExitStack
    es = ExitStack()
    SBW = 49152 - 1024
    Abig = es.enter_context(nc.sbuf_tensor("arena", [128, SBW], F32))
    PSb = es.enter_context(nc.psum_tensor("psum", [128, 8, 512], F32))
    P = Prog(nc)
    A = Arena(P, Abig, SBW)

    def psbf(bank, nb=1):
        return PSb[:, bank:bank + nb, :].rearrange("p b n -> p (b n)").bitcast(BF16)

    def TT(eng, out_, a, b, op, r, w):
        P.op(eng, lambda e: e.tensor_tensor(out=out_, in0=a, in1=b, op=op), r, w)

    def TS(eng, out_, a, s1, s2, op0, op1, r, w):
        if s2 is None:
            P.op(eng, lambda e: e.tensor_scalar(out=out_, in0=a, scalar1=s1, scalar2=None, op0=op0), r, w)
        else:
            P.op(eng, lambda e: e.tensor_scalar(out=out_, in0=a, scalar1=s1, scalar2=s2, op0=op0, op1=op1), r, w)

    def STT(eng, out_, a, s, b, op0, op1, r, w):
        P.op(eng, lambda e: e.scalar_tensor_tensor(out=out_, in0=a, scalar=s, in1=b, op0=op0, op1=op1), r, w)

    def ACT(out_, a, func, r, w, **kw):
        P.op("act", lambda e: e.activation(out=out_, in_=a, func=func, **kw), r, w)

    def CP(eng, out_, a, r, w):
        if eng == "act":
            P.op("act", lambda e: e.activation(out=out_, in_=a, func=AF.Copy), r, w)
        else:
            P.op(eng, lambda e: e.tensor_copy(out=out_, in_=a), r, w)

    def MM(out_, pairs, r, w):
        def f(e):
            n = len(pairs)
            for i, (l_, rh) in enumerate(pairs):
                ins_ = e.matmul(out_, lhsT=l_, rhs=rh, start=(i == 0), stop=(i == n - 1))
            return ins_
        P.op("pe", f, r, w)

    def MM1(out_, l_, rh, start, stop, r, w):
        P.op("pe", lambda e: e.matmul(out_, lhsT=l_, rhs=rh, start=start, stop=stop), r, w)

    def TR(outs_ins, ident, r, w):
        def f(e):
            for (o_, i_) in outs_ins:
                ins_ = e.transpose(out=o_, in_=i_, identity=ident)
            return ins_
        P.op("pe", f, r, w)

    def MS(eng, ap, val, w):
        P.op(eng, lambda e: e.memset(ap, val), (), w)

    def RECIP(ap, k):
        P.op("dve", lambda e: e.reciprocal(out=ap, in_=ap), k, k)

    def rstd_from_ssq(ssq, rs, scale, keyr, keyw):
        ACT(rs, ssq, AF.Sqrt, keyr, keyw, scale=scale, bias=EPS)
        RECIP(rs, keyw)

    identf = A.alloc("identf", [128, 128]); identb = A.alloc("identb", [128, 128], BF16)
    P.dma(identf, ident_in(), writes=["identf"])
    CP("dve", identb, identf, ["identf"], ["identb"])
    onesf = A.alloc("onesf", [128, 128])
    MS("pool", onesf, 1.0, ["onesf"])
    sil = A.alloc("sil", [128, 2, 8])
    P.dma(sil, c2T(), writes=["sil"])
    ACT(sil, sil, AF.Silu, ["sil"], ["sil"])

    stages = []

    def done(name):
        stages.append(name)
        return stop == name

    def modd_load(dst, v, j, key):
        P.dma(dst, MODD[v, j], reads=[("MODD", v, j, 0), ("MODD", v, j, 1)], writes=[key])

    def xsrc(l, t):
        if l == 0:
            return (x_in()[t * 128:(t + 1) * 128, :] if t < NLT else ctx_in()[(t - NLT) * 128:(t - NLT + 1) * 128, :]), []
        return XR[t * 128:(t + 1) * 128, :], [("XR", t)]

    def hyena(l, tag, tok0, X0T, ZT, ZTM):
        n = HN[tag]; nch = n // 128; TC = 256; ntc = n // TC
        KF = A.alloc("KF", [128, nch, 512], BF16)
        w1 = A.alloc("hw1", [33, 64]); w2 = A.alloc("hw2", [64, 64]); w3 = A.alloc("hw3", [64, 64])
        wo = A.alloc("hwo", [64, 512]); hv = A.alloc("hv", [64, 8])
        P.dma(w1, hyw1()[l], writes=["hw1"]); P.dma(w2, hyw2()[l], writes=["hw2"]); P.dma(w3, hyw3()[l], writes=["hw3"])
        P.dma(wo, hyout()[l], writes=["hwo"]); P.dma(hv[:, 0:4], hyvec()[:, l, :], writes=["hv"])
        for j in range(3):
            TT("dve", hv[:, 4 + j:5 + j], hv[:, j:j + 1], hv[:, 3:4], ALU.mult, ["hv"], ["hv"])
        CW = min(512, n)
        hz = [A.alloc(f"hz{i}", [33, CW]) for i in range(2)]
        hh = [A.alloc(f"hh{i}", [64, CW]) for i in range(3)]
        hm = A.alloc("hm", [64, CW])
        dec = [A.alloc(f"dec{i}", [128, CW // 128, 256]) for i in range(2)]
        ci = 0
        for half in range(2):
            for c0 in range(0, n, CW):
                zb = hz[ci % 2]; zk = f"hz{ci % 2}"
                P.dma(zb, hyz[tag]()[:, half * n + c0: half * n + c0 + CW], writes=[zk])
                db = dec[ci % 2]; dk = f"dec{ci % 2}"
                P.dma(db, hydec[tag]()[:, c0 // 128:(c0 + CW) // 128, half * 256:(half + 1) * 256], writes=[dk])
                src = zb; srck = zk; ws = [w1, w2, w3]; wks = ["hw1", "hw2", "hw3"]
                for j in range(3):
                    bk = ("ps", 4 + (j % 2))
                    pso = PSb[0:64, 4 + (j % 2), 0:CW]
                    MM(pso, [(ws[j], src)], [wks[j], srck], [bk])
                    hk = f"hh{j}"
                    TS("dve", hh[j], pso, hv[:, 3:4], hv[:, 4 + j:5 + j], ALU.mult, ALU.add, [bk, "hv"], [hk])
                    TS("pool", hm, hh[j], math.pi, 2.0 * math.pi, ALU.is_gt, ALU.mult, [hk], ["hm"])
                    TT("dve", hh[j], hh[j], hm, ALU.subtract, [hk, "hm"], [hk])
                    TS("pool", hm, hh[j], -math.pi, 2.0 * math.pi, ALU.is_lt, ALU.mult, [hk], ["hm"])
                    TT("dve", hh[j], hh[j], hm, ALU.add, [hk, "hm"], [hk])
                    ACT(hh[j], hh[j], AF.Sin, [hk], [hk])
                    src = hh[j]; srck = hk
                for tt_ in range(CW // 128):
                    bk = ("ps", 6 + (tt_ % 2))
                    pso = PSb[:, 6 + (tt_ % 2), 0:256]
                    MM(pso, [(hh[2][:, tt_ * 128:(tt_ + 1) * 128], wo[:, half * 256:(half + 1) * 256])], ["hh2", "hwo"], [bk])
                    TT("dve", KF[:, c0 // 128 + tt_, half * 256:(half + 1) * 256], pso, db[:, tt_, :], ALU.mult, [bk, dk], [("KF", c0 // 128 + tt_, half)])
                ci += 1
        A.free("hw1", "hw2", "hw3", "hwo", "hv", "hz0", "hz1", "hh0", "hh1", "hh2", "hm", "dec0", "dec1")
        if DBG["DBGK"] is not None and tag == "L" and l == 0:
            P.dma(DBG["DBGK"].rearrange("(c p) n -> p c n", p=128), KF, reads=[("KF", c, h) for c in range(nch) for h in range(2)], eng="pool")
        YRE = A.alloc("YRE", [128, nch, 256], BF16); YIM = A.alloc("YIM", [128, nch, 256], BF16)
        hs = A.alloc("hsc", [128, nch + 1])
        P.dma(hs, hysc[tag](), writes=["hsc"])
        ftab = [(A.alloc(f"fct{i}", [128, nch, 128], BF16), A.alloc(f"fst{i}", [128, nch, 128], BF16)) for i in range(2)]
        KA = A.alloc("KA", [128, 2, 512])
        KK = A.alloc("KK", [128, 4, 256])
        TMP = A.alloc("hTMP", [128, 4, 256])
        kfk = [("KF", c, h) for c in range(nch) for h in range(2)]
        ztk = [("ZTM", tok0 // 128 + c) for c in range(nch)]
        for fc in range(nch):
            fct, fst = ftab[fc % 2]; fk = f"fct{fc % 2}"; sk = f"fst{fc % 2}"
            P.dma(fct, Fc[tag]()[fc].rearrange("p (s j) -> p s j", j=128), writes=[fk])
            P.dma(fst, Fs[tag]()[fc].rearrange("p (s j) -> p s j", j=128), writes=[sk])
            MM(PSb[:, 0, :], [(fct[:, s_, :], KF[:, s_, :]) for s_ in range(nch)], [fk] + kfk, [("ps", 0)])
            MM(PSb[:, 1, 0:256], [(fct[:, s_, :], ZTM[:, tok0 // 128 + s_, :]) for s_ in range(nch)], [fk] + ztk, [("ps", 1)])
            MM(PSb[:, 2, :], [(fst[:, s_, :], KF[:, s_, :]) for s_ in range(nch)], [sk] + kfk, [("ps", 2)])
            MM(PSb[:, 3, 0:256], [(fst[:, s_, :], ZTM[:, tok0 // 128 + s_, :]) for s_ in range(nch)], [sk] + ztk, [("ps", 3)])
            CP("act", KA[:, 0, :], PSb[:, 0, :], [("ps", 0)], [("KA", 0)])
            CP("act", KA[:, 1, :], PSb[:, 2, :], [("ps", 2)], [("KA", 1)])
            sg = hs[:, nch:nch + 1]; scl = hs[:, fc:fc + 1]
            for ri in range(2):
                dst = KK[:, 0 if ri == 0 else 1, :]
                STT("dve", dst, KA[:, ri, 256:512], sg, KA[:, ri, 0:256], ALU.mult, ALU.add, [("KA", ri), "hsc"], [("KK", ri)])
                TS("pool", dst, dst, scl, None, ALU.mult, None, [("KK", ri), "hsc"], [("KK", ri)])
            CP("pool", KK[:, 2, :], KK[:, 1, :], [("KK", 1)], [("KK", 2)])
            CP("pool", KK[:, 3, :], KK[:, 0, :], [("KK", 0)], [("KK", 3)])
            if fc == 0:
                CP("pool", KK[0:1, 3, :], KK[0:1, 1, :], [("KK", 1), ("KK", 3)], [("KK", 3)])
                MS("pool", KK[0:1, 2, :], 0.0, [("KK", 2)])
                P.op("pool", lambda e: e.memset(KK[0:1, 1, :], 0.0), [("KK", 2), ("KK", 3)], [("KK", 1)])
            zre = PSb[:, 1, 0:256]; zim = PSb[:, 3, 0:256]
            TT("dve", TMP[:, 0, :], zre, KK[:, 0, :], ALU.mult, [("ps", 1), ("KK", 0)], [("hTMP", 0)])
            TT("dve", TMP[:, 1, :], zim, KK[:, 1, :], ALU.mult, [("ps", 3), ("KK", 1)], [("hTMP", 1)])
            TT("dve", TMP[:, 2, :], zre, KK[:, 2, :], ALU.mult, [("ps", 1), ("KK", 2)], [("hTMP", 2)])
            TT("dve", TMP[:, 3, :], zim, KK[:, 3, :], ALU.mult, [("ps", 3), ("KK", 3)], [("hTMP", 3)])
            TT("pool", YRE[:, fc, :], TMP[:, 0, :], TMP[:, 1, :], ALU.subtract, [("hTMP", 0), ("hTMP", 1)], [("YRE", fc)])
            TT("pool", YIM[:, fc, :], TMP[:, 2, :], TMP[:, 3, :], ALU.add, [("hTMP", 2), ("hTMP", 3)], [("YIM", fc)])
        A.free("KF", "hsc", "fct0", "fst0", "fct1", "fst1", "KA", "KK", "hTMP")
        gtab = [(A.alloc(f"gct{i}", [128, nch, TC], BF16), A.alloc(f"gst{i}", [128, nch, TC], BF16)) for i in range(2)]
        YH = A.alloc("YH", [128, 2, TC]); SQ = A.alloc("hSQ", [128, 2, TC]); RS = A.alloc("hRS", [128, TC])
        YO = A.alloc("hYO", [128, 2, TC], BF16)
        sk2 = A.alloc("hskip", [128, 4])
        P.dma(sk2[:, 0:2], hyskip()[:, l, :], writes=["hskip"])
        P.dma(sk2[:, 2:4], bnT()[:, l, :], writes=["hskip"])
        yk = [("YRE", c) for c in range(nch)] + [("YIM", c) for c in range(nch)]
        for tc in range(ntc):
            gct, gst = gtab[tc % 2]; gk = f"gct{tc % 2}"; gsk = f"gst{tc % 2}"
            P.dma(gct, Gc[tag]()[tc].rearrange("p (s j) -> p s j", j=TC), writes=[gk])
            P.dma(gst, Gs[tag]()[tc].rearrange("p (s j) -> p s j", j=TC), writes=[gsk])
            tsl = slice(tok0 + tc * TC, tok0 + (tc + 1) * TC)
            for cc in range(2):
                pso = PSb[:, 4 + cc, 0:TC]
                MM(pso, [(YRE[:, s_, cc * 128:(cc + 1) * 128], gct[:, s_, :]) for s_ in range(nch)] +
                        [(YIM[:, s_, cc * 128:(cc + 1) * 128], gst[:, s_, :]) for s_ in range(nch)], yk + [gk, gsk], [("ps", 4 + cc)])
                STT("dve", YH[:, cc, :], ZT[:, cc, tsl], sk2[:, cc:cc + 1], pso, ALU.mult, ALU.add, [("ZT", cc), "hskip", ("ps", 4 + cc)], [("YH", cc)])
                TT("pool", YH[:, cc, :], YH[:, cc, :], X0T[:, cc, tsl], ALU.mult, [("YH", cc), ("X0T", cc)], [("YH", cc)])
                ACT(SQ[:, cc, :], YH[:, cc, :], AF.Square, [("YH", cc)], [("hSQ", cc)])
            MM(PSb[:, 6, 0:TC], [(onesf, SQ[:, 0, :]), (onesf, SQ[:, 1, :])], ["onesf", ("hSQ", 0), ("hSQ", 1)], [("ps", 6)])
            ACT(RS, PSb[:, 6, 0:TC], AF.Sqrt, [("ps", 6)], ["hRS"], scale=1.0 / 256, bias=EPS)
            RECIP(RS, ["hRS"])
            for cc in range(2):
                STT("dve", YO[:, cc, :], YH[:, cc, :], sk2[:, 2 + cc:3 + cc], RS, ALU.mult, ALU.mult, [("YH", cc), "hskip", "hRS"], [("hYO", cc)])
            P.dma(YNT[0:2, :, tsl].rearrange("c p n -> p c n"), YO, reads=[("hYO", 0), ("hYO", 1)], writes=[("YNT", 0, tag, tc)], eng="pool")
        A.free("YRE", "YIM", "gct0", "gst0", "gct1", "gst1", "YH", "hSQ", "hRS", "hYO", "hskip")

    def attention(l, br, last):
        qi0 = 0 if br == "ga" else 5
        vi0 = 0 if br == "ga" else 2
        QT = A.alloc("QT", [128, 3, T], BF16); KT = A.alloc("KT", [128, 2, T], BF16); VA = A.alloc("VA", [128, NT, 4, 65], BF16)
        qkr = [("QKD", g_) for g_ in range(9)]
        P.dma(QT, QKD[qi0:qi0 + 3].rearrange("i p n -> p i n"), reads=qkr, writes=["QT"])
        P.dma(KT, QKD[qi0 + 3:qi0 + 5].rearrange("i p n -> p i n"), reads=qkr, writes=["KT"])
        P.dma(VA.rearrange("p t a b -> p t (a b)"), VD.rearrange("t p c -> p t c"), reads=[("VD", t) for t in range(NT)], writes=["VA"])
        BN = A.alloc("BNb", [128, 384])
        boff = 256 if br == "ga" else 640
        P.dma(BN, bnorm()[l][boff:boff + 384].partition_broadcast(128), writes=["BNb"])
        SKE = A.alloc("SKE", [128, 6])
        if br == "wa":
            P.dma(SKE, sink()[l].partition_broadcast(128), writes=["SKE"])
            ACT(SKE, SKE, AF.Exp, ["SKE"], ["SKE"])
            WM = A.alloc("WM", [128, 2, 128], BF16)
            P.dma(WM, wmask_in(), writes=["WM"])
        Yb = [A.alloc(f"Yb{i}", [128, 4, 384]) for i in range(2)]
        Pb = [A.alloc(f"Pb{i}", [128, 512], BF16) for i in range(2)]
        YNb = A.alloc("YNb", [128, 384], BF16)
        YS = [A.alloc(f"YS{i}", [128, 3, 512], BF16) for i in range(2)]
        st = A.alloc("ast", [128, 8])
        junk = A.alloc("ajunk", [128, 384])
        den = A.alloc("aden", [128, 8])
        chunks = [(c * 4, 4) for c in range(8)] + ([] if last else [(32, 2)])
        pcnt = 0
        for ci, (t0, ntl) in enumerate(chunks):
            nq = ntl * 128
            Y = Yb[ci % 2]; ykey = f"Yb{ci % 2}"
            isctx = t0 >= NLT
            for h in range(6):
                g = h // 3; pair = h // 2; b = 64 * (h % 2)
                order = 0 if b == 64 * g else 1
                if br == "ga":
                    units = [(list(range(ntl)), ([32, 33] if isctx else list(range(NT))))]
                else:
                    units = []
                    for qs in range(ntl):
                        i = t0 + qs
                        if isctx:
                            units.append(([qs], [32, 33]))
                        else:
                            units.append(([qs], [j for j in (i - 1, i, i + 1) if 0 <= j < NLT] + [32, 33]))
                for (qsl, keys) in units:
                    q0 = (t0 + qsl[0]) * 128; nqq = len(qsl) * 128
                    pend = None

                    def pv_emit(pd):
                        ji_, j_, pb__, pk__ = pd
                        for qq, qs in enumerate(qsl):
                            MM1(PSb[:, 2 + qs, 0:65], pb__[:, qq * 128:(qq + 1) * 128], VA[:, j_, vi0 + g, :], ji_ == 0, ji_ == len(keys) - 1, [pk__, "VA"], [("ps", 2 + qs)])
                    for ji, j in enumerate(keys):
                        sb_ = pcnt % 2; pcnt += 1
                        stp = PSb[:, sb_, 0:nqq]
                        MM(stp, [(KT[b:b + 64, order, j * 128:(j + 1) * 128], QT[b:b + 64, pair, q0:q0 + nqq])], ["KT", "QT"], [("ps", sb_)])
                        pb_ = Pb[sb_][:, 0:nqq]; pk = f"Pb{sb_}"
                        ACT(pb_, stp, AF.Exp, [("ps", sb_)], [pk], scale=0.125)
                        if br == "wa" and not isctx and j < NLT and j != t0 + qsl[0]:
                            mi = 0 if j < t0 + qsl[0] else 1
                            TT("pool", pb_, pb_, WM[:, mi, :], ALU.mult, [pk, "WM"], [pk])
                        if pend is not None:
                            pv_emit(pend)
                        pend = (ji, j, pb_, pk)
                    pv_emit(pend)
                    for qs in qsl:
                        dk = ("aden", qs)
                        if br == "wa":
                            TT("dve", den[:, qs:qs + 1], PSb[:, 2 + qs, 64:65], SKE[:, h:h + 1], ALU.add, [("ps", 2 + qs), "SKE"], [dk])
                        else:
                            CP("dve", den[:, qs:qs + 1], PSb[:, 2 + qs, 64:65], [("ps", 2 + qs)], [dk])
                        RECIP(den[:, qs:qs + 1], [dk])
                        TS("dve", Y[:, qs, h * 64:(h + 1) * 64], PSb[:, 2 + qs, 0:64], den[:, qs:qs + 1], None, ALU.mult, None, [("ps", 2 + qs), dk], [(ykey, qs)])
            ys = YS[ci % 2]; ysk = f"YS{ci % 2}"
            for qs in range(ntl):
                sk_ = ("ast", qs % 2)
                sq = st[:, (qs % 2) * 2:(qs % 2) * 2 + 1]; rs = st[:, (qs % 2) * 2 + 1:(qs % 2) * 2 + 2]
                ACT(junk, Y[:, qs, :], AF.Square, [(ykey, qs)], ["ajunk", sk_], accum_out=sq)
                rstd_from_ssq(sq, rs, 1.0 / 384, [sk_], [sk_])
                STT("dve", YNb, Y[:, qs, :], rs, BN, ALU.mult, ALU.mult, [(ykey, qs), sk_, "BNb"], ["YNb"])
                pb = psbf(6 + qs % 2)
                TR([(pb[:, k * 128:(k + 1) * 128], YNb[:, k * 128:(k + 1) * 128]) for k in range(3)], identb, ["YNb", "identb"], [("ps", 6 + qs % 2)])
                CP("act", ys[:, :, qs * 128:(qs + 1) * 128], pb[:, 0:384].rearrange("p (k c) -> p k c", k=3), [("ps", 6 + qs % 2)], [(ysk, qs)])
            c0 = 2 if br == "ga" else 5
            P.dma(YNT[c0:c0 + 3, :, t0 * 128:t0 * 128 + nq].rearrange("c p n -> p c n"), ys[:, :, 0:nq], reads=[(ysk, qs) for qs in range(ntl)], writes=[("YNT", br, ci)], eng="pool")
        A.free("QT", "KT", "VA", "BNb", "SKE", "Yb0", "Yb1", "Pb0", "Pb1", "YNb", "YS0", "YS1", "ast", "ajunk", "aden")
        if br == "wa":
            A.free("WM")

    for l in range(nlayers):
        last = (l == 1)
        ntl_l = NLT if last else NT
        silB = A.alloc("silB", [128, 2, 8, 128])
        for v in range(2):
            for k in range(8):
                CP("dve", silB[:, v, k, :], sil[:, v, k:k + 1].to_broadcast([128, 128]), ["sil"], [("silB", v, k)])
        wst = [A.alloc(f"wada{i}", [128, 8, 512]) for i in range(2)]
        bb = A.alloc("badab", [128, 512]); nb = A.alloc("normb", [128, 2, D])
        P.dma(nb[:, 0, :], norm_mix()[l].partition_broadcast(128), writes=[("normb", 0)])
        P.dma(nb[:, 1, :], norm_ffn()[l].partition_broadcast(128), writes=[("normb", 1)])
        mo = [A.alloc(f"modo{i}", [128, 512]) for i in range(2)]
        for n_ in range(12):
            ws = wst[n_ % 2]; wk = f"wada{n_ % 2}"
            P.dma(ws, w_ada()[l][:, n_ * 512:(n_ + 1) * 512].rearrange("(k p) n -> p k n", p=128), writes=[wk])
            P.dma(bb, b_ada()[l][n_ * 512:(n_ + 1) * 512].partition_broadcast(128), writes=["badab"])
            j = n_ // 2; hf = n_ % 2
            for v in range(2):
                MM(PSb[:, v, :], [(silB[:, v, k, :], ws[:, k, :]) for k in range(8)], [wk] + [("silB", v, k) for k in range(8)], [("ps", v)])
                m_ = mo[v]; mk = f"modo{v}"
                TT("dve", m_, PSb[:, v, :], bb, ALU.add, [("ps", v), "badab"], [mk])
                if j in (1, 4):
                    nsel = 0 if j == 1 else 1
                    STT("dve", m_, m_, 1.0, nb[:, nsel, hf * 512:(hf + 1) * 512], ALU.add, ALU.mult, [mk, ("normb", nsel)], [mk])
                P.dma(MODD[v, j, :, hf * 512:(hf + 1) * 512], m_, reads=[mk], writes=[("MODD", v, j, hf)], eng="pool")
        A.free("wada0", "wada1", "badab", "normb", "modo0", "modo1", "silB")
        if done(f"mod{l}"): break

        HT = A.alloc("HT", [128, 8, T], BF16)
        a1 = A.alloc("a1", [128, 2, D]); s1 = A.alloc("s1", [128, 2, D])
        for v in range(2):
            modd_load(a1[:, v, :], v, 1, ("a1", v)); modd_load(s1[:, v, :], v, 0, ("s1", v))
        xts = [A.alloc(f"xt{i}", [128, D]) for i in range(2)]
        junk = A.alloc("junk", [128, D])
        hb = [A.alloc(f"hb{i}", [128, D], BF16) for i in range(2)]
        st = A.alloc("stat", [128, 4])
        for t in range(NT):
            v = 0 if t < NLT else 1
            xt = xts[t % 2]; xk = f"xt{t % 2}"
            src, rk = xsrc(l, t)
            P.dma(xt, src, reads=rk, writes=[xk])
            sq = st[:, (t % 2) * 2:(t % 2) * 2 + 1]; rs = st[:, (t % 2) * 2 + 1:(t % 2) * 2 + 2]; sk = ("stat", t % 2)
            ACT(junk, xt, AF.Square, [xk], ["junk", sk], accum_out=sq)
            rstd_from_ssq(sq, rs, 1.0 / D, [sk], [sk])
            STT("dve", xt, xt, rs, a1[:, v, :], ALU.mult, ALU.mult, [xk, sk, ("a1", v)], [xk])
            h_ = hb[t % 2]; hk = f"hb{t % 2}"
            TT("pool", h_, xt, s1[:, v, :], ALU.add, [xk, ("s1", v)], [hk])
            if DBG["DBGH"] is not None and l == 0:
                P.dma(DBG["DBGH"][t * 128:(t + 1) * 128, :], h_, reads=[hk], eng="pool")
            pb = psbf(t % 2).rearrange("p (k c) -> p k c", k=8)
            TR([(pb[:, k, :], h_[:, k * 128:(k + 1) * 128]) for k in range(8)], identb, [hk, "identb"], [("ps", t % 2)])
            CP("act", HT[:, :, t * 128:(t + 1) * 128], pb, [("ps", t % 2)], [("HT", t)])
        A.free("a1", "s1", "xt0", "xt1", "junk", "hb0", "hb1", "stat")
        if done(f"A{l}"): break

        WIN = A.alloc("WIN", [128, 8, 2048], BF16)
        for k in range(8):
            P.dma(WIN[:, k, :], w_in()[l][k * 128:(k + 1) * 128, :], writes=[("WIN", k)], eng="pool")
        htk = [("HT", t) for t in range(NT)]
        wink = [("WIN", k) for k in range(8)]

        X0T = A.alloc("X0T", [128, 2, T], BF16); ZT = A.alloc("ZT", [128, 2, T], BF16)
        bT = A.alloc("bT", [128, 6]); cw = A.alloc("cw", [128, 3, 6]); cb = A.alloc("cb", [128, 6])
        P.dma(bT, b_inT()[:, l, :], writes=["bT"]); P.dma(cw, convw()[:, l], writes=["cw"]); P.dma(cb, convb()[:, l], writes=["cb"])
        U = A.alloc("U", [128, T + 4]); UC1 = A.alloc("UC1", [128, T], BF16); UC = A.alloc("UC", [128, T])
        MS("pool", U, 0.0, ["U"])
        segs = [(0, NLAT, 1), (NLAT, NCTX, NLAT + 3)]
        for m in (0, 1, 2, 4, 3, 5):
            ck = 0
            for (tk0, n_, uo) in segs:
                for c0 in range(0, n_, 512):
                    w_ = min(512, n_ - c0); bkk = ("ps", ck % 2)
                    MM(PSb[:, ck % 2, 0:w_], [(WIN[:, k, m * 128:(m + 1) * 128], HT[:, k, tk0 + c0:tk0 + c0 + w_]) for k in range(8)], wink + htk, [bkk])
                    ACT(U[:, uo + c0:uo + c0 + w_], PSb[:, ck % 2, 0:w_], AF.Identity, [bkk, "bT"], ["U"], bias=bT[:, m:m + 1])
                    ck += 1
            dst = UC1 if m in (2, 3) else UC; dk = "UC1" if m in (2, 3) else "UC"
            for (tk0, n_, uo) in segs:
                d_ = dst[:, tk0:tk0 + n_]
                TS("dve", d_, U[:, uo:uo + n_], cw[:, 1, m:m + 1], cb[:, m:m + 1], ALU.mult, ALU.add, ["U", "cw", "cb"], [dk])
                STT("dve", d_, U[:, uo - 1:uo - 1 + n_], cw[:, 0, m:m + 1], d_, ALU.mult, ALU.add, ["U", "cw", dk], [dk])
                STT("dve", d_, U[:, uo + 1:uo + 1 + n_], cw[:, 2, m:m + 1], d_, ALU.mult, ALU.add, ["U", "cw", dk], [dk])
            if m < 2:
                CP("pool", X0T[:, m, :], UC, ["UC"], [("X0T", m)])
            elif m >= 4:
                TT("pool", ZT[:, m - 4, :], UC1, UC, ALU.mult, ["UC", "UC1"], [("ZT", m - 4)])
        A.free("bT", "cw", "cb", "U", "UC1", "UC")
        ZTM = A.alloc("ZTM", [128, NT, 256], BF16)
        for t0 in range(0, NT, 4):
            nt_ = min(4, NT - t0); bnk = (t0 // 4) % 2
            pb = psbf(bnk).rearrange("p (t c) -> p t c", t=4)
            TR([(pb[:, tt_, cc * 128:(cc + 1) * 128], ZT[:, cc, (t0 + tt_) * 128:(t0 + tt_ + 1) * 128]) for tt_ in range(nt_) for cc in range(2)], identb,
               [("ZT", 0), ("ZT", 1), "identb"], [("ps", bnk)])
            for tt_ in range(nt_):
                CP("act", ZTM[:, t0 + tt_, :], pb[:, tt_, :], [("ps", bnk)], [("ZTM", t0 + tt_)])
        for i_, (nm_, tl_) in enumerate((("X0T", X0T), ("ZT", ZT), ("ZTM", ZTM))):
            P.dma(HYD[i_], tl_.rearrange("p a b -> p (a b)"), reads=[k_ for k_ in P.bykey.get(nm_, ())], writes=[("HYD", i_)], eng="pool")
        A.free("X0T", "ZT", "ZTM")
        if done(f"B1{l}"): break

        PTs = [A.alloc(f"PT{i}", [128, 1280]) for i in range(2)]
        RQs = [A.alloc(f"RQ{i}", [128, 1536], BF16) for i in range(2)]
        SQ = A.alloc("SQ", [128, 512]); st8 = A.alloc("st8", [128, 2, 8])
        QKG = A.alloc("QKG", [128, 512]); BINB = A.alloc("BINB", [128, 1280])
        P.dma(QKG, qkg()[l].partition_broadcast(128), writes=["QKG"])
        P.dma(BINB, b_in()[l][768:2048].partition_broadcast(128), writes=["BINB"])
        RC = A.alloc("RC", [128, NT, 32]); RSn = A.alloc("RSn", [128, NT, 32])
        P.dma(RC, ropeC(), writes=["RC"]); P.dma(RSn, ropeS(), writes=["RSn"])
        VAUG = [A.alloc(f"VAUG{i}", [128, 4, 65], BF16) for i in range(2)]
        for i in range(2):
            MS("pool", VAUG[i], 1.0, [f"VAUG{i}"])
        QKT = [A.alloc(f"QKT{i}", [128, 10, 512], BF16) for i in range(2)]
        TMPS = [A.alloc(f"rt{i}", [128, 8, 2, 16]) for i in range(4)]
        tcols = [(0, 128), (128, 256), (256, 384), (384, 512), (1280, 1408), (640, 768), (768, 896), (896, 1024), (1024, 1152), (1408, 1536)]
        for t in range(NT):
            PT = PTs[t % 2]; pk = f"PT{t % 2}"; RQ = RQs[t % 2]; rqk = f"RQ{t % 2}"
            b0 = 3 * (t % 2)
            for pi, (c0, c1) in enumerate(((768, 1280), (1280, 1792), (1792, 2048))):
                MM(PSb[:, b0 + pi, 0:c1 - c0], [(HT[:, k, t * 128:(t + 1) * 128], WIN[:, k, c0:c1]) for k in range(8)], wink + [("HT", t)], [("ps", b0 + pi)])
                TT("dve", PT[:, c0 - 768:c1 - 768], PSb[:, b0 + pi, 0:c1 - c0], BINB[:, c0 - 768:c1 - 768], ALU.add, [("ps", b0 + pi), "BINB"], [(pk, pi)])
            pkall = [(pk, 0), (pk, 1), (pk, 2)]
            sk = ("st8", t % 2)
            ACT(SQ, PT[:, 0:512], AF.Square, [(pk, 0)], ["SQ"])
            P.op("dve", lambda e, o_=st8[:, t % 2, :], i_=SQ.rearrange("p (h d) -> p h d", d=64): e.tensor_reduce(out=o_, in_=i_, axis=AX.X, op=ALU.add), ["SQ"], [sk])
            ACT(st8[:, t % 2, :], st8[:, t % 2, :], AF.Sqrt, [sk], [sk], scale=1.0 / 64, bias=EPS)
            RECIP(st8[:, t % 2, :], [sk])
            pv = PT[:, 0:512].rearrange("p (h d) -> p h d", d=64)
            TT("dve", pv, pv, st8[:, t % 2, :].unsqueeze(2).to_broadcast([128, 8, 64]), ALU.mult, [(pk, 0), sk], [(pk, 0)])
            TT("dve", PT[:, 0:512], PT[:, 0:512], QKG, ALU.mult, [(pk, 0), "QKG"], [(pk, 0)])
            Cb = RC[:, t, :].rearrange("p (a f) -> p a f", a=2).unsqueeze(1).to_broadcast([128, 8, 2, 16])
            Sb = RSn[:, t, :].rearrange("p (a f) -> p a f", a=2).unsqueeze(1).to_broadcast([128, 8, 2, 16])
            for ri, base in enumerate((0, 640)):
                xv = PT[:, base:base + 512].rearrange("p (h a s f) -> p h a s f", h=8, a=2, s=2)
                ov = RQ[:, base:base + 512].rearrange("p (h a s f) -> p h a s f", h=8, a=2, s=2)
                x1 = xv[:, :, :, 0, :]; x2 = xv[:, :, :, 1, :]
                rk = pkall
                tk = [(f"rt{i}", ri) for i in range(4)]
                TT("dve", TMPS[0], x1, Cb, ALU.mult, rk + ["RC"], [tk[0]])
                TT("dve", TMPS[1], x2, Sb, ALU.mult, rk + ["RSn"], [tk[1]])
                TT("dve", ov[:, :, :, 0, :], TMPS[0], TMPS[1], ALU.subtract, [tk[0], tk[1]], [(rqk, ri, 0)])
                TT("pool", TMPS[2], x2, Cb, ALU.mult, rk + ["RC"], [tk[2]])
                TT("dve", TMPS[3], x1, Sb, ALU.mult, rk + ["RSn"], [tk[3]])
                TT("pool", ov[:, :, :, 1, :], TMPS[2], TMPS[3], ALU.add, [tk[2], tk[3]], [(rqk, ri, 1)])
            rqall = [(rqk, 0, 0), (rqk, 0, 1), (rqk, 1, 0), (rqk, 1, 1)]
            for (dst0, s0) in ((1280, 448), (1344, 384), (1408, 1088), (1472, 1024)):
                CP("pool", RQ[:, dst0:dst0 + 64], RQ[:, s0:s0 + 64], rqall, [(rqk, "sw", dst0)])
            rqall = rqall + [(rqk, "sw", d_) for d_ in (1280, 1344, 1408, 1472)]
            va = VAUG[t % 2]; vk = f"VAUG{t % 2}"
            CP("act", va[:, 0:2, 0:64], PT[:, 512:640].rearrange("p (h d) -> p h d", h=2), [(pk, 1)], [vk])
            CP("act", va[:, 2:4, 0:64], PT[:, 1152:1280].rearrange("p (h d) -> p h d", h=2), [(pk, 2)], [vk])
            P.dma(VD[t], va.rearrange("p a b -> p (a b)"), reads=[vk], writes=[("VD", t)], eng="pool")
            pb = psbf(6, 2)
            TR([(pb[:, i * 128:(i + 1) * 128], RQ[:, c0:c1]) for i, (c0, c1) in enumerate(tcols)], identb, rqall + ["identb"], [("ps", 6)])
            g4 = t // 4; qt = QKT[g4 % 2]; qtk = f"QKT{g4 % 2}"
            CP("act", qt[:, :, (t % 4) * 128:(t % 4 + 1) * 128], pb[:, 0:1280].rearrange("p (i c) -> p i c", i=10), [("ps", 6)], [(qtk, t % 4)])
            if t % 4 == 3 or t == NT - 1:
                t0 = (t // 4) * 4; nn = (t - t0 + 1) * 128
                P.dma(QKD[:, :, t0 * 128:t0 * 128 + nn].rearrange("i p n -> p i n"), qt[:, :, 0:nn], reads=[(qtk, i) for i in range(t - t0 + 1)], writes=[("QKD", t // 4)], eng="pool")
        A.free("PT0", "PT1", "RQ0", "RQ1", "SQ", "st8", "QKG", "BINB", "RC", "RSn", "VAUG0", "VAUG1", "QKT0", "QKT1", "rt0", "rt1", "rt2", "rt3")
        A.free("HT", "WIN")
        if done(f"B2{l}"): break

        X0T = A.alloc("X0T", [128, 2, T], BF16); ZT = A.alloc("ZT", [128, 2, T], BF16); ZTM = A.alloc("ZTM", [128, NT, 256], BF16)
        P.dma(X0T.rearrange("p a b -> p (a b)"), HYD[0], reads=[("HYD", 0)], writes=[("X0T", 0), ("X0T", 1)])
        P.dma(ZT.rearrange("p a b -> p (a b)"), HYD[1], reads=[("HYD", 1)], writes=[("ZT", 0), ("ZT", 1)])
        P.dma(ZTM.rearrange("p a b -> p (a b)"), HYD[2], reads=[("HYD", 2)], writes=[("ZTM", t_) for t_ in range(NT)])
        hyena(l, "L", 0, X0T, ZT, ZTM)
        if not last:
            hyena(l, "C", NLAT, X0T, ZT, ZTM)
        A.free("X0T", "ZT", "ZTM")
        if done(f"C{l}"): break

        attention(l, "ga", last)
        if done(f"Dga{l}"): break
        attention(l, "wa", last)
        if done(f"D{l}"): break

        YN = A.alloc("YN", [128, 8, T], BF16)
        ynr = [("YNT", 0, "L", tc) for tc in range(NLAT // 256)] + ([] if last else [("YNT", 0, "C", 0)]) + \
              [("YNT", br, ci) for br in ("ga", "wa") for ci in range(8 if last else 9)]
        P.dma(YN, YNT.rearrange("c p n -> p c n"), reads=ynr, writes=["YN"])
        WO = A.alloc("WO", [128, 8, D], BF16)
        for k in range(8):
            P.dma(WO[:, k, :], w_out()[l][k * 128:(k + 1) * 128, :], writes=[("WO", k)], eng="pool")
        wok = [("WO", k) for k in range(8)]
        BO = A.alloc("BO", [128, D]); G1 = A.alloc("G1", [128, 2, D])
        P.dma(BO, b_out()[l].partition_broadcast(128), writes=["BO"])
        for v in range(2):
            modd_load(G1[:, v, :], v, 2, ("G1", v))
        xts = [A.alloc(f"xt{i}", [128, D]) for i in range(2)]
        tm = [A.alloc(f"tm{i}", [128, D]) for i in range(2)]
        for t in range(ntl_l):
            v = 0 if t < NLT else 1
            xt = xts[t % 2]; xk = f"xt{t % 2}"; tmp = tm[t % 2]; tk = f"tm{t % 2}"
            src, rk = xsrc(l, t)
            P.dma(xt, src, reads=rk, writes=[xk])
            for hf in range(2):
                bk = ("ps", (t % 2) * 2 + hf)
                MM(PSb[:, (t % 2) * 2 + hf, :], [(YN[:, k, t * 128:(t + 1) * 128], WO[:, k, hf * 512:(hf + 1) * 512]) for k in range(8)], ["YN"] + wok, [bk])
                TT("dve", tmp[:, hf * 512:(hf + 1) * 512], PSb[:, (t % 2) * 2 + hf, :], BO[:, hf * 512:(hf + 1) * 512], ALU.add, [bk, "BO"], [(tk, hf)])
            TT("pool", tmp, tmp, G1[:, v, :], ALU.mult, [(tk, 0), (tk, 1), ("G1", v)], [(tk, 0), (tk, 1)])
            TT("pool", xt, xt, tmp, ALU.add, [xk, (tk, 0), (tk, 1)], [xk])
            P.dma(X1[t * 128:(t + 1) * 128, :], xt, reads=[xk], writes=[("X1", t)], eng="pool")
        A.free("YN", "WO", "BO", "G1", "xt0", "xt1", "tm0", "tm1")
        if done(f"E{l}"): break

        GT = A.alloc("GT", [32, T])
        a2 = A.alloc("a2", [128, 2, D]); s2 = A.alloc("s2", [128, 2, D])
        for v in range(2):
            modd_load(a2[:, v, :], v, 4, ("a2", v)); modd_load(s2[:, v, :], v, 3, ("s2", v))
        WR = A.alloc("WR", [128, 8, NE]); BR = A.alloc("BR", [128, NE])
        P.dma(WR, w_router()[l].rearrange("(k p) e -> p k e", p=128), writes=["WR"])
        P.dma(BR, b_router()[l].partition_broadcast(128), writes=["BR"])
        H2D = dscr(f"H2D{l}", [8, 128, T], BF16)
        xts = [A.alloc(f"xt{i}", [128, D]) for i in range(2)]
        junk = A.alloc("junk", [128, D]); h2f = A.alloc("h2f", [128, 8, 128])
        h2b = [A.alloc(f"h2b{i}", [128, 8, 128], BF16) for i in range(2)]
        st = A.alloc("stat", [128, 4]); lg = A.alloc("lg", [128, NE]); m8 = A.alloc("m8", [128, 8]); msk = A.alloc("msk", [128, NE])
        gs = A.alloc("gs", [128, 4])
        for t in range(ntl_l):
            v = 0 if t < NLT else 1
            xt = xts[t % 2]; xk = f"xt{t % 2}"
            P.dma(xt, X1[t * 128:(t + 1) * 128, :], reads=[("X1", t)], writes=[xk])
            sq = st[:, (t % 2) * 2:(t % 2) * 2 + 1]; rs = st[:, (t % 2) * 2 + 1:(t % 2) * 2 + 2]; sk = ("stat", t % 2)
            ACT(junk, xt, AF.Square, [xk], ["junk", sk], accum_out=sq)
            rstd_from_ssq(sq, rs, 1.0 / D, [sk], [sk])
            STT("dve", xt, xt, rs, a2[:, v, :], ALU.mult, ALU.mult, [xk, sk, ("a2", v)], [xk])
            TT("pool", xt, xt, s2[:, v, :], ALU.add, [xk, ("s2", v)], [xk])
            pf = PSb[:, 0:2, :].rearrange("p b n -> p (b n)").rearrange("p (k c) -> p k c", k=8)
            TR([(pf[:, k, :], xt[:, k * 128:(k + 1) * 128]) for k in range(8)], identf, [xk, "identf"], [("ps", 0), ("ps", 1)])
            CP("act", h2f, pf, [("ps", 0), ("ps", 1)], ["h2f"])
            hb_ = h2b[t % 2]; hbk = f"h2b{t % 2}"
            CP("pool", hb_, h2f, ["h2f"], [hbk])
            P.dma(H2D[:, :, t * 128:(t + 1) * 128].rearrange("k p n -> p k n"), hb_, reads=[hbk], writes=[("H2D", t)], eng="pool")
            MM(PSb[:, 2, 0:NE], [(h2f[:, k, :], WR[:, k, :]) for k in range(8)], ["h2f", "WR"], [("ps", 2)])
            TT("dve", lg, PSb[:, 2, 0:NE], BR, ALU.add, [("ps", 2), "BR"], ["lg"])
            P.op("dve", lambda e: e.max(out=m8, in_=lg), ["lg"], ["m8"])
            TS("dve", msk, lg, m8[:, 3:4], None, ALU.is_ge, None, ["lg", "m8"], ["msk"])
            TS("dve", gs[:, 0:1], m8[:, 0:1], -1.0, None, ALU.mult, None, ["m8"], ["gs"])
            ACT(lg, lg, AF.Exp, ["lg", "gs"], ["lg"], bias=gs[:, 0:1])
            TT("dve", lg, lg, msk, ALU.mult, ["lg", "msk"], ["lg"])
            P.op("dve", lambda e: e.tensor_reduce(out=gs[:, 1:2], in_=lg, axis=AX.X, op=ALU.add), ["lg"], ["gs"])
            RECIP(gs[:, 1:2], ["gs"])
            TS("dve", lg, lg, gs[:, 1:2], None, ALU.mult, None, ["lg", "gs"], ["lg"])
            TR([(PSb[0:32, 3, 0:128], lg)], identf, ["lg", "identf"], [("ps", 3)])
            CP("act", GT[:, t * 128:(t + 1) * 128], PSb[0:32, 3, 0:128], [("ps", 3)], [("GT", t)])
        gtk = [("GT", t) for t in range(ntl_l)]
        ntok = ntl_l * 128
        P.dma(GATD[:, 0:ntok], GT[:, 0:ntok], reads=gtk, writes=["GATD"], eng="pool")
        A.free("a2", "s2", "WR", "BR", "xt0", "xt1", "junk", "h2f", "h2b0", "h2b1", "stat", "lg", "m8", "msk", "gs", "GT")
        if done(f"F1{l}"): break

        B2f = A.alloc("B2f", [32, D])
        P.dma(B2f, b_mlp2()[l], writes=["B2f"])
        B1 = A.alloc("B1", [128, NE, 16])
        P.dma(B1, b1T()[:, l], writes=["B1"])
        TS("dve", B1[:, :, 8:16], B1[:, :, 8:16], 1.0, None, ALU.add, None, ["B1"], ["B1"])
        G2 = A.alloc("G2", [128, 2, D])
        for v in range(2):
            modd_load(G2[:, v, :], v, 5, ("G2", v))
        NFb = A.alloc("NFb", [128, D])
        if last:
            P.dma(NFb, norm_final().partition_broadcast(128), writes=["NFb"])
        SCT = 8
        W1s = [A.alloc(f"W1{i}", [128, 8, 2048], BF16) for i in range(2)]; W2 = A.alloc("W2", [128, 8, D], BF16)
        ACC = A.alloc("ACC", [128, 8, SCT * 128]); H2T = A.alloc("H2T", [128, 8, SCT * 128], BF16)
        GB = A.alloc("GB", [128, SCT * 128]); GTs = A.alloc("GTs", [32, SCT * 128])
        mW = A.alloc("mW", [128, 8, 512])
        Gt = [mW[:, 4 * i + 0, :] for i in range(2)]; St = [mW[:, 4 * i + 1, :] for i in range(2)]
        Lt = [mW[:, 4 * i + 2, :] for i in range(2)]; Xt = [mW[:, 4 * i + 3, :] for i in range(2)]
        Ub = A.alloc("mU", [128, 8, SCT * 128], BF16)
        w2k = [("W2", k) for k in range(8)]
        wi = 0; ei = 0
        sclist = list(range(0, ntl_l, SCT))

        def load_w1(e_, buf):
            for k in range(0, 8, 2):
                P.dma(W1s[buf][:, k:k + 2, :], w_mlp1()[l, e_, k * 128:(k + 2) * 128, :].rearrange("(a p) n -> p a n", p=128), writes=[(f"W1{buf}", k), (f"W1{buf}", k + 1)], eng="pool")

        def load_w2(e_):
            for k in range(0, 8, 4):
                P.dma(W2[:, k:k + 4, :], w_mlp2()[l, e_, k * 128:(k + 4) * 128, :].rearrange("(a p) n -> p a n", p=128), writes=[("W2", k_) for k_ in range(k, k + 4)], eng="pool")
        load_w1(0, 0)
        load_w2(0)
        for sci, sc0 in enumerate(sclist):
            nts = min(SCT, ntl_l - sc0); tk0 = sc0 * 128; ntk = nts * 128
            P.dma(H2T[:, :, 0:ntk], H2D[:, :, tk0:tk0 + ntk].rearrange("k p n -> p k n"), reads=[("H2D", t) for t in range(sc0, sc0 + nts)], writes=["H2T"])
            P.dma(GTs[:, 0:ntk], GATD[:, tk0:tk0 + ntk], reads=["GATD"], writes=["GTs"])
            cks = [(c0, min(512, ntk - c0)) for c0 in range(0, ntk, 512)]
            for (c0, w_) in cks:
                for dc in range(8):
                    bk = ("ps", 6 + dc % 2)
                    MM(PSb[:, 6 + dc % 2, 0:w_], [(B2f[:, dc * 128:(dc + 1) * 128], GTs[:, c0:c0 + w_])], ["B2f", "GTs"], [bk])
                    CP("act", ACC[:, dc, c0:c0 + w_], PSb[:, 6 + dc % 2, 0:w_], [bk], [("ACC", dc, c0)])
            for e_ in range(NE):
                cur = ei % 2; ei += 1
                W1 = W1s[cur]; w1k = [(f"W1{cur}", k) for k in range(8)]
                nxt_e = e_ + 1 if e_ + 1 < NE else (0 if sci + 1 < len(sclist) else None)
                if nxt_e is not None:
                    load_w1(nxt_e, 1 - cur)
                P.dma(GB[:, 0:ntk], GATD[e_, tk0:tk0 + ntk].partition_broadcast(128), reads=["GATD"], writes=["GB"])
                for (c0, w_) in cks:
                    for jf in range(8):
                        bg = jf % 2; bl = 2 + jf % 2; wb = wi % 2; wi += 1
                        MM(PSb[:, bg, 0:w_], [(W1[:, k, jf * 128:(jf + 1) * 128], H2T[:, k, c0:c0 + w_]) for k in range(8)], w1k + ["H2T"], [("ps", bg)])
                        MM(PSb[:, bl, 0:w_], [(W1[:, k, 1024 + jf * 128:1024 + (jf + 1) * 128], H2T[:, k, c0:c0 + w_]) for k in range(8)], w1k + ["H2T"], [("ps", bl)])
                        G_ = Gt[wb][:, 0:w_]; S_ = St[wb][:, 0:w_]; L_ = Lt[wb][:, 0:w_]; X_ = Xt[wb][:, 0:w_]
                        gk_ = ("mW", "G", wb); sk_ = ("mW", "S", wb); lk_ = ("mW", "L", wb); xk_ = ("mW", "X", wb)
                        TS("dve", G_, PSb[:, bg, 0:w_], B1[:, e_, jf:jf + 1], 7.0, ALU.add, ALU.min, [("ps", bg), "B1"], [gk_])
                        ACT(S_, G_, AF.Sigmoid, [gk_], [sk_], scale=1.702)
                        TS("dve", L_, PSb[:, bl, 0:w_], B1[:, e_, 8 + jf:9 + jf], 8.0, ALU.add, ALU.min, [("ps", bl), "B1"], [lk_])
                        TT("pool", X_, G_, S_, ALU.mult, [gk_, sk_], [xk_])
                        STT("dve", L_, L_, -6.0, X_, ALU.max, ALU.mult, [lk_, xk_], [lk_])
                        TT("dve", Ub[:, jf, c0:c0 + w_], L_, GB[:, c0:c0 + w_], ALU.mult, [lk_, "GB"], [("mU", jf, c0)])
                for (c0, w_) in cks:
                    for dc in range(8):
                        bk = ("ps", 4 + dc % 2)
                        MM(PSb[:, 4 + dc % 2, 0:w_], [(W2[:, jf, dc * 128:(dc + 1) * 128], Ub[:, jf, c0:c0 + w_]) for jf in range(8)], w2k + [("mU", jf, c0) for jf in range(8)], [bk])
                        TT("dve", ACC[:, dc, c0:c0 + w_], ACC[:, dc, c0:c0 + w_], PSb[:, 4 + dc % 2, 0:w_], ALU.add, [bk, ("ACC", dc, c0)], [("ACC", dc, c0)])
                if nxt_e is not None:
                    load_w2(nxt_e)
            acck = [("ACC", dc, c0) for dc in range(8) for (c0, _) in cks]
            for tt_ in range(nts):
                t = sc0 + tt_; v = 0 if t < NLT else 1
                eb = tt_ % 2
                ebk = [("mW", r_, eb) for r_ in "GSLX"]
                ebuf = mW[:, 4 * eb:4 * eb + 4, :].rearrange("p a n -> p (a n)")
                xt = ebuf[:, 0:D]; y_ = ebuf[:, D:2 * D]
                P.dma(xt, X1[t * 128:(t + 1) * 128, :], reads=[("X1", t)], writes=ebk)
                pf = PSb[:, 6:8, :].rearrange("p b n -> p (b n)").rearrange("p (k c) -> p k c", k=8)
                TR([(pf[:, k, :], ACC[:, k, tt_ * 128:(tt_ + 1) * 128]) for k in range(8)], identf, acck + ["identf"], [("ps", 6), ("ps", 7)])
                TT("dve", y_, PSb[:, 6:8, :].rearrange("p b n -> p (b n)"), G2[:, v, :], ALU.mult, [("ps", 6), ("ps", 7), ("G2", v)], ebk)
                TT("pool", xt, xt, y_, ALU.add, ebk, ebk)
                if not last:
                    P.dma(XR[t * 128:(t + 1) * 128, :], xt, reads=ebk, writes=[("XR", t)], eng="pool")
                else:
                    sq = GB[:, 0:1]; rs = GB[:, 1:2]
                    ACT(y_, xt, AF.Square, ebk, ebk + ["GB"], accum_out=sq)
                    rstd_from_ssq(sq, rs, 1.0 / D, ["GB"], ["GB"])
                    STT("dve", xt, xt, rs, NFb, ALU.mult, ALU.mult, ebk + ["GB", "NFb"], ebk)
                    P.dma(out[t * 128:(t + 1) * 128, :], xt, reads=ebk, writes=[("out", t)], eng="pool")
        A.free("B2f", "B1", "G2", "NFb", "W10", "W11", "W2", "ACC", "H2T", "GB", "GTs", "mW", "mU")
        if done(f"F{l}"): break

    P.emit()
    P.close()
    es.close()
    return nc, stages, ins


def make_inputs(inputs, b, names=None):
    global _TB
    if _TB is None:
        _TB = _tables()
    f = lambda a: np.ascontiguousarray(a, dtype=np.float32)
    d = {}
    lay = {
        "x": lambda: f(inputs["x"][b]),
        "ctx": lambda: f(inputs["ctx"][b]),
        "c2T": lambda: f(np.stack([inputs["c"][b], inputs["c_ctx"]], 0).reshape(2, 8, 128).transpose(2, 0, 1)),
        "b_inT": lambda: f(inputs["b_in"][:, :768].reshape(2, 6, 128).transpose(2, 0, 1)),
        "convw": lambda: f(inputs["hy_conv_w"].reshape(2, 3, 6, 128).transpose(3, 0, 1, 2)),
        "convb": lambda: f(inputs["hy_conv_b"].reshape(2, 6, 128).transpose(2, 0, 1)),
        "hyw1": lambda: f(inputs["hy_filt_w1"]), "hyw2": lambda: f(inputs["hy_filt_w2"]), "hyw3": lambda: f(inputs["hy_filt_w3"]),
        "hyvec": lambda: f(np.stack([inputs["hy_filt_b1"], inputs["hy_filt_b2"], inputs["hy_filt_b3"], inputs["hy_filt_freq"]], -1).transpose(1, 0, 2)),
        "hyout": lambda: f(inputs["hy_filt_out"]),
        "hyskip": lambda: f(inputs["hy_skip"].reshape(2, 2, 128).transpose(2, 0, 1)),
        "qkg": lambda: f(np.concatenate([np.tile(inputs["ga_q_norm"], (1, 6)), np.tile(inputs["ga_k_norm"], (1, 2))], 1)),
        "sink": lambda: f(inputs["wa_sink"]),
        "bnorm": lambda: f(inputs["branch_norm"]),
        "bnT": lambda: f(inputs["branch_norm"][:, :256].reshape(2, 2, 128).transpose(2, 0, 1)),
        "b1T": lambda: f(inputs["b_mlp1"].reshape(2, NE, 16, 128).transpose(3, 0, 1, 2)),
    }
    for k in ["w_ada", "b_ada", "norm_mix", "norm_ffn", "w_in", "b_in", "w_out", "b_out", "w_router", "b_router",
              "w_mlp1", "w_mlp2", "b_mlp2", "norm_final"]:
        lay[k] = (lambda k=k: f(inputs[k]))
    for k in (names if names is not None else list(lay.keys()) + list(_TB.keys())):
        d[k] = _TB[k] if k in _TB else lay[k]()
    return d


_SHARED = {}


def kernel(**inputs):
    nc, _, ins = build()
    names = list(ins.keys())
    shared = {}
    in_maps = []
    for b in range(8):
        m = make_inputs(inputs, b, [n for n in names if n in ("x", "ctx", "c2T")])
        if not shared:
            shared = make_inputs(inputs, 0, [n for n in names if n not in ("x", "ctx", "c2T")])
        m.update(shared)
        in_maps.append(m)
    res = run_bass_kernel_spmd(nc, in_maps, core_ids=list(range(8)))
    return np.stack([np.asarray(r["out"], dtype=np.float32) for r in res.results], 0)
```

```python
import math
import numpy as np
import ml_dtypes
import concourse.bass as bass
import concourse.mybir as mybir
from concourse.bass_utils import run_bass_kernel_spmd

F32 = mybir.dt.float32
BF16 = mybir.dt.bfloat16
ALU = mybir.AluOpType
AF = mybir.ActivationFunctionType
AX = mybir.AxisListType

SEM_LIMIT = 30000
DMA_POOL = 6

D = 1024
NLAT = 4096
NCTX = 256
T = NLAT + NCTX
NT = T // 128
NLT = NLAT // 128
EPS = 1e-6
NE = 32


class Op:
    __slots__ = ("eng", "fn", "sem", "val", "deps", "is_dma", "pre")

    def __init__(self, eng, fn, is_dma):
        self.eng = eng
        self.fn = fn
        self.is_dma = is_dma
        self.sem = None
        self.val = 0
        self.deps = []
        self.pre = None


class Prog:
    ENGS = ("pe", "act", "dve", "pool", "sp")

    def __init__(self, nc):
        self.nc = nc
        self.ops = []
        self.last_w = {}
        self.readers = {}
        self.cnt = {e: 0 for e in self.ENGS}
        self.cur_sem = {e: None for e in self.ENGS}
        self.dma_cnt = {e: 0 for e in self.ENGS}
        self.dma_sems = {e: [] for e in self.ENGS}
        self.dma_hist = {e: [] for e in self.ENGS}
        self._semctx = []
        self.bykey = {}

    def _new_sem(self, name):
        ctx = self.nc.semaphore(name)
        s = ctx.__enter__()
        self._semctx.append(ctx)
        return s

    @staticmethod
    def base(k):
        return k[0] if isinstance(k, tuple) else k

    def hazards_of(self, basename):
        ops = []
        for k in self.bykey.get(basename, ()):
            w = self.last_w.get(k)
            if w is not None:
                ops.append(w)
            ops.extend(self.readers.get(k, ()))
        return ops

    def inherit(self, newbase, ops):
        self.readers.setdefault(("#inh", newbase), []).extend(ops)

    def op(self, eng, fn, reads=(), writes=(), dma=False):
        o = Op(eng, fn, dma)
        deps = set()
        for r in reads:
            w = self.last_w.get(r)
            if w is not None:
                deps.add(w)
        for w in writes:
            lw = self.last_w.get(w)
            if lw is not None:
                deps.add(lw)
            for rd in self.readers.get(w, ()):
                deps.add(rd)
            inh = self.readers.get(("#inh", self.base(w)))
            if inh:
                deps.update(inh)
        for w in writes:
            self.last_w[w] = o
            self.readers[w] = []
            self.bykey.setdefault(self.base(w), set()).add(w)
        for r in reads:
            if r in writes:
                continue
            self.readers.setdefault(r, []).append(o)
            self.bykey.setdefault(self.base(r), set()).add(r)
        deps.discard(o)
        o.deps = list(deps)
        if dma:
            j = self.dma_cnt[eng]
            self.dma_cnt[eng] += 1
            if j < DMA_POOL:
                self.dma_sems[eng].append(self._new_sem(f"d_{eng}_{j}"))
            o.sem = self.dma_sems[eng][j % DMA_POOL]
            o.val = 16 * (j // DMA_POOL + 1)
            if j >= DMA_POOL:
                o.pre = self.dma_hist[eng][j - DMA_POOL]
            self.dma_hist[eng].append(o)
        else:
            if self.cur_sem[eng] is None or self.cnt[eng] >= SEM_LIMIT:
                self.cur_sem[eng] = self._new_sem(f"c_{eng}_{len(self._semctx)}")
                self.cnt[eng] = 0
            self.cnt[eng] += 1
            o.sem = self.cur_sem[eng]
            o.val = self.cnt[eng]
        self.ops.append(o)
        return o

    def dma(self, out, in_, reads=(), writes=(), eng="sp", **kw):
        return self.op(eng, lambda e: e.dma_start(out=out, in_=in_, **kw), reads, writes, dma=True)

    def emit(self):
        nc = self.nc
        per = {e: [] for e in self.ENGS}
        for o in self.ops:
            per[o.eng].append(o)
        engmap = {"pe": "tensor", "act": "scalar", "dve": "vector", "pool": "gpsimd", "sp": "sync"}

        def run(eng_name):
            def body(e):
                seen = {}
                for o in per[eng_name]:
                    waits = {}
                    dl = list(o.deps)
                    if o.pre is not None:
                        dl.append(o.pre)
                    for d in dl:
                        if d.eng == eng_name and eng_name == "pe" and not d.is_dma:
                            continue
                        k = id(d.sem)
                        if seen.get(k, 0) >= d.val:
                            continue
                        if k not in waits or waits[k][1] < d.val:
                            waits[k] = (d.sem, d.val)
                    for k, (s, v) in waits.items():
                        e.wait_ge(s, v)
                        seen[k] = v
                    ins = o.fn(e)
                    ins.then_inc(o.sem, 16 if o.is_dma else 1)
                tail = {}
                for o in per[eng_name]:
                    if o.is_dma:
                        k = id(o.sem)
                        if k not in tail or tail[k][1] < o.val:
                            tail[k] = (o.sem, o.val)
                for k, (s, v) in tail.items():
                    if seen.get(k, 0) < v:
                        e.wait_ge(s, v)
            return body

        with nc.Block() as block:
            for en in self.ENGS:
                if per[en]:
                    getattr(block, engmap[en])(run(en))

    def close(self):
        for c in reversed(self._semctx):
            c.__exit__(None, None, None)
        self._semctx = []


class Arena:
    def __init__(self, P, tensor, nwords):
        self.P = P
        self.t = tensor
        self.n = nwords
        self.live = {}
        self.dead = []

    def alloc(self, name, shape, dtype=F32):
        per = 1
        for s in shape[1:]:
            per *= s
        words = per if dtype == F32 else (per + 1) // 2
        segs = sorted(self.live.values())
        lo = 0
        pos = None
        for (a, b) in segs + [(self.n, self.n)]:
            if a - lo >= words:
                pos = lo
                break
            lo = max(lo, b)
        if pos is None:
            raise RuntimeError(f"arena full allocating {name} {words} words; live={self.live}")
        hi = pos + words
        self.live[name] = (pos, hi)
        ops = []
        nd = []
        for (a, b, o) in self.dead:
            if a < hi and pos < b:
                ops.extend(o)
            nd.append((a, b, o))
        self.dead = nd
        if ops:
            self.P.inherit(name, ops)
        v = self.t[:shape[0], pos:hi]
        if dtype != F32:
            v = v.bitcast(dtype)[:, 0:per]
        if len(shape) > 2:
            names = " ".join(f"d{i}" for i in range(len(shape) - 1))
            kw = {f"d{i}": shape[i + 1] for i in range(len(shape) - 1)}
            v = v.rearrange(f"p ({names}) -> p {names}", **kw)
        return v

    def free(self, *names):
        for name in names:
            lo, hi = self.live.pop(name)
            self.dead.append((lo, hi, self.P.hazards_of(name)))


def _tables():
    tb = {}
    rows = NLAT // 64
    row = np.repeat(np.arange(rows, dtype=np.float32), 64)
    col = np.tile(np.arange(64, dtype=np.float32), rows)
    pos = np.stack([row, col], -1)
    inv = (10000.0 ** (-np.arange(16, dtype=np.float32) / 16)).astype(np.float32)
    ang = pos[:, :, None] * inv
    cos = np.ones((T, 2, 16), np.float32)
    sin = np.zeros((T, 2, 16), np.float32)
    cos[:NLAT] = np.cos(ang)
    sin[:NLAT] = np.sin(ang)
    tb["ropeC"] = np.ascontiguousarray(cos.reshape(NT, 128, 32).transpose(1, 0, 2))
    tb["ropeS"] = np.ascontiguousarray(sin.reshape(NT, 128, 32).transpose(1, 0, 2))
    tb["ident"] = np.eye(128, dtype=np.float32)
    a = np.arange(128)
    m = np.zeros((128, 2, 128), np.float32)
    m[:, 0, :] = (a[None, :] <= a[:, None])
    m[:, 1, :] = (a[:, None] <= a[None, :])
    tb["wmask"] = m.astype(ml_dtypes.bfloat16)
    for n, tag in ((NLAT, "L"), (NCTX, "C")):
        nch = n // 128
        t = np.linspace(0.0, 1.0, n, dtype=np.float32)[:, None]
        wpos = (2.0 * math.pi / n) * np.arange(n, dtype=np.float32)[:, None]
        bands = np.linspace(1e-4, 15, 16, dtype=np.float32)
        z = np.concatenate([t, np.cos(wpos * bands), -np.sin(wpos * bands)], -1).astype(np.float32)
        zz = np.concatenate([z, z[::-1]], 0)
        tb["hyz" + tag] = np.ascontiguousarray(zz.T)
        deltas = np.linspace(math.log(1e-2) / 1.5, math.log(1e-2) / 0.3, 256, dtype=np.float32)
        decay = np.exp(-t * np.abs(deltas)).astype(np.float32)
        dd = np.stack([decay, decay[::-1]], 1)
        tb["hydec" + tag] = np.ascontiguousarray(dd.reshape(nch, 128, 512).transpose(1, 0, 2))
        N2 = 2 * n
        s = np.arange(n, dtype=np.int64)
        ph = (np.outer(s, s) % N2).astype(np.float64) * (2 * math.pi / N2)
        Fc = np.cos(ph)
        Fs = -np.sin(ph)
        Gs = Fs.copy()
        Fs[:, 0] = (-1.0) ** s
        Gs[0, :] = (-1.0) ** s
        def fwd_layout(M):
            return np.ascontiguousarray(M.reshape(nch, 128, nch, 128).transpose(2, 1, 0, 3).reshape(nch, 128, nch * 128)).astype(ml_dtypes.bfloat16)
        TC = 256
        ntc = n // TC
        def inv_layout(M):
            return np.ascontiguousarray(M.reshape(nch, 128, ntc, TC).transpose(2, 1, 0, 3).reshape(ntc, 128, nch * TC)).astype(ml_dtypes.bfloat16)
        tb["Fc" + tag] = fwd_layout(Fc)
        tb["Fs" + tag] = fwd_layout(Fs)
        tb["Gc" + tag] = inv_layout(Fc)
        tb["Gs" + tag] = inv_layout(Gs)
        sc = np.full((128, nch), 2.0 / N2, np.float32)
        sc[0, 0] = 1.0 / N2
        sg = np.where(np.arange(128) % 2 == 0, 1.0, -1.0).astype(np.float32)[:, None]
        tb["hysc" + tag] = np.ascontiguousarray(np.concatenate([sc, sg], 1))
    return tb


_TB = None


def build(nlayers=2, stop=None, dbg=()):
    nc = bass.Bass("TRN2", target_bir_lowering=False)
    ins = {}

    class Lazy:
        def __init__(self, name, shape, dt=F32):
            self.name, self.shape, self.dt, self.ap = name, list(shape), dt, None

        def __call__(self):
            if self.ap is None:
                self.ap = nc.dram_tensor(self.name, self.shape, self.dt, kind="ExternalInput").ap()
                ins[self.name] = self.ap
            return self.ap

    def din(name, shape, dt=F32):
        return Lazy(name, shape, dt)

    def dscr(name, shape, dt=F32):
        kind = "ExternalOutput" if name in dbg else "Internal"
        return nc.dram_tensor(name, list(shape), dt, kind=kind).ap()

    x_in = din("x", [NLAT, D]); ctx_in = din("ctx", [NCTX, D]); c2T = din("c2T", [128, 2, 8])
    w_ada = din("w_ada", [2, D, 6 * D]); b_ada = din("b_ada", [2, 6 * D])
    norm_mix = din("norm_mix", [2, D]); norm_ffn = din("norm_ffn", [2, D])
    w_in = din("w_in", [2, D, 2048]); b_in = din("b_in", [2, 2048]); b_inT = din("b_inT", [128, 2, 6])
    convw = din("convw", [128, 2, 3, 6]); convb = din("convb", [128, 2, 6])
    hyw1 = din("hyw1", [2, 33, 64]); hyw2 = din("hyw2", [2, 64, 64]); hyw3 = din("hyw3", [2, 64, 64])
    hyvec = din("hyvec", [64, 2, 4]); hyout = din("hyout", [2, 64, 512]); hyskip = din("hyskip", [128, 2, 2])
    qkg = din("qkg", [2, 512]); sink = din("sink", [2, 6]); bnorm = din("bnorm", [2, D]); bnT = din("bnT", [128, 2, 2])
    w_out = din("w_out", [2, D, D]); b_out = din("b_out", [2, D])
    w_router = din("w_router", [2, D, NE]); b_router = din("b_router", [2, NE])
    w_mlp1 = din("w_mlp1", [2, NE, D, 2 * D]); b1T = din("b1T", [128, 2, NE, 16])
    w_mlp2 = din("w_mlp2", [2, NE, D, D]); b_mlp2 = din("b_mlp2", [2, NE, D])
    norm_final = din("norm_final", [D])
    ropeC = din("ropeC", [128, NT, 32]); ropeS = din("ropeS", [128, NT, 32])
    ident_in = din("ident", [128, 128]); wmask_in = din("wmask", [128, 2, 128], BF16)
    HN = {"L": NLAT, "C": NCTX}
    hyz = {g: din("hyz" + g, [33, 2 * HN[g]]) for g in "LC"}
    hydec = {g: din("hydec" + g, [128, HN[g] // 128, 512]) for g in "LC"}
    hysc = {g: din("hysc" + g, [128, HN[g] // 128 + 1]) for g in "LC"}
    Fc = {g: din("Fc" + g, [HN[g] // 128, 128, HN[g]], BF16) for g in "LC"}
    Fs = {g: din("Fs" + g, [HN[g] // 128, 128, HN[g]], BF16) for g in "LC"}
    Gc = {g: din("Gc" + g, [HN[g] // 256, 128, HN[g] * 2], BF16) for g in "LC"}
    Gs = {g: din("Gs" + g, [HN[g] // 256, 128, HN[g] * 2], BF16) for g in "LC"}

    out = nc.dram_tensor("out", [NLAT, D], F32, kind="ExternalOutput").ap()
    MODD = dscr("MODD", [2, 6, 128, D])
    XR = dscr("XR", [T, D]); X1 = dscr("X1", [T, D])
    QKD = dscr("QKD", [10, 128, T], BF16)
    VD = dscr("VD", [NT, 128, 260], BF16)
    YNT = dscr("YNT", [8, 128, T], BF16)
    GATD = dscr("GATD", [NE, T])
    HYD = dscr("HYD", [3, 128, 2 * T], BF16)
    DBG = {k: None for k in ("DBGH", "DBGU", "DBGK", "DBGY", "DBGM")}
    if "DBGH" in dbg: DBG["DBGH"] = dscr("DBGH", [T, D], BF16)
    if "DBGK" in dbg: DBG["DBGK"] = dscr("DBGK", [NLAT, 512], BF16)
    if "DBGM" in dbg: DBG["DBGM"] = dscr("DBGM", [D, T])

    from contextlib import ExitStack
    es = ExitStack()
    SBW = 49152 - 1024
    Abig = es.enter_context(nc.sbuf_tensor("arena", [128, SBW], F32))
    PSb = es.enter_context(nc.psum_tensor("psum", [128, 8, 512], F32))
    P = Prog(nc)
    A = Arena(P, Abig, SBW)

    def psbf(bank, nb=1):
        return PSb[:, bank:bank + nb, :].rearrange("p b n -> p (b n)").bitcast(BF16)

    def TT(eng, out_, a, b, op, r, w):
        P.op(eng, lambda e: e.tensor_tensor(out=out_, in0=a, in1=b, op=op), r, w)

    def TS(eng, out_, a, s1, s2, op0, op1, r, w):
        if s2 is None:
            P.op(eng, lambda e: e.tensor_scalar(out=out_, in0=a, scalar1=s1, scalar2=None, op0=op0), r, w)
        else:
            P.op(eng, lambda e: e.tensor_scalar(out=out_, in0=a, scalar1=s1, scalar2=s2, op0=op0, op1=op1), r, w)

    def STT(eng, out_, a, s, b, op0, op1, r, w):
        P.op(eng, lambda e: e.scalar_tensor_tensor(out=out_, in0=a, scalar=s, in1=b, op0=op0, op1=op1), r, w)

    def ACT(out_, a, func, r, w, **kw):
        P.op("act", lambda e: e.activation(out=out_, in_=a, func=func, **kw), r, w)

    def CP(eng, out_, a, r, w):
        if eng == "act":
            P.op("act", lambda e: e.activation(out=out_, in_=a, func=AF.Copy), r, w)
        else:
            P.op(eng, lambda e: e.tensor_copy(out=out_, in_=a), r, w)

    def MM(out_, pairs, r, w):
        def f(e):
            n = len(pairs)
            for i, (l_, rh) in enumerate(pairs):
                ins_ = e.matmul(out_, lhsT=l_, rhs=rh, start=(i == 0), stop=(i == n - 1))
            return ins_
        P.op("pe", f, r, w)

    def MM1(out_, l_, rh, start, stop, r, w):
        P.op("pe", lambda e: e.matmul(out_, lhsT=l_, rhs=rh, start=start, stop=stop), r, w)

    def TR(outs_ins, ident, r, w):
        def f(e):
            for (o_, i_) in outs_ins:
                ins_ = e.transpose(out=o_, in_=i_, identity=ident)
            return ins_
        P.op("pe", f, r, w)

    def MS(eng, ap, val, w):
        P.op(eng, lambda e: e.memset(ap, val), (), w)

    def RECIP(ap, k):
        P.op("dve", lambda e: e.reciprocal(out=ap, in_=ap), k, k)

    def rstd_from_ssq(ssq, rs, scale, keyr, keyw):
        ACT(rs, ssq, AF.Sqrt, keyr, keyw, scale=scale, bias=EPS)
        RECIP(rs, keyw)

    identf = A.alloc("identf", [128, 128]); identb = A.alloc("identb", [128, 128], BF16)
    P.dma(identf, ident_in(), writes=["identf"])
    CP("dve", identb, identf, ["identf"], ["identb"])
    onesf = A.alloc("onesf", [128, 128])
    MS("pool", onesf, 1.0, ["onesf"])
    sil = A.alloc("sil", [128, 2, 8])
    P.dma(sil, c2T(), writes=["sil"])
    ACT(sil, sil, AF.Silu, ["sil"], ["sil"])

    stages = []

    def done(name):
        stages.append(name)
        return stop == name

    def modd_load(dst, v, j, key):
        P.dma(dst, MODD[v, j], reads=[("MODD", v, j, 0), ("MODD", v, j, 1)], writes=[key])

    def xsrc(l, t):
        if l == 0:
            return (x_in()[t * 128:(t + 1) * 128, :] if t < NLT else ctx_in()[(t - NLT) * 128:(t - NLT + 1) * 128, :]), []
        return XR[t * 128:(t + 1) * 128, :], [("XR", t)]

    def hyena(l, tag, tok0, X0T, ZT, ZTM):
        n = HN[tag]; nch = n // 128; TC = 256; ntc = n // TC
        KF = A.alloc("KF", [128, nch, 512], BF16)
        w1 = A.alloc("hw1", [33, 64]); w2 = A.alloc("hw2", [64, 64]); w3 = A.alloc("hw3", [64, 64])
        wo = A.alloc("hwo", [64, 512]); hv = A.alloc("hv", [64, 8])
        P.dma(w1, hyw1()[l], writes=["hw1"]); P.dma(w2, hyw2()[l], writes=["hw2"]); P.dma(w3, hyw3()[l], writes=["hw3"])
        P.dma(wo, hyout()[l], writes=["hwo"]); P.dma(hv[:, 0:4], hyvec()[:, l, :], writes=["hv"])
        for j in range(3):
            TT("dve", hv[:, 4 + j:5 + j], hv[:, j:j + 1], hv[:, 3:4], ALU.mult, ["hv"], ["hv"])
        CW = min(512, n)
        hz = [A.alloc(f"hz{i}", [33, CW]) for i in range(2)]
        hh = [A.alloc(f"hh{i}", [64, CW]) for i in range(3)]
        hm = A.alloc("hm", [64, CW])
        dec = [A.alloc(f"dec{i}", [128, CW // 128, 256]) for i in range(2)]
        ci = 0
        for half in range(2):
            for c0 in range(0, n, CW):
                zb = hz[ci % 2]; zk = f"hz{ci % 2}"
                P.dma(zb, hyz[tag]()[:, half * n + c0: half * n + c0 + CW], writes=[zk])
                db = dec[ci % 2]; dk = f"dec{ci % 2}"
                P.dma(db, hydec[tag]()[:, c0 // 128:(c0 + CW) // 128, half * 256:(half + 1) * 256], writes=[dk])
                src = zb; srck = zk; ws = [w1, w2, w3]; wks = ["hw1", "hw2", "hw3"]
                for j in range(3):
                    bk = ("ps", 4 + (j % 2))
                    pso = PSb[0:64, 4 + (j % 2), 0:CW]
                    MM(pso, [(ws[j], src)], [wks[j], srck], [bk])
                    hk = f"hh{j}"
                    TS("dve", hh[j], pso, hv[:, 3:4], hv[:, 4 + j:5 + j], ALU.mult, ALU.add, [bk, "hv"], [hk])
                    TS("pool", hm, hh[j], math.pi, 2.0 * math.pi, ALU.is_gt, ALU.mult, [hk], ["hm"])
                    TT("dve", hh[j], hh[j], hm, ALU.subtract, [hk, "hm"], [hk])
                    TS("pool", hm, hh[j], -math.pi, 2.0 * math.pi, ALU.is_lt, ALU.mult, [hk], ["hm"])
                    TT("dve", hh[j], hh[j], hm, ALU.add, [hk, "hm"], [hk])
                    ACT(hh[j], hh[j], AF.Sin, [hk], [hk])
                    src = hh[j]; srck = hk
                for tt_ in range(CW // 128):
                    bk = ("ps", 6 + (tt_ % 2))
                    pso = PSb[:, 6 + (tt_ % 2), 0:256]
                    MM(pso, [(hh[2][:, tt_ * 128:(tt_ + 1) * 128], wo[:, half * 256:(half + 1) * 256])], ["hh2", "hwo"], [bk])
                    TT("dve", KF[:, c0 // 128 + tt_, half * 256:(half + 1) * 256], pso, db[:, tt_, :], ALU.mult, [bk, dk], [("KF", c0 // 128 + tt_, half)])
                ci += 1
        A.free("hw1", "hw2", "hw3", "hwo", "hv", "hz0", "hz1", "hh0", "hh1", "hh2", "hm", "dec0", "dec1")
        if DBG["DBGK"] is not None and tag == "L" and l == 0:
            P.dma(DBG["DBGK"].rearrange("(c p) n -> p c n", p=128), KF, reads=[("KF", c, h) for c in range(nch) for h in range(2)], eng="pool")
        YRE = A.alloc("YRE", [128, nch, 256], BF16); YIM = A.alloc("YIM", [128, nch, 256], BF16)
        hs = A.alloc("hsc", [128, nch + 1])
        P.dma(hs, hysc[tag](), writes=["hsc"])
        ftab = [(A.alloc(f"fct{i}", [128, nch, 128], BF16), A.alloc(f"fst{i}", [128, nch, 128], BF16)) for i in range(2)]
        KA = A.alloc("KA", [128, 2, 512])
        KK = A.alloc("KK", [128, 4, 256])
        TMP = A.alloc("hTMP", [128, 4, 256])
        kfk = [("KF", c, h) for c in range(nch) for h in range(2)]
        ztk = [("ZTM", tok0 // 128 + c) for c in range(nch)]
        for fc in range(nch):
            fct, fst = ftab[fc % 2]; fk = f"fct{fc % 2}"; sk = f"fst{fc % 2}"
            P.dma(fct, Fc[tag]()[fc].rearrange("p (s j) -> p s j", j=128), writes=[fk])
            P.dma(fst, Fs[tag]()[fc].rearrange("p (s j) -> p s j", j=128), writes=[sk])
            MM(PSb[:, 0, :], [(fct[:, s_, :], KF[:, s_, :]) for s_ in range(nch)], [fk] + kfk, [("ps", 0)])
            MM(PSb[:, 1, 0:256], [(fct[:, s_, :], ZTM[:, tok0 // 128 + s_, :]) for s_ in range(nch)], [fk] + ztk, [("ps", 1)])
            MM(PSb[:, 2, :], [(fst[:, s_, :], KF[:, s_, :]) for s_ in range(nch)], [sk] + kfk, [("ps", 2)])
            MM(PSb[:, 3, 0:256], [(fst[:, s_, :], ZTM[:, tok0 // 128 + s_, :]) for s_ in range(nch)], [sk] + ztk, [("ps", 3)])
            CP("act", KA[:, 0, :], PSb[:, 0, :], [("ps", 0)], [("KA", 0)])
            CP("act", KA[:, 1, :], PSb[:, 2, :], [("ps", 2)], [("KA", 1)])
            sg = hs[:, nch:nch + 1]; scl = hs[:, fc:fc + 1]
            for ri in range(2):
                dst = KK[:, 0 if ri == 0 else 1, :]
                STT("dve", dst, KA[:, ri, 256:512], sg, KA[:, ri, 0:256], ALU.mult, ALU.add, [("KA", ri), "hsc"], [("KK", ri)])
                TS("pool", dst, dst, scl, None, ALU.mult, None, [("KK", ri), "hsc"], [("KK", ri)])
            CP("pool", KK[:, 2, :], KK[:, 1, :], [("KK", 1)], [("KK", 2)])
            CP("pool", KK[:, 3, :], KK[:, 0, :], [("KK", 0)], [("KK", 3)])
            if fc == 0:
                CP("pool", KK[0:1, 3, :], KK[0:1, 1, :], [("KK", 1), ("KK", 3)], [("KK", 3)])
                MS("pool", KK[0:1, 2, :], 0.0, [("KK", 2)])
                P.op("pool", lambda e: e.memset(KK[0:1, 1, :], 0.0), [("KK", 2), ("KK", 3)], [("KK", 1)])
            zre = PSb[:, 1, 0:256]; zim = PSb[:, 3, 0:256]
            TT("dve", TMP[:, 0, :], zre, KK[:, 0, :], ALU.mult, [("ps", 1), ("KK", 0)], [("hTMP", 0)])
            TT("dve", TMP[:, 1, :], zim, KK[:, 1, :], ALU.mult, [("ps", 3), ("KK", 1)], [("hTMP", 1)])
            TT("dve", TMP[:, 2, :], zre, KK[:, 2, :], ALU.mult, [("ps", 1), ("KK", 2)], [("hTMP", 2)])
            TT("dve", TMP[:, 3, :], zim, KK[:, 3, :], ALU.mult, [("ps", 3), ("KK", 3)], [("hTMP", 3)])
            TT("pool", YRE[:, fc, :], TMP[:, 0, :], TMP[:, 1, :], ALU.subtract, [("hTMP", 0), ("hTMP", 1)], [("YRE", fc)])
            TT("pool", YIM[:, fc, :], TMP[:, 2, :], TMP[:, 3, :], ALU.add, [("hTMP", 2), ("hTMP", 3)], [("YIM", fc)])
        A.free("KF", "hsc", "fct0", "fst0", "fct1", "fst1", "KA", "KK", "hTMP")
        gtab = [(A.alloc(f"gct{i}", [128, nch, TC], BF16), A.alloc(f"gst{i}", [128, nch, TC], BF16)) for i in range(2)]
        YH = A.alloc("YH", [128, 2, TC]); SQ = A.alloc("hSQ", [128, 2, TC]); RS = A.alloc("hRS", [128, TC])
        YO = A.alloc("hYO", [128, 2, TC], BF16)
        sk2 = A.alloc("hskip", [128, 4])
        P.dma(sk2[:, 0:2], hyskip()[:, l, :], writes=["hskip"])
        P.dma(sk2[:, 2:4], bnT()[:, l, :], writes=["hskip"])
        yk = [("YRE", c) for c in range(nch)] + [("YIM", c) for c in range(nch)]
        for tc in range(ntc):
            gct, gst = gtab[tc % 2]; gk = f"gct{tc % 2}"; gsk = f"gst{tc % 2}"
            P.dma(gct, Gc[tag]()[tc].rearrange("p (s j) -> p s j", j=TC), writes=[gk])
            P.dma(gst, Gs[tag]()[tc].rearrange("p (s j) -> p s j", j=TC), writes=[gsk])
            tsl = slice(tok0 + tc * TC, tok0 + (tc + 1) * TC)
            for cc in range(2):
                pso = PSb[:, 4 + cc, 0:TC]
                MM(pso, [(YRE[:, s_, cc * 128:(cc + 1) * 128], gct[:, s_, :]) for s_ in range(nch)] +
                        [(YIM[:, s_, cc * 128:(cc + 1) * 128], gst[:, s_, :]) for s_ in range(nch)], yk + [gk, gsk], [("ps", 4 + cc)])
                STT("dve", YH[:, cc, :], ZT[:, cc, tsl], sk2[:, cc:cc + 1], pso, ALU.mult, ALU.add, [("ZT", cc), "hskip", ("ps", 4 + cc)], [("YH", cc)])
                TT("pool", YH[:, cc, :], YH[:, cc, :], X0T[:, cc, tsl], ALU.mult, [("YH", cc), ("X0T", cc)], [("YH", cc)])
                ACT(SQ[:, cc, :], YH[:, cc, :], AF.Square, [("YH", cc)], [("hSQ", cc)])
            MM(PSb[:, 6, 0:TC], [(onesf, SQ[:, 0, :]), (onesf, SQ[:, 1, :])], ["onesf", ("hSQ", 0), ("hSQ", 1)], [("ps", 6)])
            ACT(RS, PSb[:, 6, 0:TC], AF.Sqrt, [("ps", 6)], ["hRS"], scale=1.0 / 256, bias=EPS)
            RECIP(RS, ["hRS"])
            for cc in range(2):
                STT("dve", YO[:, cc, :], YH[:, cc, :], sk2[:, 2 + cc:3 + cc], RS, ALU.mult, ALU.mult, [("YH", cc), "hskip", "hRS"], [("hYO", cc)])
            P.dma(YNT[0:2, :, tsl].rearrange("c p n -> p c n"), YO, reads=[("hYO", 0), ("hYO", 1)], writes=[("YNT", 0, tag, tc)], eng="pool")
        A.free("YRE", "YIM", "gct0", "gst0", "gct1", "gst1", "YH", "hSQ", "hRS", "hYO", "hskip")

    def attention(l, br, last):
        qi0 = 0 if br == "ga" else 5
        vi0 = 0 if br == "ga" else 2
        QT = A.alloc("QT", [128, 3, T], BF16); KT = A.alloc("KT", [128, 2, T], BF16); VA = A.alloc("VA", [128, NT, 4, 65], BF16)
        qkr = [("QKD", g_) for g_ in range(9)]
        P.dma(QT, QKD[qi0:qi0 + 3].rearrange("i p n -> p i n"), reads=qkr, writes=["QT"])
        P.dma(KT, QKD[qi0 + 3:qi0 + 5].rearrange("i p n -> p i n"), reads=qkr, writes=["KT"])
        P.dma(VA.rearrange("p t a b -> p t (a b)"), VD.rearrange("t p c -> p t c"), reads=[("VD", t) for t in range(NT)], writes=["VA"])
        BN = A.alloc("BNb", [128, 384])
        boff = 256 if br == "ga" else 640
        P.dma(BN, bnorm()[l][boff:boff + 384].partition_broadcast(128), writes=["BNb"])
        SKE = A.alloc("SKE", [128, 6])
        if br == "wa":
            P.dma(SKE, sink()[l].partition_broadcast(128), writes=["SKE"])
            ACT(SKE, SKE, AF.Exp, ["SKE"], ["SKE"])
            WM = A.alloc("WM", [128, 2, 128], BF16)
            P.dma(WM, wmask_in(), writes=["WM"])
        Yb = [A.alloc(f"Yb{i}", [128, 4, 384]) for i in range(2)]
        Pb = [A.alloc(f"Pb{i}", [128, 512], BF16) for i in range(2)]
        YNb = A.alloc("YNb", [128, 384], BF16)
        YS = [A.alloc(f"YS{i}", [128, 3, 512], BF16) for i in range(2)]
        st = A.alloc("ast", [128, 8])
        junk = A.alloc("ajunk", [128, 384])
        den = A.alloc("aden", [128, 8])
        chunks = [(c * 4, 4) for c in range(8)] + ([] if last else [(32, 2)])
        pcnt = 0
        for ci, (t0, ntl) in enumerate(chunks):
            nq = ntl * 128
            Y = Yb[ci % 2]; ykey = f"Yb{ci % 2}"
            isctx = t0 >= NLT
            for h in range(6):
                g = h // 3; pair = h // 2; b = 64 * (h % 2)
                order = 0 if b == 64 * g else 1
                if br == "ga":
                    units = [(list(range(ntl)), ([32, 33] if isctx else list(range(NT))))]
                else:
                    units = []
                    for qs in range(ntl):
                        i = t0 + qs
                        if isctx:
                            units.append(([qs], [32, 33]))
                        else:
                            units.append(([qs], [j for j in (i - 1, i, i + 1) if 0 <= j < NLT] + [32, 33]))
                for (qsl, keys) in units:
                    q0 = (t0 + qsl[0]) * 128; nqq = len(qsl) * 128
                    pend = None

                    def pv_emit(pd):
                        ji_, j_, pb__, pk__ = pd
                        for qq, qs in enumerate(qsl):
                            MM1(PSb[:, 2 + qs, 0:65], pb__[:, qq * 128:(qq + 1) * 128], VA[:, j_, vi0 + g, :], ji_ == 0, ji_ == len(keys) - 1, [pk__, "VA"], [("ps", 2 + qs)])
                    for ji, j in enumerate(keys):
                        sb_ = pcnt % 2; pcnt += 1
                        stp = PSb[:, sb_, 0:nqq]
                        MM(stp, [(KT[b:b + 64, order, j * 128:(j + 1) * 128], QT[b:b + 64, pair, q0:q0 + nqq])], ["KT", "QT"], [("ps", sb_)])
                        pb_ = Pb[sb_][:, 0:nqq]; pk = f"Pb{sb_}"
                        ACT(pb_, stp, AF.Exp, [("ps", sb_)], [pk], scale=0.125)
                        if br == "wa" and not isctx and j < NLT and j != t0 + qsl[0]:
                            mi = 0 if j < t0 + qsl[0] else 1
                            TT("pool", pb_, pb_, WM[:, mi, :], ALU.mult, [pk, "WM"], [pk])
                        if pend is not None:
                            pv_emit(pend)
                        pend = (ji, j, pb_, pk)
                    pv_emit(pend)
                    for qs in qsl:
                        dk = ("aden", qs)
                        if br == "wa":
                            TT("dve", den[:, qs:qs + 1], PSb[:, 2 + qs, 64:65], SKE[:, h:h + 1], ALU.add, [("ps", 2 + qs), "SKE"], [dk])
                        else:
                            CP("dve", den[:, qs:qs + 1], PSb[:, 2 + qs, 64:65], [("ps", 2 + qs)], [dk])
                        RECIP(den[:, qs:qs + 1], [dk])
                        TS("dve", Y[:, qs, h * 64:(h + 1) * 64], PSb[:, 2 + qs, 0:64], den[:, qs:qs + 1], None, ALU.mult, None, [("ps", 2 + qs), dk], [(ykey, qs)])
            ys = YS[ci % 2]; ysk = f"YS{ci % 2}"
            for qs in range(ntl):
                sk_ = ("ast", qs % 2)
                sq = st[:, (qs % 2) * 2:(qs % 2) * 2 + 1]; rs = st[:, (qs % 2) * 2 + 1:(qs % 2) * 2 + 2]
                ACT(junk, Y[:, qs, :], AF.Square, [(ykey, qs)], ["ajunk", sk_], accum_out=sq)
                rstd_from_ssq(sq, rs, 1.0 / 384, [sk_], [sk_])
                STT("dve", YNb, Y[:, qs, :], rs, BN, ALU.mult, ALU.mult, [(ykey, qs), sk_, "BNb"], ["YNb"])
                pb = psbf(6 + qs % 2)
                TR([(pb[:, k * 128:(k + 1) * 128], YNb[:, k * 128:(k + 1) * 128]) for k in range(3)], identb, ["YNb", "identb"], [("ps", 6 + qs % 2)])
                CP("act", ys[:, :, qs * 128:(qs + 1) * 128], pb[:, 0:384].rearrange("p (k c) -> p k c", k=3), [("ps", 6 + qs % 2)], [(ysk, qs)])
            c0 = 2 if br == "ga" else 5
            P.dma(YNT[c0:c0 + 3, :, t0 * 128:t0 * 128 + nq].rearrange("c p n -> p c n"), ys[:, :, 0:nq], reads=[(ysk, qs) for qs in range(ntl)], writes=[("YNT", br, ci)], eng="pool")
        A.free("QT", "KT", "VA", "BNb", "SKE", "Yb0", "Yb1", "Pb0", "Pb1", "YNb", "YS0", "YS1", "ast", "ajunk", "aden")
        if br == "wa":
            A.free("WM")

    for l in range(nlayers):
        last = (l == 1)
        ntl_l = NLT if last else NT
        silB = A.alloc("silB", [128, 2, 8, 128])
        for v in range(2):
            for k in range(8):
                CP("dve", silB[:, v, k, :], sil[:, v, k:k + 1].to_broadcast([128, 128]), ["sil"], [("silB", v, k)])
        wst = [A.alloc(f"wada{i}", [128, 8, 512]) for i in range(2)]
        bb = A.alloc("badab", [128, 512]); nb = A.alloc("normb", [128, 2, D])
        P.dma(nb[:, 0, :], norm_mix()[l].partition_broadcast(128), writes=[("normb", 0)])
        P.dma(nb[:, 1, :], norm_ffn()[l].partition_broadcast(128), writes=[("normb", 1)])
        mo = [A.alloc(f"modo{i}", [128, 512]) for i in range(2)]
        for n_ in range(12):
            ws = wst[n_ % 2]; wk = f"wada{n_ % 2}"
            P.dma(ws, w_ada()[l][:, n_ * 512:(n_ + 1) * 512].rearrange("(k p) n -> p k n", p=128), writes=[wk])
            P.dma(bb, b_ada()[l][n_ * 512:(n_ + 1) * 512].partition_broadcast(128), writes=["badab"])
            j = n_ // 2; hf = n_ % 2
            for v in range(2):
                MM(PSb[:, v, :], [(silB[:, v, k, :], ws[:, k, :]) for k in range(8)], [wk] + [("silB", v, k) for k in range(8)], [("ps", v)])
                m_ = mo[v]; mk = f"modo{v}"
                TT("dve", m_, PSb[:, v, :], bb, ALU.add, [("ps", v), "badab"], [mk])
                if j in (1, 4):
                    nsel = 0 if j == 1 else 1
                    STT("dve", m_, m_, 1.0, nb[:, nsel, hf * 512:(hf + 1) * 512], ALU.add, ALU.mult, [mk, ("normb", nsel)], [mk])
                P.dma(MODD[v, j, :, hf * 512:(hf + 1) * 512], m_, reads=[mk], writes=[("MODD", v, j, hf)], eng="pool")
        A.free("wada0", "wada1", "badab", "normb", "modo0", "modo1", "silB")
        if done(f"mod{l}"): break

        HT = A.alloc("HT", [128, 8, T], BF16)
        a1 = A.alloc("a1", [128, 2, D]); s1 = A.alloc("s1", [128, 2, D])
        for v in range(2):
            modd_load(a1[:, v, :], v, 1, ("a1", v)); modd_load(s1[:, v, :], v, 0, ("s1", v))
        xts = [A.alloc(f"xt{i}", [128, D]) for i in range(2)]
        junk = A.alloc("junk", [128, D])
        hb = [A.alloc(f"hb{i}", [128, D], BF16) for i in range(2)]
        st = A.alloc("stat", [128, 4])
        for t in range(NT):
            v = 0 if t < NLT else 1
            xt = xts[t % 2]; xk = f"xt{t % 2}"
            src, rk = xsrc(l, t)
            P.dma(xt, src, reads=rk, writes=[xk])
            sq = st[:, (t % 2) * 2:(t % 2) * 2 + 1]; rs = st[:, (t % 2) * 2 + 1:(t % 2) * 2 + 2]; sk = ("stat", t % 2)
            ACT(junk, xt, AF.Square, [xk], ["junk", sk], accum_out=sq)
            rstd_from_ssq(sq, rs, 1.0 / D, [sk], [sk])
            STT("dve", xt, xt, rs, a1[:, v, :], ALU.mult, ALU.mult, [xk, sk, ("a1", v)], [xk])
            h_ = hb[t % 2]; hk = f"hb{t % 2}"
            TT("pool", h_, xt, s1[:, v, :], ALU.add, [xk, ("s1", v)], [hk])
            if DBG["DBGH"] is not None and l == 0:
                P.dma(DBG["DBGH"][t * 128:(t + 1) * 128, :], h_, reads=[hk], eng="pool")
            pb = psbf(t % 2).rearrange("p (k c) -> p k c", k=8)
            TR([(pb[:, k, :], h_[:, k * 128:(k + 1) * 128]) for k in range(8)], identb, [hk, "identb"], [("ps", t % 2)])
            CP("act", HT[:, :, t * 128:(t + 1) * 128], pb, [("ps", t % 2)], [("HT", t)])
        A.free("a1", "s1", "xt0", "xt1", "junk", "hb0", "hb1", "stat")
        if done(f"A{l}"): break

        WIN = A.alloc("WIN", [128, 8, 2048], BF16)
        for k in range(8):
            P.dma(WIN[:, k, :], w_in()[l][k * 128:(k + 1) * 128, :], writes=[("WIN", k)], eng="pool")
        htk = [("HT", t) for t in range(NT)]
        wink = [("WIN", k) for k in range(8)]

        X0T = A.alloc("X0T", [128, 2, T], BF16); ZT = A.alloc("ZT", [128, 2, T], BF16)
        bT = A.alloc("bT", [128, 6]); cw = A.alloc("cw", [128, 3, 6]); cb = A.alloc("cb", [128, 6])
        P.dma(bT, b_inT()[:, l, :], writes=["bT"]); P.dma(cw, convw()[:, l], writes=["cw"]); P.dma(cb, convb()[:, l], writes=["cb"])
        U = A.alloc("U", [128, T + 4]); UC1 = A.alloc("UC1", [128, T], BF16); UC = A.alloc("UC", [128, T])
        MS("pool", U, 0.0, ["U"])
        segs = [(0, NLAT, 1), (NLAT, NCTX, NLAT + 3)]
        for m in (0, 1, 2, 4, 3, 5):
            ck = 0
            for (tk0, n_, uo) in segs:
                for c0 in range(0, n_, 512):
                    w_ = min(512, n_ - c0); bkk = ("ps", ck % 2)
                    MM(PSb[:, ck % 2, 0:w_], [(WIN[:, k, m * 128:(m + 1) * 128], HT[:, k, tk0 + c0:tk0 + c0 + w_]) for k in range(8)], wink + htk, [bkk])
                    ACT(U[:, uo + c0:uo + c0 + w_], PSb[:, ck % 2, 0:w_], AF.Identity, [bkk, "bT"], ["U"], bias=bT[:, m:m + 1])
                    ck += 1
            dst = UC1 if m in (2, 3) else UC; dk = "UC1" if m in (2, 3) else "UC"
            for (tk0, n_, uo) in segs:
                d_ = dst[:, tk0:tk0 + n_]
                TS("dve", d_, U[:, uo:uo + n_], cw[:, 1, m:m + 1], cb[:, m:m + 1], ALU.mult, ALU.add, ["U", "cw", "cb"], [dk])
                STT("dve", d_, U[:, uo - 1:uo - 1 + n_], cw[:, 0, m:m + 1], d_, ALU.mult, ALU.add, ["U", "cw", dk], [dk])
                STT("dve", d_, U[:, uo + 1:uo + 1 + n_], cw[:, 2, m:m + 1], d_, ALU.mult, ALU.add, ["U", "cw", dk], [dk])
            if m < 2:
                CP("pool", X0T[:, m, :], UC, ["UC"], [("X0T", m)])
            elif m >= 4:
                TT("pool", ZT[:, m - 4, :], UC1, UC, ALU.mult, ["UC", "UC1"], [("ZT", m - 4)])
        A.free("bT", "cw", "cb", "U", "UC1", "UC")
        ZTM = A.alloc("ZTM", [128, NT, 256], BF16)
        for t0 in range(0, NT, 4):
            nt_ = min(4, NT - t0); bnk = (t0 // 4) % 2
            pb = psbf(bnk).rearrange("p (t c) -> p t c", t=4)
            TR([(pb[:, tt_, cc * 128:(cc + 1) * 128], ZT[:, cc, (t0 + tt_) * 128:(t0 + tt_ + 1) * 128]) for tt_ in range(nt_) for cc in range(2)], identb,
               [("ZT", 0), ("ZT", 1), "identb"], [("ps", bnk)])
            for tt_ in range(nt_):
                CP("act", ZTM[:, t0 + tt_, :], pb[:, tt_, :], [("ps", bnk)], [("ZTM", t0 + tt_)])
        for i_, (nm_, tl_) in enumerate((("X0T", X0T), ("ZT", ZT), ("ZTM", ZTM))):
            P.dma(HYD[i_], tl_.rearrange("p a b -> p (a b)"), reads=[k_ for k_ in P.bykey.get(nm_, ())], writes=[("HYD", i_)], eng="pool")
        A.free("X0T", "ZT", "ZTM")
        if done(f"B1{l}"): break

        PTs = [A.alloc(f"PT{i}", [128, 1280]) for i in range(2)]
        RQs = [A.alloc(f"RQ{i}", [128, 1536], BF16) for i in range(2)]
        SQ = A.alloc("SQ", [128, 512]); st8 = A.alloc("st8", [128, 2, 8])
        QKG = A.alloc("QKG", [128, 512]); BINB = A.alloc("BINB", [128, 1280])
        P.dma(QKG, qkg()[l].partition_broadcast(128), writes=["QKG"])
        P.dma(BINB, b_in()[l][768:2048].partition_broadcast(128), writes=["BINB"])
        RC = A.alloc("RC", [128, NT, 32]); RSn = A.alloc("RSn", [128, NT, 32])
        P.dma(RC, ropeC(), writes=["RC"]); P.dma(RSn, ropeS(), writes=["RSn"])
        VAUG = [A.alloc(f"VAUG{i}", [128, 4, 65], BF16) for i in range(2)]
        for i in range(2):
            MS("pool", VAUG[i], 1.0, [f"VAUG{i}"])
        QKT = [A.alloc(f"QKT{i}", [128, 10, 512], BF16) for i in range(2)]
        TMPS = [A.alloc(f"rt{i}", [128, 8, 2, 16]) for i in range(4)]
        tcols = [(0, 128), (128, 256), (256, 384), (384, 512), (1280, 1408), (640, 768), (768, 896), (896, 1024), (1024, 1152), (1408, 1536)]
        for t in range(NT):
            PT = PTs[t % 2]; pk = f"PT{t % 2}"; RQ = RQs[t % 2]; rqk = f"RQ{t % 2}"
            b0 = 3 * (t % 2)
            for pi, (c0, c1) in enumerate(((768, 1280), (1280, 1792), (1792, 2048))):
                MM(PSb[:, b0 + pi, 0:c1 - c0], [(HT[:, k, t * 128:(t + 1) * 128], WIN[:, k, c0:c1]) for k in range(8)], wink + [("HT", t)], [("ps", b0 + pi)])
                TT("dve", PT[:, c0 - 768:c1 - 768], PSb[:, b0 + pi, 0:c1 - c0], BINB[:, c0 - 768:c1 - 768], ALU.add, [("ps", b0 + pi), "BINB"], [(pk, pi)])
            pkall = [(pk, 0), (pk, 1), (pk, 2)]
            sk = ("st8", t % 2)
            ACT(SQ, PT[:, 0:512], AF.Square, [(pk, 0)], ["SQ"])
            P.op("dve", lambda e, o_=st8[:, t % 2, :], i_=SQ.rearrange("p (h d) -> p h d", d=64): e.tensor_reduce(out=o_, in_=i_, axis=AX.X, op=ALU.add), ["SQ"], [sk])
            ACT(st8[:, t % 2, :], st8[:, t % 2, :], AF.Sqrt, [sk], [sk], scale=1.0 / 64, bias=EPS)
            RECIP(st8[:, t % 2, :], [sk])
            pv = PT[:, 0:512].rearrange("p (h d) -> p h d", d=64)
            TT("dve", pv, pv, st8[:, t % 2, :].unsqueeze(2).to_broadcast([128, 8, 64]), ALU.mult, [(pk, 0), sk], [(pk, 0)])
            TT("dve", PT[:, 0:512], PT[:, 0:512], QKG, ALU.mult, [(pk, 0), "QKG"], [(pk, 0)])
            Cb = RC[:, t, :].rearrange("p (a f) -> p a f", a=2).unsqueeze(1).to_broadcast([128, 8, 2, 16])
            Sb = RSn[:, t, :].rearrange("p (a f) -> p a f", a=2).unsqueeze(1).to_broadcast([128, 8, 2, 16])
            for ri, base in enumerate((0, 640)):
                xv = PT[:, base:base + 512].rearrange("p (h a s f) -> p h a s f", h=8, a=2, s=2)
                ov = RQ[:, base:base + 512].rearrange("p (h a s f) -> p h a s f", h=8, a=2, s=2)
                x1 = xv[:, :, :, 0, :]; x2 = xv[:, :, :, 1, :]
                rk = pkall
                tk = [(f"rt{i}", ri) for i in range(4)]
                TT("dve", TMPS[0], x1, Cb, ALU.mult, rk + ["RC"], [tk[0]])
                TT("dve", TMPS[1], x2, Sb, ALU.mult, rk + ["RSn"], [tk[1]])
                TT("dve", ov[:, :, :, 0, :], TMPS[0], TMPS[1], ALU.subtract, [tk[0], tk[1]], [(rqk, ri, 0)])
                TT("pool", TMPS[2], x2, Cb, ALU.mult, rk + ["RC"], [tk[2]])
                TT("dve", TMPS[3], x1, Sb, ALU.mult, rk + ["RSn"], [tk[3]])
                TT("pool", ov[:, :, :, 1, :], TMPS[2], TMPS[3], ALU.add, [tk[2], tk[3]], [(rqk, ri, 1)])
            rqall = [(rqk, 0, 0), (rqk, 0, 1), (rqk, 1, 0), (rqk, 1, 1)]
            for (dst0, s0) in ((1280, 448), (1344, 384), (1408, 1088), (1472, 1024)):
                CP("pool", RQ[:, dst0:dst0 + 64], RQ[:, s0:s0 + 64], rqall, [(rqk, "sw", dst0)])
            rqall = rqall + [(rqk, "sw", d_) for d_ in (1280, 1344, 1408, 1472)]
            va = VAUG[t % 2]; vk = f"VAUG{t % 2}"
            CP("act", va[:, 0:2, 0:64], PT[:, 512:640].rearrange("p (h d) -> p h d", h=2), [(pk, 1)], [vk])
            CP("act", va[:, 2:4, 0:64], PT[:, 1152:1280].rearrange("p (h d) -> p h d", h=2), [(pk, 2)], [vk])
            P.dma(VD[t], va.rearrange("p a b -> p (a b)"), reads=[vk], writes=[("VD", t)], eng="pool")
            pb = psbf(6, 2)
            TR([(pb[:, i * 128:(i + 1) * 128], RQ[:, c0:c1]) for i, (c0, c1) in enumerate(tcols)], identb, rqall + ["identb"], [("ps", 6)])
            g4 = t // 4; qt = QKT[g4 % 2]; qtk = f"QKT{g4 % 2}"
            CP("act", qt[:, :, (t % 4) * 128:(t % 4 + 1) * 128], pb[:, 0:1280].rearrange("p (i c) -> p i c", i=10), [("ps", 6)], [(qtk, t % 4)])
            if t % 4 == 3 or t == NT - 1:
                t0 = (t // 4) * 4; nn = (t - t0 + 1) * 128
                P.dma(QKD[:, :, t0 * 128:t0 * 128 + nn].rearrange("i p n -> p i n"), qt[:, :, 0:nn], reads=[(qtk, i) for i in range(t - t0 + 1)], writes=[("QKD", t // 4)], eng="pool")
        A.free("PT0", "PT1", "RQ0", "RQ1", "SQ", "st8", "QKG", "BINB", "RC", "RSn", "VAUG0", "VAUG1", "QKT0", "QKT1", "rt0", "rt1", "rt2", "rt3")
        A.free("HT", "WIN")
        if done(f"B2{l}"): break

        X0T = A.alloc("X0T", [128, 2, T], BF16); ZT = A.alloc("ZT", [128, 2, T], BF16); ZTM = A.alloc("ZTM", [128, NT, 256], BF16)
        P.dma(X0T.rearrange("p a b -> p (a b)"), HYD[0], reads=[("HYD", 0)], writes=[("X0T", 0), ("X0T", 1)])
        P.dma(ZT.rearrange("p a b -> p (a b)"), HYD[1], reads=[("HYD", 1)], writes=[("ZT", 0), ("ZT", 1)])
        P.dma(ZTM.rearrange("p a b -> p (a b)"), HYD[2], reads=[("HYD", 2)], writes=[("ZTM", t_) for t_ in range(NT)])
        hyena(l, "L", 0, X0T, ZT, ZTM)
        if not last:
            hyena(l, "C", NLAT, X0T, ZT, ZTM)
        A.free("X0T", "ZT", "ZTM")
        if done(f"C{l}"): break

        attention(l, "ga", last)
        if done(f"Dga{l}"): break
        attention(l, "wa", last)
        if done(f"D{l}"): break

        YN = A.alloc("YN", [128, 8, T], BF16)
        ynr = [("YNT", 0, "L", tc) for tc in range(NLAT // 256)] + ([] if last else [("YNT", 0, "C", 0)]) + \
              [("YNT", br, ci) for br in ("ga", "wa") for ci in range(8 if last else 9)]
        P.dma(YN, YNT.rearrange("c p n -> p c n"), reads=ynr, writes=["YN"])
        WO = A.alloc("WO", [128, 8, D], BF16)
        for k in range(8):
            P.dma(WO[:, k, :], w_out()[l][k * 128:(k + 1) * 128, :], writes=[("WO", k)], eng="pool")
        wok = [("WO", k) for k in range(8)]
        BO = A.alloc("BO", [128, D]); G1 = A.alloc("G1", [128, 2, D])
        P.dma(BO, b_out()[l].partition_broadcast(128), writes=["BO"])
        for v in range(2):
            modd_load(G1[:, v, :], v, 2, ("G1", v))
        xts = [A.alloc(f"xt{i}", [128, D]) for i in range(2)]
        tm = [A.alloc(f"tm{i}", [128, D]) for i in range(2)]
        for t in range(ntl_l):
            v = 0 if t < NLT else 1
            xt = xts[t % 2]; xk = f"xt{t % 2}"; tmp = tm[t % 2]; tk = f"tm{t % 2}"
            src, rk = xsrc(l, t)
            P.dma(xt, src, reads=rk, writes=[xk])
            for hf in range(2):
                bk = ("ps", (t % 2) * 2 + hf)
                MM(PSb[:, (t % 2) * 2 + hf, :], [(YN[:, k, t * 128:(t + 1) * 128], WO[:, k, hf * 512:(hf + 1) * 512]) for k in range(8)], ["YN"] + wok, [bk])
                TT("dve", tmp[:, hf * 512:(hf + 1) * 512], PSb[:, (t % 2) * 2 + hf, :], BO[:, hf * 512:(hf + 1) * 512], ALU.add, [bk, "BO"], [(tk, hf)])
            TT("pool", tmp, tmp, G1[:, v, :], ALU.mult, [(tk, 0), (tk, 1), ("G1", v)], [(tk, 0), (tk, 1)])
            TT("pool", xt, xt, tmp, ALU.add, [xk, (tk, 0), (tk, 1)], [xk])
            P.dma(X1[t * 128:(t + 1) * 128, :], xt, reads=[xk], writes=[("X1", t)], eng="pool")
        A.free("YN", "WO", "BO", "G1", "xt0", "xt1", "tm0", "tm1")
        if done(f"E{l}"): break

        GT = A.alloc("GT", [32, T])
        a2 = A.alloc("a2", [128, 2, D]); s2 = A.alloc("s2", [128, 2, D])
        for v in range(2):
            modd_load(a2[:, v, :], v, 4, ("a2", v)); modd_load(s2[:, v, :], v, 3, ("s2", v))
        WR = A.alloc("WR", [128, 8, NE]); BR = A.alloc("BR", [128, NE])
        P.dma(WR, w_router()[l].rearrange("(k p) e -> p k e", p=128), writes=["WR"])
        P.dma(BR, b_router()[l].partition_broadcast(128), writes=["BR"])
        H2D = dscr(f"H2D{l}", [8, 128, T], BF16)
        xts = [A.alloc(f"xt{i}", [128, D]) for i in range(2)]
        junk = A.alloc("junk", [128, D]); h2f = A.alloc("h2f", [128, 8, 128])
        h2b = [A.alloc(f"h2b{i}", [128, 8, 128], BF16) for i in range(2)]
        st = A.alloc("stat", [128, 4]); lg = A.alloc("lg", [128, NE]); m8 = A.alloc("m8", [128, 8]); msk = A.alloc("msk", [128, NE])
        gs = A.alloc("gs", [128, 4])
        for t in range(ntl_l):
            v = 0 if t < NLT else 1
            xt = xts[t % 2]; xk = f"xt{t % 2}"
            P.dma(xt, X1[t * 128:(t + 1) * 128, :], reads=[("X1", t)], writes=[xk])
            sq = st[:, (t % 2) * 2:(t % 2) * 2 + 1]; rs = st[:, (t % 2) * 2 + 1:(t % 2) * 2 + 2]; sk = ("stat", t % 2)
            ACT(junk, xt, AF.Square, [xk], ["junk", sk], accum_out=sq)
            rstd_from_ssq(sq, rs, 1.0 / D, [sk], [sk])
            STT("dve", xt, xt, rs, a2[:, v, :], ALU.mult, ALU.mult, [xk, sk, ("a2", v)], [xk])
            TT("pool", xt, xt, s2[:, v, :], ALU.add, [xk, ("s2", v)], [xk])
            pf = PSb[:, 0:2, :].rearrange("p b n -> p (b n)").rearrange("p (k c) -> p k c", k=8)
            TR([(pf[:, k, :], xt[:, k * 128:(k + 1) * 128]) for k in range(8)], identf, [xk, "identf"], [("ps", 0), ("ps", 1)])
            CP("act", h2f, pf, [("ps", 0), ("ps", 1)], ["h2f"])
            hb_ = h2b[t % 2]; hbk = f"h2b{t % 2}"
            CP("pool", hb_, h2f, ["h2f"], [hbk])
            P.dma(H2D[:, :, t * 128:(t + 1) * 128].rearrange("k p n -> p k n"), hb_, reads=[hbk], writes=[("H2D", t)], eng="pool")
            MM(PSb[:, 2, 0:NE], [(h2f[:, k, :], WR[:, k, :]) for k in range(8)], ["h2f", "WR"], [("ps", 2)])
            TT("dve", lg, PSb[:, 2, 0:NE], BR, ALU.add, [("ps", 2), "BR"], ["lg"])
            P.op("dve", lambda e: e.max(out=m8, in_=lg), ["lg"], ["m8"])
            TS("dve", msk, lg, m8[:, 3:4], None, ALU.is_ge, None, ["lg", "m8"], ["msk"])
            TS("dve", gs[:, 0:1], m8[:, 0:1], -1.0, None, ALU.mult, None, ["m8"], ["gs"])
            ACT(lg, lg, AF.Exp, ["lg", "gs"], ["lg"], bias=gs[:, 0:1])
            TT("dve", lg, lg, msk, ALU.mult, ["lg", "msk"], ["lg"])
            P.op("dve", lambda e: e.tensor_reduce(out=gs[:, 1:2], in_=lg, axis=AX.X, op=ALU.add), ["lg"], ["gs"])
            RECIP(gs[:, 1:2], ["gs"])
            TS("dve", lg, lg, gs[:, 1:2], None, ALU.mult, None, ["lg", "gs"], ["lg"])
            TR([(PSb[0:32, 3, 0:128], lg)], identf, ["lg", "identf"], [("ps", 3)])
            CP("act", GT[:, t * 128:(t + 1) * 128], PSb[0:32, 3, 0:128], [("ps", 3)], [("GT", t)])
        gtk = [("GT", t) for t in range(ntl_l)]
        ntok = ntl_l * 128
        P.dma(GATD[:, 0:ntok], GT[:, 0:ntok], reads=gtk, writes=["GATD"], eng="pool")
        A.free("a2", "s2", "WR", "BR", "xt0", "xt1", "junk", "h2f", "h2b0", "h2b1", "stat", "lg", "m8", "msk", "gs", "GT")
        if done(f"F1{l}"): break

        B2f = A.alloc("B2f", [32, D])
        P.dma(B2f, b_mlp2()[l], writes=["B2f"])
        B1 = A.alloc("B1", [128, NE, 16])
        P.dma(B1, b1T()[:, l], writes=["B1"])
        TS("dve", B1[:, :, 8:16], B1[:, :, 8:16], 1.0, None, ALU.add, None, ["B1"], ["B1"])
        G2 = A.alloc("G2", [128, 2, D])
        for v in range(2):
            modd_load(G2[:, v, :], v, 5, ("G2", v))
        NFb = A.alloc("NFb", [128, D])
        if last:
            P.dma(NFb, norm_final().partition_broadcast(128), writes=["NFb"])
        scsz = [8, 8, 8, 8] if last else [9, 9, 8, 8]
        SCT = max(scsz)
        W1s = [A.alloc(f"W1{i}", [128, 8, 2048], BF16) for i in range(2)]; W2 = A.alloc("W2", [128, 8, D], BF16)
        ACC = A.alloc("ACC", [128, 8, SCT * 128]); H2T = A.alloc("H2T", [128, 8, SCT * 128], BF16)
        GB = A.alloc("GB", [128, SCT * 128]); GTs = A.alloc("GTs", [32, SCT * 128])
        mW = A.alloc("mW", [128, 8, 512])
        Gt = [mW[:, 4 * i + 0, :] for i in range(2)]; St = [mW[:, 4 * i + 1, :] for i in range(2)]
        Lt = [mW[:, 4 * i + 2, :] for i in range(2)]; Xt = [mW[:, 4 * i + 3, :] for i in range(2)]
        Ub = A.alloc("mU", [128, 8, 512], BF16)
        w2k = [("W2", k) for k in range(8)]
        wi = 0; ei = 0
        sclist = []
        acc_ = 0
        for sz in scsz:
            sclist.append((acc_, sz)); acc_ += sz
        assert acc_ == ntl_l

        def load_w1(e_, buf):
            for k in range(0, 8, 2):
                P.dma(W1s[buf][:, k:k + 2, :], w_mlp1()[l, e_, k * 128:(k + 2) * 128, :].rearrange("(a p) n -> p a n", p=128), writes=[(f"W1{buf}", k), (f"W1{buf}", k + 1)], eng="pool")

        def load_w2(e_):
            for k in range(0, 8, 4):
                P.dma(W2[:, k:k + 4, :], w_mlp2()[l, e_, k * 128:(k + 4) * 128, :].rearrange("(a p) n -> p a n", p=128), writes=[("W2", k_) for k_ in range(k, k + 4)], eng="pool")
        load_w1(0, 0)
        load_w2(0)
        for sci, (sc0, nts) in enumerate(sclist):
            tk0 = sc0 * 128; ntk = nts * 128
            P.dma(H2T[:, :, 0:ntk], H2D[:, :, tk0:tk0 + ntk].rearrange("k p n -> p k n"), reads=[("H2D", t) for t in range(sc0, sc0 + nts)], writes=["H2T"])
            P.dma(GTs[:, 0:ntk], GATD[:, tk0:tk0 + ntk], reads=["GATD"], writes=["GTs"])
            cks = [(c0, min(512, ntk - c0)) for c0 in range(0, ntk, 512)]
            for (c0, w_) in cks:
                for dc in range(8):
                    bk = ("ps", 6 + dc % 2)
                    MM(PSb[:, 6 + dc % 2, 0:w_], [(B2f[:, dc * 128:(dc + 1) * 128], GTs[:, c0:c0 + w_])], ["B2f", "GTs"], [bk])
                    CP("act", ACC[:, dc, c0:c0 + w_], PSb[:, 6 + dc % 2, 0:w_], [bk], [("ACC", dc, c0)])
            units = [(e_, ci_, jf) for e_ in range(NE) for ci_ in range(len(cks)) for jf in range(8)]
            pend = None
            pend2 = None

            def tail(pd):
                L__, X__, lk__, xk__, jf__, c0__, w__ = pd
                STT("dve", L__, L__, -6.0, X__, ALU.max, ALU.mult, [lk__, xk__], [lk__])
                TT("dve", Ub[:, jf__, 0:w__], L__, GB[:, c0__:c0__ + w__], ALU.mult, [lk__, "GB"], [("mU", jf__)])

            def mlp2(ci_):
                c0, w_ = cks[ci_]
                for dc in range(8):
                    bk = ("ps", 4 + dc % 2)
                    MM(PSb[:, 4 + dc % 2, 0:w_], [(W2[:, jf, dc * 128:(dc + 1) * 128], Ub[:, jf, 0:w_]) for jf in range(8)], w2k + [("mU", jf) for jf in range(8)], [bk])
                    TT("dve", ACC[:, dc, c0:c0 + w_], ACC[:, dc, c0:c0 + w_], PSb[:, 4 + dc % 2, 0:w_], ALU.add, [bk, ("ACC", dc, c0)], [("ACC", dc, c0)])

            def next_expert(e_):
                return e_ + 1 if e_ + 1 < NE else (0 if sci + 1 < len(sclist) else None)
            for (e_, ci_, jf) in units:
                c0, w_ = cks[ci_]
                if ci_ == 0 and jf == 0:
                    cur = ei % 2; ei += 1
                    W1 = W1s[cur]; w1k = [(f"W1{cur}", k) for k in range(8)]
                    nx = next_expert(e_)
                    if nx is not None:
                        load_w1(nx, 1 - cur)
                bg = jf % 2; bl = 2 + jf % 2; wb = wi % 2; wi += 1
                MM(PSb[:, bg, 0:w_], [(W1[:, k, jf * 128:(jf + 1) * 128], H2T[:, k, c0:c0 + w_]) for k in range(8)], w1k + ["H2T"], [("ps", bg)])
                MM(PSb[:, bl, 0:w_], [(W1[:, k, 1024 + jf * 128:1024 + (jf + 1) * 128], H2T[:, k, c0:c0 + w_]) for k in range(8)], w1k + ["H2T"], [("ps", bl)])
                G_ = Gt[wb][:, 0:w_]; S_ = St[wb][:, 0:w_]; L_ = Lt[wb][:, 0:w_]; X_ = Xt[wb][:, 0:w_]
                gk_ = ("mW", "G", wb); sk_ = ("mW", "S", wb); lk_ = ("mW", "L", wb); xk_ = ("mW", "X", wb)
                TS("dve", G_, PSb[:, bg, 0:w_], B1[:, e_, jf:jf + 1], 7.0, ALU.add, ALU.min, [("ps", bg), "B1"], [gk_])
                ACT(S_, G_, AF.Sigmoid, [gk_], [sk_], scale=1.702)
                TS("dve", L_, PSb[:, bl, 0:w_], B1[:, e_, 8 + jf:9 + jf], 8.0, ALU.add, ALU.min, [("ps", bl), "B1"], [lk_])
                TT("pool", X_, G_, S_, ALU.mult, [gk_, sk_], [xk_])
                if pend is not None:
                    tail(pend)
                if jf == 0 and pend2 is not None:
                    pe_, pc_ = pend2
                    mlp2(pc_)
                    if pc_ == len(cks) - 1:
                        load_w2(e_)
                    pend2 = None
                if ci_ == 0 and jf == 0:
                    P.dma(GB[:, 0:ntk], GATD[e_, tk0:tk0 + ntk].partition_broadcast(128), reads=["GATD"], writes=["GB"])
                pend = (L_, X_, lk_, xk_, jf, c0, w_)
                if jf == 7:
                    pend2 = (e_, ci_)
            tail(pend)
            mlp2(pend2[1])
            nx = next_expert(NE - 1)
            if nx is not None:
                load_w2(nx)
            acck = [("ACC", dc, c0) for dc in range(8) for (c0, _) in cks]
            for tt_ in range(nts):
                t = sc0 + tt_; v = 0 if t < NLT else 1
                eb = tt_ % 2
                ebk = [("mW", r_, eb) for r_ in "GSLX"]
                ebuf = mW[:, 4 * eb:4 * eb + 4, :].rearrange("p a n -> p (a n)")
                xt = ebuf[:, 0:D]; y_ = ebuf[:, D:2 * D]
                P.dma(xt, X1[t * 128:(t + 1) * 128, :], reads=[("X1", t)], writes=ebk)
                pf = PSb[:, 6:8, :].rearrange("p b n -> p (b n)").rearrange("p (k c) -> p k c", k=8)
                TR([(pf[:, k, :], ACC[:, k, tt_ * 128:(tt_ + 1) * 128]) for k in range(8)], identf, acck + ["identf"], [("ps", 6), ("ps", 7)])
                TT("dve", y_, PSb[:, 6:8, :].rearrange("p b n -> p (b n)"), G2[:, v, :], ALU.mult, [("ps", 6), ("ps", 7), ("G2", v)], ebk)
                TT("pool", xt, xt, y_, ALU.add, ebk, ebk)
                if not last:
                    P.dma(XR[t * 128:(t + 1) * 128, :], xt, reads=ebk, writes=[("XR", t)], eng="pool")
                else:
                    sq = GB[:, 0:1]; rs = GB[:, 1:2]
                    ACT(y_, xt, AF.Square, ebk, ebk + ["GB"], accum_out=sq)
                    rstd_from_ssq(sq, rs, 1.0 / D, ["GB"], ["GB"])
                    STT("dve", xt, xt, rs, NFb, ALU.mult, ALU.mult, ebk + ["GB", "NFb"], ebk)
                    P.dma(out[t * 128:(t + 1) * 128, :], xt, reads=ebk, writes=[("out", t)], eng="pool")
        A.free("B2f", "B1", "G2", "NFb", "W10", "W11", "W2", "ACC", "H2T", "GB", "GTs", "mW", "mU")
        if done(f"F{l}"): break

    P.emit()
    P.close()
    es.close()
    return nc, stages, ins


def make_inputs(inputs, b, names=None):
    global _TB
    if _TB is None:
        _TB = _tables()
    f = lambda a: np.ascontiguousarray(a, dtype=np.float32)
    d = {}
    lay = {
        "x": lambda: f(inputs["x"][b]),
        "ctx": lambda: f(inputs["ctx"][b]),
        "c2T": lambda: f(np.stack([inputs["c"][b], inputs["c_ctx"]], 0).reshape(2, 8, 128).transpose(2, 0, 1)),
        "b_inT": lambda: f(inputs["b_in"][:, :768].reshape(2, 6, 128).transpose(2, 0, 1)),
        "convw": lambda: f(inputs["hy_conv_w"].reshape(2, 3, 6, 128).transpose(3, 0, 1, 2)),
        "convb": lambda: f(inputs["hy_conv_b"].reshape(2, 6, 128).transpose(2, 0, 1)),
        "hyw1": lambda: f(inputs["hy_filt_w1"]), "hyw2": lambda: f(inputs["hy_filt_w2"]), "hyw3": lambda: f(inputs["hy_filt_w3"]),
        "hyvec": lambda: f(np.stack([inputs["hy_filt_b1"], inputs["hy_filt_b2"], inputs["hy_filt_b3"], inputs["hy_filt_freq"]], -1).transpose(1, 0, 2)),
        "hyout": lambda: f(inputs["hy_filt_out"]),
        "hyskip": lambda: f(inputs["hy_skip"].reshape(2, 2, 128).transpose(2, 0, 1)),
        "qkg": lambda: f(np.concatenate([np.tile(inputs["ga_q_norm"], (1, 6)), np.tile(inputs["ga_k_norm"], (1, 2))], 1)),
        "sink": lambda: f(inputs["wa_sink"]),
        "bnorm": lambda: f(inputs["branch_norm"]),
        "bnT": lambda: f(inputs["branch_norm"][:, :256].reshape(2, 2, 128).transpose(2, 0, 1)),
        "b1T": lambda: f(inputs["b_mlp1"].reshape(2, NE, 16, 128).transpose(3, 0, 1, 2)),
    }
    for k in ["w_ada", "b_ada", "norm_mix", "norm_ffn", "w_in", "b_in", "w_out", "b_out", "w_router", "b_router",
              "w_mlp1", "w_mlp2", "b_mlp2", "norm_final"]:
        lay[k] = (lambda k=k: f(inputs[k]))
    for k in (names if names is not None else list(lay.keys()) + list(_TB.keys())):
        d[k] = _TB[k] if k in _TB else lay[k]()
    return d


_SHARED = {}


def kernel(**inputs):
    nc, _, ins = build()
    names = list(ins.keys())
    shared = {}
    in_maps = []
    for b in range(8):
        m = make_inputs(inputs, b, [n for n in names if n in ("x", "ctx", "c2T")])
        if not shared:
            shared = make_inputs(inputs, 0, [n for n in names if n not in ("x", "ctx", "c2T")])
        m.update(shared)
        in_maps.append(m)
    res = run_bass_kernel_spmd(nc, in_maps, core_ids=list(range(8)))
    return np.stack([np.asarray(r["out"], dtype=np.float32) for r in res.results], 0)
```

```python
import math
import numpy as np
import ml_dtypes
import concourse.bass as bass
import concourse.mybir as mybir
from concourse.bass_utils import run_bass_kernel_spmd

F32 = mybir.dt.float32
BF16 = mybir.dt.bfloat16
ALU = mybir.AluOpType
AF = mybir.ActivationFunctionType
AX = mybir.AxisListType

SEM_LIMIT = 30000
DMA_POOL = 6

D = 1024
NLAT = 4096
NCTX = 256
T = NLAT + NCTX
NT = T // 128
NLT = NLAT // 128
EPS = 1e-6
NE = 32


class Op:
    __slots__ = ("eng", "fn", "sem", "val", "deps", "is_dma", "pre")

    def __init__(self, eng, fn, is_dma):
        self.eng = eng
        self.fn = fn
        self.is_dma = is_dma
        self.sem = None
        self.val = 0
        self.deps = []
        self.pre = None


class Prog:
    ENGS = ("pe", "act", "dve", "pool", "sp")

    def __init__(self, nc):
        self.nc = nc
        self.ops = []
        self.last_w = {}
        self.readers = {}
        self.cnt = {e: 0 for e in self.ENGS}
        self.cur_sem = {e: None for e in self.ENGS}
        self.dma_cnt = {e: 0 for e in self.ENGS}
        self.dma_sems = {e: [] for e in self.ENGS}
        self.dma_hist = {e: [] for e in self.ENGS}
        self._semctx = []
        self.bykey = {}

    def _new_sem(self, name):
        ctx = self.nc.semaphore(name)
        s = ctx.__enter__()
        self._semctx.append(ctx)
        return s

    @staticmethod
    def base(k):
        return k[0] if isinstance(k, tuple) else k

    def hazards_of(self, basename):
        ops = []
        for k in self.bykey.get(basename, ()):
            w = self.last_w.get(k)
            if w is not None:
                ops.append(w)
            ops.extend(self.readers.get(k, ()))
        return ops

    def inherit(self, newbase, ops):
        self.readers.setdefault(("#inh", newbase), []).extend(ops)

    def op(self, eng, fn, reads=(), writes=(), dma=False):
        o = Op(eng, fn, dma)
        deps = set()
        for r in reads:
            w = self.last_w.get(r)
            if w is not None:
                deps.add(w)
        for w in writes:
            lw = self.last_w.get(w)
            if lw is not None:
                deps.add(lw)
            for rd in self.readers.get(w, ()):
                deps.add(rd)
            inh = self.readers.get(("#inh", self.base(w)))
            if inh:
                deps.update(inh)
        for w in writes:
            self.last_w[w] = o
            self.readers[w] = []
            self.bykey.setdefault(self.base(w), set()).add(w)
        for r in reads:
            if r in writes:
                continue
            self.readers.setdefault(r, []).append(o)
            self.bykey.setdefault(self.base(r), set()).add(r)
        deps.discard(o)
        o.deps = list(deps)
        if dma:
            j = self.dma_cnt[eng]
            self.dma_cnt[eng] += 1
            if j < DMA_POOL:
                self.dma_sems[eng].append(self._new_sem(f"d_{eng}_{j}"))
            o.sem = self.dma_sems[eng][j % DMA_POOL]
            o.val = 16 * (j // DMA_POOL + 1)
            if j >= DMA_POOL:
                o.pre = self.dma_hist[eng][j - DMA_POOL]
            self.dma_hist[eng].append(o)
        else:
            if self.cur_sem[eng] is None or self.cnt[eng] >= SEM_LIMIT:
                self.cur_sem[eng] = self._new_sem(f"c_{eng}_{len(self._semctx)}")
                self.cnt[eng] = 0
            self.cnt[eng] += 1
            o.sem = self.cur_sem[eng]
            o.val = self.cnt[eng]
        self.ops.append(o)
        return o

    def dma(self, out, in_, reads=(), writes=(), eng="sp", **kw):
        return self.op(eng, lambda e: e.dma_start(out=out, in_=in_, **kw), reads, writes, dma=True)

    def emit(self):
        nc = self.nc
        per = {e: [] for e in self.ENGS}
        for o in self.ops:
            per[o.eng].append(o)
        engmap = {"pe": "tensor", "act": "scalar", "dve": "vector", "pool": "gpsimd", "sp": "sync"}

        def run(eng_name):
            def body(e):
                seen = {}
                for o in per[eng_name]:
                    waits = {}
                    dl = list(o.deps)
                    if o.pre is not None:
                        dl.append(o.pre)
                    for d in dl:
                        if d.eng == eng_name and eng_name == "pe" and not d.is_dma:
                            continue
                        k = id(d.sem)
                        if seen.get(k, 0) >= d.val:
                            continue
                        if k not in waits or waits[k][1] < d.val:
                            waits[k] = (d.sem, d.val)
                    for k, (s, v) in waits.items():
                        e.wait_ge(s, v)
                        seen[k] = v
                    ins = o.fn(e)
                    ins.then_inc(o.sem, 16 if o.is_dma else 1)
                tail = {}
                for o in per[eng_name]:
                    if o.is_dma:
                        k = id(o.sem)
                        if k not in tail or tail[k][1] < o.val:
                            tail[k] = (o.sem, o.val)
                for k, (s, v) in tail.items():
                    if seen.get(k, 0) < v:
                        e.wait_ge(s, v)
            return body

        with nc.Block() as block:
            for en in self.ENGS:
                if per[en]:
                    getattr(block, engmap[en])(run(en))

    def close(self):
        for c in reversed(self._semctx):
            c.__exit__(None, None, None)
        self._semctx = []


class Arena:
    def __init__(self, P, tensor, nwords):
        self.P = P
        self.t = tensor
        self.n = nwords
        self.live = {}
        self.dead = []

    def alloc(self, name, shape, dtype=F32):
        per = 1
        for s in shape[1:]:
            per *= s
        words = per if dtype == F32 else (per + 1) // 2
        segs = sorted(self.live.values())
        lo = 0
        pos = None
        for (a, b) in segs + [(self.n, self.n)]:
            if a - lo >= words:
                pos = lo
                break
            lo = max(lo, b)
        if pos is None:
            raise RuntimeError(f"arena full allocating {name} {words} words; live={self.live}")
        hi = pos + words
        self.live[name] = (pos, hi)
        ops = []
        nd = []
        for (a, b, o) in self.dead:
            if a < hi and pos < b:
                ops.extend(o)
            nd.append((a, b, o))
        self.dead = nd
        if ops:
            self.P.inherit(name, ops)
        v = self.t[:shape[0], pos:hi]
        if dtype != F32:
            v = v.bitcast(dtype)[:, 0:per]
        if len(shape) > 2:
            names = " ".join(f"d{i}" for i in range(len(shape) - 1))
            kw = {f"d{i}": shape[i + 1] for i in range(len(shape) - 1)}
            v = v.rearrange(f"p ({names}) -> p {names}", **kw)
        return v

    def free(self, *names):
        for name in names:
            lo, hi = self.live.pop(name)
            self.dead.append((lo, hi, self.P.hazards_of(name)))


def _tables():
    tb = {}
    rows = NLAT // 64
    row = np.repeat(np.arange(rows, dtype=np.float32), 64)
    col = np.tile(np.arange(64, dtype=np.float32), rows)
    pos = np.stack([row, col], -1)
    inv = (10000.0 ** (-np.arange(16, dtype=np.float32) / 16)).astype(np.float32)
    ang = pos[:, :, None] * inv
    cos = np.ones((T, 2, 16), np.float32)
    sin = np.zeros((T, 2, 16), np.float32)
    cos[:NLAT] = np.cos(ang)
    sin[:NLAT] = np.sin(ang)
    tb["ropeC"] = np.ascontiguousarray(cos.reshape(NT, 128, 32).transpose(1, 0, 2))
    tb["ropeS"] = np.ascontiguousarray(sin.reshape(NT, 128, 32).transpose(1, 0, 2))
    tb["ident"] = np.eye(128, dtype=np.float32)
    a = np.arange(128)
    m = np.zeros((128, 2, 128), np.float32)
    m[:, 0, :] = (a[None, :] <= a[:, None])
    m[:, 1, :] = (a[:, None] <= a[None, :])
    tb["wmask"] = m.astype(ml_dtypes.bfloat16)
    for n, tag in ((NLAT, "L"), (NCTX, "C")):
        nch = n // 128
        t = np.linspace(0.0, 1.0, n, dtype=np.float32)[:, None]
        wpos = (2.0 * math.pi / n) * np.arange(n, dtype=np.float32)[:, None]
        bands = np.linspace(1e-4, 15, 16, dtype=np.float32)
        z = np.concatenate([t, np.cos(wpos * bands), -np.sin(wpos * bands)], -1).astype(np.float32)
        zz = np.concatenate([z, z[::-1]], 0)
        tb["hyz" + tag] = np.ascontiguousarray(zz.T)
        deltas = np.linspace(math.log(1e-2) / 1.5, math.log(1e-2) / 0.3, 256, dtype=np.float32)
        decay = np.exp(-t * np.abs(deltas)).astype(np.float32)
        dd = np.stack([decay, decay[::-1]], 1)
        tb["hydec" + tag] = np.ascontiguousarray(dd.reshape(nch, 128, 512).transpose(1, 0, 2))
        N2 = 2 * n
        s = np.arange(n, dtype=np.int64)
        ph = (np.outer(s, s) % N2).astype(np.float64) * (2 * math.pi / N2)
        Fc = np.cos(ph)
        Fs = -np.sin(ph)
        Gs = Fs.copy()
        Fs[:, 0] = (-1.0) ** s
        Gs[0, :] = (-1.0) ** s
        def fwd_layout(M):
            return np.ascontiguousarray(M.reshape(nch, 128, nch, 128).transpose(2, 1, 0, 3).reshape(nch, 128, nch * 128)).astype(ml_dtypes.bfloat16)
        TC = 256
        ntc = n // TC
        def inv_layout(M):
            return np.ascontiguousarray(M.reshape(nch, 128, ntc, TC).transpose(2, 1, 0, 3).reshape(ntc, 128, nch * TC)).astype(ml_dtypes.bfloat16)
        tb["Fc" + tag] = fwd_layout(Fc)
        tb["Fs" + tag] = fwd_layout(Fs)
        tb["Gc" + tag] = inv_layout(Fc)
        tb["Gs" + tag] = inv_layout(Gs)
        sc = np.full((128, nch), 2.0 / N2, np.float32)
        sc[0, 0] = 1.0 / N2
        sg = np.where(np.arange(128) % 2 == 0, 1.0, -1.0).astype(np.float32)[:, None]
        tb["hysc" + tag] = np.ascontiguousarray(np.concatenate([sc, sg], 1))
    return tb


_TB = None


def build(nlayers=2, stop=None, dbg=()):
    nc = bass.Bass("TRN2", target_bir_lowering=False)
    ins = {}

    class Lazy:
        def __init__(self, name, shape, dt=F32):
            self.name, self.shape, self.dt, self.ap = name, list(shape), dt, None

        def __call__(self):
            if self.ap is None:
                self.ap = nc.dram_tensor(self.name, self.shape, self.dt, kind="ExternalInput").ap()
                ins[self.name] = self.ap
            return self.ap

    def din(name, shape, dt=F32):
        return Lazy(name, shape, dt)

    def dscr(name, shape, dt=F32):
        kind = "ExternalOutput" if name in dbg else "Internal"
        return nc.dram_tensor(name, list(shape), dt, kind=kind).ap()

    x_in = din("x", [NLAT, D]); ctx_in = din("ctx", [NCTX, D]); c2T = din("c2T", [128, 2, 8])
    w_ada = din("w_ada", [2, D, 6 * D]); b_ada = din("b_ada", [2, 6 * D])
    norm_mix = din("norm_mix", [2, D]); norm_ffn = din("norm_ffn", [2, D])
    w_in = din("w_in", [2, D, 2048]); b_in = din("b_in", [2, 2048]); b_inT = din("b_inT", [128, 2, 6])
    convw = din("convw", [128, 2, 3, 6]); convb = din("convb", [128, 2, 6])
    hyw1 = din("hyw1", [2, 33, 64]); hyw2 = din("hyw2", [2, 64, 64]); hyw3 = din("hyw3", [2, 64, 64])
    hyvec = din("hyvec", [64, 2, 4]); hyout = din("hyout", [2, 64, 512]); hyskip = din("hyskip", [128, 2, 2])
    qkg = din("qkg", [2, 512]); sink = din("sink", [2, 6]); bnorm = din("bnorm", [2, D]); bnT = din("bnT", [128, 2, 2])
    w_out = din("w_out", [2, D, D]); b_out = din("b_out", [2, D])
    w_router = din("w_router", [2, D, NE]); b_router = din("b_router", [2, NE])
    w_mlp1 = din("w_mlp1", [2, NE, D, 2 * D]); b1T = din("b1T", [128, 2, NE, 16])
    w_mlp2 = din("w_mlp2", [2, NE, D, D]); b_mlp2 = din("b_mlp2", [2, NE, D])
    norm_final = din("norm_final", [D])
    ropeC = din("ropeC", [128, NT, 32]); ropeS = din("ropeS", [128, NT, 32])
    ident_in = din("ident", [128, 128]); wmask_in = din("wmask", [128, 2, 128], BF16)
    HN = {"L": NLAT, "C": NCTX}
    hyz = {g: din("hyz" + g, [33, 2 * HN[g]]) for g in "LC"}
    hydec = {g: din("hydec" + g, [128, HN[g] // 128, 512]) for g in "LC"}
    hysc = {g: din("hysc" + g, [128, HN[g] // 128 + 1]) for g in "LC"}
    Fc = {g: din("Fc" + g, [HN[g] // 128, 128, HN[g]], BF16) for g in "LC"}
    Fs = {g: din("Fs" + g, [HN[g] // 128, 128, HN[g]], BF16) for g in "LC"}
    Gc = {g: din("Gc" + g, [HN[g] // 256, 128, HN[g] * 2], BF16) for g in "LC"}
    Gs = {g: din("Gs" + g, [HN[g] // 256, 128, HN[g] * 2], BF16) for g in "LC"}

    out = nc.dram_tensor("out", [NLAT, D], F32, kind="ExternalOutput").ap()
    MODD = dscr("MODD", [2, 6, 128, D])
    XR = dscr("XR", [T, D]); X1 = dscr("X1", [T, D])
    QKD = dscr("QKD", [10, 128, T], BF16)
    VD = dscr("VD", [NT, 128, 260], BF16)
    YNT = dscr("YNT", [8, 128, T], BF16)
    GATD = dscr("GATD", [NE, T])
    HYD = dscr("HYD", [3, 128, 2 * T], BF16)
    DBG = {k: None for k in ("DBGH", "DBGU", "DBGK", "DBGY", "DBGM")}
    if "DBGH" in dbg: DBG["DBGH"] = dscr("DBGH", [T, D], BF16)
    if "DBGK" in dbg: DBG["DBGK"] = dscr("DBGK", [NLAT, 512], BF16)
    if "DBGM" in dbg: DBG["DBGM"] = dscr("DBGM", [D, T])

    from contextlib import ExitStack
    es = ExitStack()
    SBW = 49152 - 1024
    Abig = es.enter_context(nc.sbuf_tensor("arena", [128, SBW], F32))
    PSb = es.enter_context(nc.psum_tensor("psum", [128, 8, 512], F32))
    P = Prog(nc)
    A = Arena(P, Abig, SBW)

    def psbf(bank, nb=1):
        return PSb[:, bank:bank + nb, :].rearrange("p b n -> p (b n)").bitcast(BF16)

    def TT(eng, out_, a, b, op, r, w):
        P.op(eng, lambda e: e.tensor_tensor(out=out_, in0=a, in1=b, op=op), r, w)

    def TS(eng, out_, a, s1, s2, op0, op1, r, w):
        if s2 is None:
            P.op(eng, lambda e: e.tensor_scalar(out=out_, in0=a, scalar1=s1, scalar2=None, op0=op0), r, w)
        else:
            P.op(eng, lambda e: e.tensor_scalar(out=out_, in0=a, scalar1=s1, scalar2=s2, op0=op0, op1=op1), r, w)

    def STT(eng, out_, a, s, b, op0, op1, r, w):
        P.op(eng, lambda e: e.scalar_tensor_tensor(out=out_, in0=a, scalar=s, in1=b, op0=op0, op1=op1), r, w)

    def ACT(out_, a, func, r, w, **kw):
        P.op("act", lambda e: e.activation(out=out_, in_=a, func=func, **kw), r, w)

    def CP(eng, out_, a, r, w):
        if eng == "act":
            P.op("act", lambda e: e.activation(out=out_, in_=a, func=AF.Copy), r, w)
        else:
            P.op(eng, lambda e: e.tensor_copy(out=out_, in_=a), r, w)

    def MM(out_, pairs, r, w):
        def f(e):
            n = len(pairs)
            for i, (l_, rh) in enumerate(pairs):
                ins_ = e.matmul(out_, lhsT=l_, rhs=rh, start=(i == 0), stop=(i == n - 1))
            return ins_
        P.op("pe", f, r, w)

    def MM1(out_, l_, rh, start, stop, r, w):
        P.op("pe", lambda e: e.matmul(out_, lhsT=l_, rhs=rh, start=start, stop=stop), r, w)

    def TR(outs_ins, ident, r, w):
        def f(e):
            for (o_, i_) in outs_ins:
                ins_ = e.transpose(out=o_, in_=i_, identity=ident)
            return ins_
        P.op("pe", f, r, w)

    def MS(eng, ap, val, w):
        P.op(eng, lambda e: e.memset(ap, val), (), w)

    def RECIP(ap, k):
        P.op("dve", lambda e: e.reciprocal(out=ap, in_=ap), k, k)

    def rstd_from_ssq(ssq, rs, scale, keyr, keyw):
        ACT(rs, ssq, AF.Sqrt, keyr, keyw, scale=scale, bias=EPS)
        RECIP(rs, keyw)

    identf = A.alloc("identf", [128, 128]); identb = A.alloc("identb", [128, 128], BF16)
    P.dma(identf, ident_in(), writes=["identf"])
    CP("dve", identb, identf, ["identf"], ["identb"])
    onesf = A.alloc("onesf", [128, 128])
    MS("pool", onesf, 1.0, ["onesf"])
    sil = A.alloc("sil", [128, 2, 8])
    P.dma(sil, c2T(), writes=["sil"])
    ACT(sil, sil, AF.Silu, ["sil"], ["sil"])

    stages = []

    def done(name):
        stages.append(name)
        return stop == name

    def modd_load(dst, v, j, key):
        P.dma(dst, MODD[v, j], reads=[("MODD", v, j, 0), ("MODD", v, j, 1)], writes=[key])

    def xsrc(l, t):
        if l == 0:
            return (x_in()[t * 128:(t + 1) * 128, :] if t < NLT else ctx_in()[(t - NLT) * 128:(t - NLT + 1) * 128, :]), []
        return XR[t * 128:(t + 1) * 128, :], [("XR", t)]

    def hyena(l, tag, tok0, X0T, ZT, ZTM):
        n = HN[tag]; nch = n // 128; TC = 256; ntc = n // TC
        KF = A.alloc("KF", [128, nch, 512], BF16)
        w1 = A.alloc("hw1", [33, 64]); w2 = A.alloc("hw2", [64, 64]); w3 = A.alloc("hw3", [64, 64])
        wo = A.alloc("hwo", [64, 512]); hv = A.alloc("hv", [64, 8])
        P.dma(w1, hyw1()[l], writes=["hw1"]); P.dma(w2, hyw2()[l], writes=["hw2"]); P.dma(w3, hyw3()[l], writes=["hw3"])
        P.dma(wo, hyout()[l], writes=["hwo"]); P.dma(hv[:, 0:4], hyvec()[:, l, :], writes=["hv"])
        for j in range(3):
            TT("dve", hv[:, 4 + j:5 + j], hv[:, j:j + 1], hv[:, 3:4], ALU.mult, ["hv"], ["hv"])
        CW = min(512, n)
        hz = [A.alloc(f"hz{i}", [33, CW]) for i in range(2)]
        hh = [A.alloc(f"hh{i}", [64, CW]) for i in range(3)]
        hm = A.alloc("hm", [64, CW])
        dec = [A.alloc(f"dec{i}", [128, CW // 128, 256]) for i in range(2)]
        ci = 0
        for half in range(2):
            for c0 in range(0, n, CW):
                zb = hz[ci % 2]; zk = f"hz{ci % 2}"
                P.dma(zb, hyz[tag]()[:, half * n + c0: half * n + c0 + CW], writes=[zk])
                db = dec[ci % 2]; dk = f"dec{ci % 2}"
                P.dma(db, hydec[tag]()[:, c0 // 128:(c0 + CW) // 128, half * 256:(half + 1) * 256], writes=[dk])
                src = zb; srck = zk; ws = [w1, w2, w3]; wks = ["hw1", "hw2", "hw3"]
                for j in range(3):
                    bk = ("ps", 4 + (j % 2))
                    pso = PSb[0:64, 4 + (j % 2), 0:CW]
                    MM(pso, [(ws[j], src)], [wks[j], srck], [bk])
                    hk = f"hh{j}"
                    TS("dve", hh[j], pso, hv[:, 3:4], hv[:, 4 + j:5 + j], ALU.mult, ALU.add, [bk, "hv"], [hk])
                    TS("pool", hm, hh[j], math.pi, 2.0 * math.pi, ALU.is_gt, ALU.mult, [hk], ["hm"])
                    TT("dve", hh[j], hh[j], hm, ALU.subtract, [hk, "hm"], [hk])
                    TS("pool", hm, hh[j], -math.pi, 2.0 * math.pi, ALU.is_lt, ALU.mult, [hk], ["hm"])
                    TT("dve", hh[j], hh[j], hm, ALU.add, [hk, "hm"], [hk])
                    ACT(hh[j], hh[j], AF.Sin, [hk], [hk])
                    src = hh[j]; srck = hk
                for tt_ in range(CW // 128):
                    bk = ("ps", 6 + (tt_ % 2))
                    pso = PSb[:, 6 + (tt_ % 2), 0:256]
                    MM(pso, [(hh[2][:, tt_ * 128:(tt_ + 1) * 128], wo[:, half * 256:(half + 1) * 256])], ["hh2", "hwo"], [bk])
                    TT("dve", KF[:, c0 // 128 + tt_, half * 256:(half + 1) * 256], pso, db[:, tt_, :], ALU.mult, [bk, dk], [("KF", c0 // 128 + tt_, half)])
                ci += 1
        A.free("hw1", "hw2", "hw3", "hwo", "hv", "hz0", "hz1", "hh0", "hh1", "hh2", "hm", "dec0", "dec1")
        if DBG["DBGK"] is not None and tag == "L" and l == 0:
            P.dma(DBG["DBGK"].rearrange("(c p) n -> p c n", p=128), KF, reads=[("KF", c, h) for c in range(nch) for h in range(2)], eng="pool")
        YRE = A.alloc("YRE", [128, nch, 256], BF16); YIM = A.alloc("YIM", [128, nch, 256], BF16)
        hs = A.alloc("hsc", [128, nch + 1])
        P.dma(hs, hysc[tag](), writes=["hsc"])
        ftab = [(A.alloc(f"fct{i}", [128, nch, 128], BF16), A.alloc(f"fst{i}", [128, nch, 128], BF16)) for i in range(2)]
        KA = A.alloc("KA", [128, 2, 512])
        KK = A.alloc("KK", [128, 4, 256])
        TMP = A.alloc("hTMP", [128, 4, 256])
        kfk = [("KF", c, h) for c in range(nch) for h in range(2)]
        ztk = [("ZTM", tok0 // 128 + c) for c in range(nch)]
        for fc in range(nch):
            fct, fst = ftab[fc % 2]; fk = f"fct{fc % 2}"; sk = f"fst{fc % 2}"
            P.dma(fct, Fc[tag]()[fc].rearrange("p (s j) -> p s j", j=128), writes=[fk])
            P.dma(fst, Fs[tag]()[fc].rearrange("p (s j) -> p s j", j=128), writes=[sk])
            MM(PSb[:, 0, :], [(fct[:, s_, :], KF[:, s_, :]) for s_ in range(nch)], [fk] + kfk, [("ps", 0)])
            MM(PSb[:, 1, 0:256], [(fct[:, s_, :], ZTM[:, tok0 // 128 + s_, :]) for s_ in range(nch)], [fk] + ztk, [("ps", 1)])
            MM(PSb[:, 2, :], [(fst[:, s_, :], KF[:, s_, :]) for s_ in range(nch)], [sk] + kfk, [("ps", 2)])
            MM(PSb[:, 3, 0:256], [(fst[:, s_, :], ZTM[:, tok0 // 128 + s_, :]) for s_ in range(nch)], [sk] + ztk, [("ps", 3)])
            CP("act", KA[:, 0, :], PSb[:, 0, :], [("ps", 0)], [("KA", 0)])
            CP("act", KA[:, 1, :], PSb[:, 2, :], [("ps", 2)], [("KA", 1)])
            sg = hs[:, nch:nch + 1]; scl = hs[:, fc:fc + 1]
            for ri in range(2):
                dst = KK[:, 0 if ri == 0 else 1, :]
                STT("dve", dst, KA[:, ri, 256:512], sg, KA[:, ri, 0:256], ALU.mult, ALU.add, [("KA", ri), "hsc"], [("KK", ri)])
                TS("pool", dst, dst, scl, None, ALU.mult, None, [("KK", ri), "hsc"], [("KK", ri)])
            CP("pool", KK[:, 2, :], KK[:, 1, :], [("KK", 1)], [("KK", 2)])
            CP("pool", KK[:, 3, :], KK[:, 0, :], [("KK", 0)], [("KK", 3)])
            if fc == 0:
                CP("pool", KK[0:1, 3, :], KK[0:1, 1, :], [("KK", 1), ("KK", 3)], [("KK", 3)])
                MS("pool", KK[0:1, 2, :], 0.0, [("KK", 2)])
                P.op("pool", lambda e: e.memset(KK[0:1, 1, :], 0.0), [("KK", 2), ("KK", 3)], [("KK", 1)])
            zre = PSb[:, 1, 0:256]; zim = PSb[:, 3, 0:256]
            TT("dve", TMP[:, 0, :], zre, KK[:, 0, :], ALU.mult, [("ps", 1), ("KK", 0)], [("hTMP", 0)])
            TT("dve", TMP[:, 1, :], zim, KK[:, 1, :], ALU.mult, [("ps", 3), ("KK", 1)], [("hTMP", 1)])
            TT("dve", TMP[:, 2, :], zre, KK[:, 2, :], ALU.mult, [("ps", 1), ("KK", 2)], [("hTMP", 2)])
            TT("dve", TMP[:, 3, :], zim, KK[:, 3, :], ALU.mult, [("ps", 3), ("KK", 3)], [("hTMP", 3)])
            TT("pool", YRE[:, fc, :], TMP[:, 0, :], TMP[:, 1, :], ALU.subtract, [("hTMP", 0), ("hTMP", 1)], [("YRE", fc)])
            TT("pool", YIM[:, fc, :], TMP[:, 2, :], TMP[:, 3, :], ALU.add, [("hTMP", 2), ("hTMP", 3)], [("YIM", fc)])
        A.free("KF", "hsc", "fct0", "fst0", "fct1", "fst1", "KA", "KK", "hTMP")
        gtab = [(A.alloc(f"gct{i}", [128, nch, TC], BF16), A.alloc(f"gst{i}", [128, nch, TC], BF16)) for i in range(2)]
        YH = A.alloc("YH", [128, 2, TC]); SQ = A.alloc("hSQ", [128, 2, TC]); RS = A.alloc("hRS", [128, TC])
        YO = A.alloc("hYO", [128, 2, TC], BF16)
        sk2 = A.alloc("hskip", [128, 4])
        P.dma(sk2[:, 0:2], hyskip()[:, l, :], writes=["hskip"])
        P.dma(sk2[:, 2:4], bnT()[:, l, :], writes=["hskip"])
        yk = [("YRE", c) for c in range(nch)] + [("YIM", c) for c in range(nch)]
        for tc in range(ntc):
            gct, gst = gtab[tc % 2]; gk = f"gct{tc % 2}"; gsk = f"gst{tc % 2}"
            P.dma(gct, Gc[tag]()[tc].rearrange("p (s j) -> p s j", j=TC), writes=[gk])
            P.dma(gst, Gs[tag]()[tc].rearrange("p (s j) -> p s j", j=TC), writes=[gsk])
            tsl = slice(tok0 + tc * TC, tok0 + (tc + 1) * TC)
            for cc in range(2):
                pso = PSb[:, 4 + cc, 0:TC]
                MM(pso, [(YRE[:, s_, cc * 128:(cc + 1) * 128], gct[:, s_, :]) for s_ in range(nch)] +
                        [(YIM[:, s_, cc * 128:(cc + 1) * 128], gst[:, s_, :]) for s_ in range(nch)], yk + [gk, gsk], [("ps", 4 + cc)])
                STT("dve", YH[:, cc, :], ZT[:, cc, tsl], sk2[:, cc:cc + 1], pso, ALU.mult, ALU.add, [("ZT", cc), "hskip", ("ps", 4 + cc)], [("YH", cc)])
                TT("pool", YH[:, cc, :], YH[:, cc, :], X0T[:, cc, tsl], ALU.mult, [("YH", cc), ("X0T", cc)], [("YH", cc)])
                ACT(SQ[:, cc, :], YH[:, cc, :], AF.Square, [("YH", cc)], [("hSQ", cc)])
            MM(PSb[:, 6, 0:TC], [(onesf, SQ[:, 0, :]), (onesf, SQ[:, 1, :])], ["onesf", ("hSQ", 0), ("hSQ", 1)], [("ps", 6)])
            ACT(RS, PSb[:, 6, 0:TC], AF.Sqrt, [("ps", 6)], ["hRS"], scale=1.0 / 256, bias=EPS)
            RECIP(RS, ["hRS"])
            for cc in range(2):
                STT("dve", YO[:, cc, :], YH[:, cc, :], sk2[:, 2 + cc:3 + cc], RS, ALU.mult, ALU.mult, [("YH", cc), "hskip", "hRS"], [("hYO", cc)])
            P.dma(YNT[0:2, :, tsl].rearrange("c p n -> p c n"), YO, reads=[("hYO", 0), ("hYO", 1)], writes=[("YNT", 0, tag, tc)], eng="pool")
        A.free("YRE", "YIM", "gct0", "gst0", "gct1", "gst1", "YH", "hSQ", "hRS", "hYO", "hskip")

    def attention(l, br, last):
        qi0 = 0 if br == "ga" else 5
        vi0 = 0 if br == "ga" else 2
        QT = A.alloc("QT", [128, 3, T], BF16); KT = A.alloc("KT", [128, 2, T], BF16); VA = A.alloc("VA", [128, NT, 4, 65], BF16)
        qkr = [("QKD", g_) for g_ in range(9)]
        P.dma(QT, QKD[qi0:qi0 + 3].rearrange("i p n -> p i n"), reads=qkr, writes=["QT"])
        P.dma(KT, QKD[qi0 + 3:qi0 + 5].rearrange("i p n -> p i n"), reads=qkr, writes=["KT"])
        P.dma(VA.rearrange("p t a b -> p t (a b)"), VD.rearrange("t p c -> p t c"), reads=[("VD", t) for t in range(NT)], writes=["VA"])
        BN = A.alloc("BNb", [128, 384])
        boff = 256 if br == "ga" else 640
        P.dma(BN, bnorm()[l][boff:boff + 384].partition_broadcast(128), writes=["BNb"])
        SKE = A.alloc("SKE", [128, 6])
        if br == "wa":
            P.dma(SKE, sink()[l].partition_broadcast(128), writes=["SKE"])
            ACT(SKE, SKE, AF.Exp, ["SKE"], ["SKE"])
            WM = A.alloc("WM", [128, 2, 128], BF16)
            P.dma(WM, wmask_in(), writes=["WM"])
        Yb = [A.alloc(f"Yb{i}", [128, 4, 384]) for i in range(2)]
        Pb = [A.alloc(f"Pb{i}", [128, 512], BF16) for i in range(2)]
        YNb = A.alloc("YNb", [128, 384], BF16)
        YS = [A.alloc(f"YS{i}", [128, 3, 512], BF16) for i in range(2)]
        st = A.alloc("ast", [128, 8])
        junk = A.alloc("ajunk", [128, 384])
        den = A.alloc("aden", [128, 8])
        chunks = [(c * 4, 4) for c in range(8)] + ([] if last else [(32, 2)])
        pcnt = 0
        for ci, (t0, ntl) in enumerate(chunks):
            nq = ntl * 128
            Y = Yb[ci % 2]; ykey = f"Yb{ci % 2}"
            isctx = t0 >= NLT
            for h in range(6):
                g = h // 3; pair = h // 2; b = 64 * (h % 2)
                order = 0 if b == 64 * g else 1
                if br == "ga":
                    units = [(list(range(ntl)), ([32, 33] if isctx else list(range(NT))))]
                else:
                    units = []
                    for qs in range(ntl):
                        i = t0 + qs
                        if isctx:
                            units.append(([qs], [32, 33]))
                        else:
                            units.append(([qs], [j for j in (i - 1, i, i + 1) if 0 <= j < NLT] + [32, 33]))
                for (qsl, keys) in units:
                    q0 = (t0 + qsl[0]) * 128; nqq = len(qsl) * 128
                    pend = None

                    def pv_emit(pd):
                        ji_, j_, pb__, pk__ = pd
                        for qq, qs in enumerate(qsl):
                            MM1(PSb[:, 2 + qs, 0:65], pb__[:, qq * 128:(qq + 1) * 128], VA[:, j_, vi0 + g, :], ji_ == 0, ji_ == len(keys) - 1, [pk__, "VA"], [("ps", 2 + qs)])
                    for ji, j in enumerate(keys):
                        sb_ = pcnt % 2; pcnt += 1
                        stp = PSb[:, sb_, 0:nqq]
                        MM(stp, [(KT[b:b + 64, order, j * 128:(j + 1) * 128], QT[b:b + 64, pair, q0:q0 + nqq])], ["KT", "QT"], [("ps", sb_)])
                        pb_ = Pb[sb_][:, 0:nqq]; pk = f"Pb{sb_}"
                        ACT(pb_, stp, AF.Exp, [("ps", sb_)], [pk], scale=0.125)
                        if br == "wa" and not isctx and j < NLT and j != t0 + qsl[0]:
                            mi = 0 if j < t0 + qsl[0] else 1
                            TT("dve", pb_, pb_, WM[:, mi, :], ALU.mult, [pk, "WM"], [pk])
                        if pend is not None:
                            pv_emit(pend)
                        pend = (ji, j, pb_, pk)
                    pv_emit(pend)
                    for qs in qsl:
                        dk = ("aden", qs)
                        if br == "wa":
                            TT("dve", den[:, qs:qs + 1], PSb[:, 2 + qs, 64:65], SKE[:, h:h + 1], ALU.add, [("ps", 2 + qs), "SKE"], [dk])
                        else:
                            CP("dve", den[:, qs:qs + 1], PSb[:, 2 + qs, 64:65], [("ps", 2 + qs)], [dk])
                        RECIP(den[:, qs:qs + 1], [dk])
                        TS("dve", Y[:, qs, h * 64:(h + 1) * 64], PSb[:, 2 + qs, 0:64], den[:, qs:qs + 1], None, ALU.mult, None, [("ps", 2 + qs), dk], [(ykey, qs)])
            ys = YS[ci % 2]; ysk = f"YS{ci % 2}"
            for qs in range(ntl):
                sk_ = ("ast", qs % 2)
                sq = st[:, (qs % 2) * 2:(qs % 2) * 2 + 1]; rs = st[:, (qs % 2) * 2 + 1:(qs % 2) * 2 + 2]
                ACT(junk, Y[:, qs, :], AF.Square, [(ykey, qs)], ["ajunk", sk_], accum_out=sq)
                rstd_from_ssq(sq, rs, 1.0 / 384, [sk_], [sk_])
                STT("dve", YNb, Y[:, qs, :], rs, BN, ALU.mult, ALU.mult, [(ykey, qs), sk_, "BNb"], ["YNb"])
                pb = psbf(6 + qs % 2)
                TR([(pb[:, k * 128:(k + 1) * 128], YNb[:, k * 128:(k + 1) * 128]) for k in range(3)], identb, ["YNb", "identb"], [("ps", 6 + qs % 2)])
                CP("act", ys[:, :, qs * 128:(qs + 1) * 128], pb[:, 0:384].rearrange("p (k c) -> p k c", k=3), [("ps", 6 + qs % 2)], [(ysk, qs)])
            c0 = 2 if br == "ga" else 5
            P.dma(YNT[c0:c0 + 3, :, t0 * 128:t0 * 128 + nq].rearrange("c p n -> p c n"), ys[:, :, 0:nq], reads=[(ysk, qs) for qs in range(ntl)], writes=[("YNT", br, ci)], eng="pool")
        A.free("QT", "KT", "VA", "BNb", "SKE", "Yb0", "Yb1", "Pb0", "Pb1", "YNb", "YS0", "YS1", "ast", "ajunk", "aden")
        if br == "wa":
            A.free("WM")

    for l in range(nlayers):
        last = (l == 1)
        ntl_l = NLT if last else NT
        silB = A.alloc("silB", [128, 2, 8, 128])
        for v in range(2):
            for k in range(8):
                CP("dve", silB[:, v, k, :], sil[:, v, k:k + 1].to_broadcast([128, 128]), ["sil"], [("silB", v, k)])
        wst = [A.alloc(f"wada{i}", [128, 8, 512]) for i in range(2)]
        bb = A.alloc("badab", [128, 512]); nb = A.alloc("normb", [128, 2, D])
        P.dma(nb[:, 0, :], norm_mix()[l].partition_broadcast(128), writes=[("normb", 0)])
        P.dma(nb[:, 1, :], norm_ffn()[l].partition_broadcast(128), writes=[("normb", 1)])
        mo = [A.alloc(f"modo{i}", [128, 512]) for i in range(2)]
        for n_ in range(12):
            ws = wst[n_ % 2]; wk = f"wada{n_ % 2}"
            P.dma(ws, w_ada()[l][:, n_ * 512:(n_ + 1) * 512].rearrange("(k p) n -> p k n", p=128), writes=[wk])
            P.dma(bb, b_ada()[l][n_ * 512:(n_ + 1) * 512].partition_broadcast(128), writes=["badab"])
            j = n_ // 2; hf = n_ % 2
            for v in range(2):
                MM(PSb[:, v, :], [(silB[:, v, k, :], ws[:, k, :]) for k in range(8)], [wk] + [("silB", v, k) for k in range(8)], [("ps", v)])
                m_ = mo[v]; mk = f"modo{v}"
                TT("dve", m_, PSb[:, v, :], bb, ALU.add, [("ps", v), "badab"], [mk])
                if j in (1, 4):
                    nsel = 0 if j == 1 else 1
                    STT("dve", m_, m_, 1.0, nb[:, nsel, hf * 512:(hf + 1) * 512], ALU.add, ALU.mult, [mk, ("normb", nsel)], [mk])
                P.dma(MODD[v, j, :, hf * 512:(hf + 1) * 512], m_, reads=[mk], writes=[("MODD", v, j, hf)], eng="pool")
        A.free("wada0", "wada1", "badab", "normb", "modo0", "modo1", "silB")
        if done(f"mod{l}"): break

        HT = A.alloc("HT", [128, 8, T], BF16)
        a1 = A.alloc("a1", [128, 2, D]); s1 = A.alloc("s1", [128, 2, D])
        for v in range(2):
            modd_load(a1[:, v, :], v, 1, ("a1", v)); modd_load(s1[:, v, :], v, 0, ("s1", v))
        xts = [A.alloc(f"xt{i}", [128, D]) for i in range(2)]
        junk = A.alloc("junk", [128, D])
        hb = [A.alloc(f"hb{i}", [128, D], BF16) for i in range(2)]
        st = A.alloc("stat", [128, 4])
        for t in range(NT):
            v = 0 if t < NLT else 1
            xt = xts[t % 2]; xk = f"xt{t % 2}"
            src, rk = xsrc(l, t)
            P.dma(xt, src, reads=rk, writes=[xk])
            sq = st[:, (t % 2) * 2:(t % 2) * 2 + 1]; rs = st[:, (t % 2) * 2 + 1:(t % 2) * 2 + 2]; sk = ("stat", t % 2)
            ACT(junk, xt, AF.Square, [xk], ["junk", sk], accum_out=sq)
            rstd_from_ssq(sq, rs, 1.0 / D, [sk], [sk])
            STT("dve", xt, xt, rs, a1[:, v, :], ALU.mult, ALU.mult, [xk, sk, ("a1", v)], [xk])
            h_ = hb[t % 2]; hk = f"hb{t % 2}"
            TT("pool", h_, xt, s1[:, v, :], ALU.add, [xk, ("s1", v)], [hk])
            if DBG["DBGH"] is not None and l == 0:
                P.dma(DBG["DBGH"][t * 128:(t + 1) * 128, :], h_, reads=[hk], eng="pool")
            pb = psbf(t % 2).rearrange("p (k c) -> p k c", k=8)
            TR([(pb[:, k, :], h_[:, k * 128:(k + 1) * 128]) for k in range(8)], identb, [hk, "identb"], [("ps", t % 2)])
            CP("act", HT[:, :, t * 128:(t + 1) * 128], pb, [("ps", t % 2)], [("HT", t)])
        A.free("a1", "s1", "xt0", "xt1", "junk", "hb0", "hb1", "stat")
        if done(f"A{l}"): break

        WIN = A.alloc("WIN", [128, 8, 2048], BF16)
        for k in range(8):
            P.dma(WIN[:, k, :], w_in()[l][k * 128:(k + 1) * 128, :], writes=[("WIN", k)], eng="pool")
        htk = [("HT", t) for t in range(NT)]
        wink = [("WIN", k) for k in range(8)]

        X0T = A.alloc("X0T", [128, 2, T], BF16); ZT = A.alloc("ZT", [128, 2, T], BF16)
        bT = A.alloc("bT", [128, 6]); cw = A.alloc("cw", [128, 3, 6]); cb = A.alloc("cb", [128, 6])
        P.dma(bT, b_inT()[:, l, :], writes=["bT"]); P.dma(cw, convw()[:, l], writes=["cw"]); P.dma(cb, convb()[:, l], writes=["cb"])
        U = A.alloc("U", [128, T + 4]); UC1 = A.alloc("UC1", [128, T], BF16); UC = A.alloc("UC", [128, T])
        MS("pool", U, 0.0, ["U"])
        segs = [(0, NLAT, 1), (NLAT, NCTX, NLAT + 3)]
        for m in (0, 1, 2, 4, 3, 5):
            ck = 0
            for (tk0, n_, uo) in segs:
                for c0 in range(0, n_, 512):
                    w_ = min(512, n_ - c0); bkk = ("ps", ck % 2)
                    MM(PSb[:, ck % 2, 0:w_], [(WIN[:, k, m * 128:(m + 1) * 128], HT[:, k, tk0 + c0:tk0 + c0 + w_]) for k in range(8)], wink + htk, [bkk])
                    ACT(U[:, uo + c0:uo + c0 + w_], PSb[:, ck % 2, 0:w_], AF.Identity, [bkk, "bT"], ["U"], bias=bT[:, m:m + 1])
                    ck += 1
            dst = UC1 if m in (2, 3) else UC; dk = "UC1" if m in (2, 3) else "UC"
            for (tk0, n_, uo) in segs:
                d_ = dst[:, tk0:tk0 + n_]
                TS("dve", d_, U[:, uo:uo + n_], cw[:, 1, m:m + 1], cb[:, m:m + 1], ALU.mult, ALU.add, ["U", "cw", "cb"], [dk])
                STT("dve", d_, U[:, uo - 1:uo - 1 + n_], cw[:, 0, m:m + 1], d_, ALU.mult, ALU.add, ["U", "cw", dk], [dk])
                STT("dve", d_, U[:, uo + 1:uo + 1 + n_], cw[:, 2, m:m + 1], d_, ALU.mult, ALU.add, ["U", "cw", dk], [dk])
            if m < 2:
                CP("pool", X0T[:, m, :], UC, ["UC"], [("X0T", m)])
            elif m >= 4:
                TT("pool", ZT[:, m - 4, :], UC1, UC, ALU.mult, ["UC", "UC1"], [("ZT", m - 4)])
        A.free("bT", "cw", "cb", "U", "UC1", "UC")
        ZTM = A.alloc("ZTM", [128, NT, 256], BF16)
        for t0 in range(0, NT, 4):
            nt_ = min(4, NT - t0); bnk = (t0 // 4) % 2
            pb = psbf(bnk).rearrange("p (t c) -> p t c", t=4)
            TR([(pb[:, tt_, cc * 128:(cc + 1) * 128], ZT[:, cc, (t0 + tt_) * 128:(t0 + tt_ + 1) * 128]) for tt_ in range(nt_) for cc in range(2)], identb,
               [("ZT", 0), ("ZT", 1), "identb"], [("ps", bnk)])
            for tt_ in range(nt_):
                CP("act", ZTM[:, t0 + tt_, :], pb[:, tt_, :], [("ps", bnk)], [("ZTM", t0 + tt_)])
        for i_, (nm_, tl_) in enumerate((("X0T", X0T), ("ZT", ZT), ("ZTM", ZTM))):
            P.dma(HYD[i_], tl_.rearrange("p a b -> p (a b)"), reads=[k_ for k_ in P.bykey.get(nm_, ())], writes=[("HYD", i_)], eng="pool")
        A.free("X0T", "ZT", "ZTM")
        if done(f"B1{l}"): break

        PTs = [A.alloc(f"PT{i}", [128, 1280]) for i in range(2)]
        RQs = [A.alloc(f"RQ{i}", [128, 1536], BF16) for i in range(2)]
        SQ = A.alloc("SQ", [128, 512]); st8 = A.alloc("st8", [128, 2, 8])
        QKG = A.alloc("QKG", [128, 512]); BINB = A.alloc("BINB", [128, 1280])
        P.dma(QKG, qkg()[l].partition_broadcast(128), writes=["QKG"])
        P.dma(BINB, b_in()[l][768:2048].partition_broadcast(128), writes=["BINB"])
        RC = A.alloc("RC", [128, NT, 32]); RSn = A.alloc("RSn", [128, NT, 32])
        P.dma(RC, ropeC(), writes=["RC"]); P.dma(RSn, ropeS(), writes=["RSn"])
        VAUG = [A.alloc(f"VAUG{i}", [128, 4, 65], BF16) for i in range(2)]
        for i in range(2):
            MS("pool", VAUG[i], 1.0, [f"VAUG{i}"])
        QKT = [A.alloc(f"QKT{i}", [128, 10, 512], BF16) for i in range(2)]
        TMPS = [A.alloc(f"rt{i}", [128, 8, 2, 16]) for i in range(4)]
        tcols = [(0, 128), (128, 256), (256, 384), (384, 512), (1280, 1408), (640, 768), (768, 896), (896, 1024), (1024, 1152), (1408, 1536)]
        for t in range(NT):
            PT = PTs[t % 2]; pk = f"PT{t % 2}"; RQ = RQs[t % 2]; rqk = f"RQ{t % 2}"
            b0 = 3 * (t % 2)
            for pi, (c0, c1) in enumerate(((768, 1280), (1280, 1792), (1792, 2048))):
                MM(PSb[:, b0 + pi, 0:c1 - c0], [(HT[:, k, t * 128:(t + 1) * 128], WIN[:, k, c0:c1]) for k in range(8)], wink + [("HT", t)], [("ps", b0 + pi)])
                TT("dve", PT[:, c0 - 768:c1 - 768], PSb[:, b0 + pi, 0:c1 - c0], BINB[:, c0 - 768:c1 - 768], ALU.add, [("ps", b0 + pi), "BINB"], [(pk, pi)])
            pkall = [(pk, 0), (pk, 1), (pk, 2)]
            sk = ("st8", t % 2)
            ACT(SQ, PT[:, 0:512], AF.Square, [(pk, 0)], ["SQ"])
            P.op("dve", lambda e, o_=st8[:, t % 2, :], i_=SQ.rearrange("p (h d) -> p h d", d=64): e.tensor_reduce(out=o_, in_=i_, axis=AX.X, op=ALU.add), ["SQ"], [sk])
            ACT(st8[:, t % 2, :], st8[:, t % 2, :], AF.Sqrt, [sk], [sk], scale=1.0 / 64, bias=EPS)
            RECIP(st8[:, t % 2, :], [sk])
            pv = PT[:, 0:512].rearrange("p (h d) -> p h d", d=64)
            TT("dve", pv, pv, st8[:, t % 2, :].unsqueeze(2).to_broadcast([128, 8, 64]), ALU.mult, [(pk, 0), sk], [(pk, 0)])
            TT("dve", PT[:, 0:512], PT[:, 0:512], QKG, ALU.mult, [(pk, 0), "QKG"], [(pk, 0)])
            Cb = RC[:, t, :].rearrange("p (a f) -> p a f", a=2).unsqueeze(1).to_broadcast([128, 8, 2, 16])
            Sb = RSn[:, t, :].rearrange("p (a f) -> p a f", a=2).unsqueeze(1).to_broadcast([128, 8, 2, 16])
            for ri, base in enumerate((0, 640)):
                xv = PT[:, base:base + 512].rearrange("p (h a s f) -> p h a s f", h=8, a=2, s=2)
                ov = RQ[:, base:base + 512].rearrange("p (h a s f) -> p h a s f", h=8, a=2, s=2)
                x1 = xv[:, :, :, 0, :]; x2 = xv[:, :, :, 1, :]
                rk = pkall
                tk = [(f"rt{i}", ri) for i in range(4)]
                TT("dve", TMPS[0], x1, Cb, ALU.mult, rk + ["RC"], [tk[0]])
                TT("dve", TMPS[1], x2, Sb, ALU.mult, rk + ["RSn"], [tk[1]])
                TT("dve", ov[:, :, :, 0, :], TMPS[0], TMPS[1], ALU.subtract, [tk[0], tk[1]], [(rqk, ri, 0)])
                TT("dve", TMPS[2], x2, Cb, ALU.mult, rk + ["RC"], [tk[2]])
                TT("dve", TMPS[3], x1, Sb, ALU.mult, rk + ["RSn"], [tk[3]])
                TT("dve", ov[:, :, :, 1, :], TMPS[2], TMPS[3], ALU.add, [tk[2], tk[3]], [(rqk, ri, 1)])
            rqall = [(rqk, 0, 0), (rqk, 0, 1), (rqk, 1, 0), (rqk, 1, 1)]
            for (dst0, s0) in ((1280, 448), (1344, 384), (1408, 1088), (1472, 1024)):
                CP("act", RQ[:, dst0:dst0 + 64], RQ[:, s0:s0 + 64], rqall, [(rqk, "sw", dst0)])
            rqall = rqall + [(rqk, "sw", d_) for d_ in (1280, 1344, 1408, 1472)]
            va = VAUG[t % 2]; vk = f"VAUG{t % 2}"
            CP("act", va[:, 0:2, 0:64], PT[:, 512:640].rearrange("p (h d) -> p h d", h=2), [(pk, 1)], [vk])
            CP("act", va[:, 2:4, 0:64], PT[:, 1152:1280].rearrange("p (h d) -> p h d", h=2), [(pk, 2)], [vk])
            P.dma(VD[t], va.rearrange("p a b -> p (a b)"), reads=[vk], writes=[("VD", t)], eng="pool")
            pb = psbf(6, 2)
            TR([(pb[:, i * 128:(i + 1) * 128], RQ[:, c0:c1]) for i, (c0, c1) in enumerate(tcols)], identb, rqall + ["identb"], [("ps", 6)])
            g4 = t // 4; qt = QKT[g4 % 2]; qtk = f"QKT{g4 % 2}"
            CP("act", qt[:, :, (t % 4) * 128:(t % 4 + 1) * 128], pb[:, 0:1280].rearrange("p (i c) -> p i c", i=10), [("ps", 6)], [(qtk, t % 4)])
            if t % 4 == 3 or t == NT - 1:
                t0 = (t // 4) * 4; nn = (t - t0 + 1) * 128
                P.dma(QKD[:, :, t0 * 128:t0 * 128 + nn].rearrange("i p n -> p i n"), qt[:, :, 0:nn], reads=[(qtk, i) for i in range(t - t0 + 1)], writes=[("QKD", t // 4)], eng="pool")
        A.free("PT0", "PT1", "RQ0", "RQ1", "SQ", "st8", "QKG", "BINB", "RC", "RSn", "VAUG0", "VAUG1", "QKT0", "QKT1", "rt0", "rt1", "rt2", "rt3")
        A.free("HT", "WIN")
        if done(f"B2{l}"): break

        X0T = A.alloc("X0T", [128, 2, T], BF16); ZT = A.alloc("ZT", [128, 2, T], BF16); ZTM = A.alloc("ZTM", [128, NT, 256], BF16)
        P.dma(X0T.rearrange("p a b -> p (a b)"), HYD[0], reads=[("HYD", 0)], writes=[("X0T", 0), ("X0T", 1)])
        P.dma(ZT.rearrange("p a b -> p (a b)"), HYD[1], reads=[("HYD", 1)], writes=[("ZT", 0), ("ZT", 1)])
        P.dma(ZTM.rearrange("p a b -> p (a b)"), HYD[2], reads=[("HYD", 2)], writes=[("ZTM", t_) for t_ in range(NT)])
        hyena(l, "L", 0, X0T, ZT, ZTM)
        if not last:
            hyena(l, "C", NLAT, X0T, ZT, ZTM)
        A.free("X0T", "ZT", "ZTM")
        if done(f"C{l}"): break

        attention(l, "ga", last)
        if done(f"Dga{l}"): break
        attention(l, "wa", last)
        if done(f"D{l}"): break

        YN = A.alloc("YN", [128, 8, T], BF16)
        ynr = [("YNT", 0, "L", tc) for tc in range(NLAT // 256)] + ([] if last else [("YNT", 0, "C", 0)]) + \
              [("YNT", br, ci) for br in ("ga", "wa") for ci in range(8 if last else 9)]
        P.dma(YN, YNT.rearrange("c p n -> p c n"), reads=ynr, writes=["YN"])
        WO = A.alloc("WO", [128, 8, D], BF16)
        for k in range(8):
            P.dma(WO[:, k, :], w_out()[l][k * 128:(k + 1) * 128, :], writes=[("WO", k)], eng="pool")
        wok = [("WO", k) for k in range(8)]
        BO = A.alloc("BO", [128, D]); G1 = A.alloc("G1", [128, 2, D])
        P.dma(BO, b_out()[l].partition_broadcast(128), writes=["BO"])
        for v in range(2):
            modd_load(G1[:, v, :], v, 2, ("G1", v))
        xts = [A.alloc(f"xt{i}", [128, D]) for i in range(2)]
        tm = [A.alloc(f"tm{i}", [128, D]) for i in range(2)]
        for t in range(ntl_l):
            v = 0 if t < NLT else 1
            xt = xts[t % 2]; xk = f"xt{t % 2}"; tmp = tm[t % 2]; tk = f"tm{t % 2}"
            src, rk = xsrc(l, t)
            P.dma(xt, src, reads=rk, writes=[xk])
            for hf in range(2):
                bk = ("ps", (t % 2) * 2 + hf)
                MM(PSb[:, (t % 2) * 2 + hf, :], [(YN[:, k, t * 128:(t + 1) * 128], WO[:, k, hf * 512:(hf + 1) * 512]) for k in range(8)], ["YN"] + wok, [bk])
                TT("dve", tmp[:, hf * 512:(hf + 1) * 512], PSb[:, (t % 2) * 2 + hf, :], BO[:, hf * 512:(hf + 1) * 512], ALU.add, [bk, "BO"], [(tk, hf)])
            TT("dve", tmp, tmp, G1[:, v, :], ALU.mult, [(tk, 0), (tk, 1), ("G1", v)], [(tk, 0), (tk, 1)])
            TT("dve", xt, xt, tmp, ALU.add, [xk, (tk, 0), (tk, 1)], [xk])
            P.dma(X1[t * 128:(t + 1) * 128, :], xt, reads=[xk], writes=[("X1", t)], eng="pool")
        A.free("YN", "WO", "BO", "G1", "xt0", "xt1", "tm0", "tm1")
        if done(f"E{l}"): break

        GT = A.alloc("GT", [32, T])
        a2 = A.alloc("a2", [128, 2, D]); s2 = A.alloc("s2", [128, 2, D])
        for v in range(2):
            modd_load(a2[:, v, :], v, 4, ("a2", v)); modd_load(s2[:, v, :], v, 3, ("s2", v))
        WR = A.alloc("WR", [128, 8, NE]); BR = A.alloc("BR", [128, NE])
        P.dma(WR, w_router()[l].rearrange("(k p) e -> p k e", p=128), writes=["WR"])
        P.dma(BR, b_router()[l].partition_broadcast(128), writes=["BR"])
        H2D = dscr(f"H2D{l}", [8, 128, T], BF16)
        xts = [A.alloc(f"xt{i}", [128, D]) for i in range(2)]
        junk = A.alloc("junk", [128, D]); h2f = A.alloc("h2f", [128, 8, 128])
        h2b = [A.alloc(f"h2b{i}", [128, 8, 128], BF16) for i in range(2)]
        st = A.alloc("stat", [128, 4]); lg = A.alloc("lg", [128, NE]); m8 = A.alloc("m8", [128, 8]); msk = A.alloc("msk", [128, NE])
        gs = A.alloc("gs", [128, 4])
        for t in range(ntl_l):
            v = 0 if t < NLT else 1
            xt = xts[t % 2]; xk = f"xt{t % 2}"
            P.dma(xt, X1[t * 128:(t + 1) * 128, :], reads=[("X1", t)], writes=[xk])
            sq = st[:, (t % 2) * 2:(t % 2) * 2 + 1]; rs = st[:, (t % 2) * 2 + 1:(t % 2) * 2 + 2]; sk = ("stat", t % 2)
            ACT(junk, xt, AF.Square, [xk], ["junk", sk], accum_out=sq)
            rstd_from_ssq(sq, rs, 1.0 / D, [sk], [sk])
            STT("dve", xt, xt, rs, a2[:, v, :], ALU.mult, ALU.mult, [xk, sk, ("a2", v)], [xk])
            TT("dve", xt, xt, s2[:, v, :], ALU.add, [xk, ("s2", v)], [xk])
            pf = PSb[:, 0:2, :].rearrange("p b n -> p (b n)").rearrange("p (k c) -> p k c", k=8)
            TR([(pf[:, k, :], xt[:, k * 128:(k + 1) * 128]) for k in range(8)], identf, [xk, "identf"], [("ps", 0), ("ps", 1)])
            CP("act", h2f, pf, [("ps", 0), ("ps", 1)], ["h2f"])
            hb_ = h2b[t % 2]; hbk = f"h2b{t % 2}"
            CP("act", hb_, h2f, ["h2f"], [hbk])
            P.dma(H2D[:, :, t * 128:(t + 1) * 128].rearrange("k p n -> p k n"), hb_, reads=[hbk], writes=[("H2D", t)], eng="pool")
            MM(PSb[:, 2, 0:NE], [(h2f[:, k, :], WR[:, k, :]) for k in range(8)], ["h2f", "WR"], [("ps", 2)])
            TT("dve", lg, PSb[:, 2, 0:NE], BR, ALU.add, [("ps", 2), "BR"], ["lg"])
            P.op("dve", lambda e: e.max(out=m8, in_=lg), ["lg"], ["m8"])
            TS("dve", msk, lg, m8[:, 3:4], None, ALU.is_ge, None, ["lg", "m8"], ["msk"])
            TS("dve", gs[:, 0:1], m8[:, 0:1], -1.0, None, ALU.mult, None, ["m8"], ["gs"])
            ACT(lg, lg, AF.Exp, ["lg", "gs"], ["lg"], bias=gs[:, 0:1])
            TT("dve", lg, lg, msk, ALU.mult, ["lg", "msk"], ["lg"])
            P.op("dve", lambda e: e.tensor_reduce(out=gs[:, 1:2], in_=lg, axis=AX.X, op=ALU.add), ["lg"], ["gs"])
            RECIP(gs[:, 1:2], ["gs"])
            TS("dve", lg, lg, gs[:, 1:2], None, ALU.mult, None, ["lg", "gs"], ["lg"])
            TR([(PSb[0:32, 3, 0:128], lg)], identf, ["lg", "identf"], [("ps", 3)])
            CP("act", GT[:, t * 128:(t + 1) * 128], PSb[0:32, 3, 0:128], [("ps", 3)], [("GT", t)])
        gtk = [("GT", t) for t in range(ntl_l)]
        ntok = ntl_l * 128
        P.dma(GATD[:, 0:ntok], GT[:, 0:ntok], reads=gtk, writes=["GATD"], eng="pool")
        A.free("a2", "s2", "WR", "BR", "xt0", "xt1", "junk", "h2f", "h2b0", "h2b1", "stat", "lg", "m8", "msk", "gs", "GT")
        if done(f"F1{l}"): break

        B2f = A.alloc("B2f", [32, D])
        P.dma(B2f, b_mlp2()[l], writes=["B2f"])
        B1 = A.alloc("B1", [128, NE, 16])
        P.dma(B1, b1T()[:, l], writes=["B1"])
        TS("dve", B1[:, :, 8:16], B1[:, :, 8:16], 1.0, None, ALU.add, None, ["B1"], ["B1"])
        G2 = A.alloc("G2", [128, 2, D])
        for v in range(2):
            modd_load(G2[:, v, :], v, 5, ("G2", v))
        NFb = A.alloc("NFb", [128, D])
        if last:
            P.dma(NFb, norm_final().partition_broadcast(128), writes=["NFb"])
        scsz = [8, 8, 8, 8] if last else [9, 9, 8, 8]
        SCT = max(scsz)
        W1s = [A.alloc(f"W1{i}", [128, 8, 2048], BF16) for i in range(2)]; W2 = A.alloc("W2", [128, 8, D], BF16)
        ACC = A.alloc("ACC", [128, 8, SCT * 128]); H2T = A.alloc("H2T", [128, 8, SCT * 128], BF16)
        GB = A.alloc("GB", [128, SCT * 128]); GTs = A.alloc("GTs", [32, SCT * 128])
        mW = A.alloc("mW", [128, 8, 512])
        Gt = [mW[:, 4 * i + 0, :] for i in range(2)]; St = [mW[:, 4 * i + 1, :] for i in range(2)]
        Lt = [mW[:, 4 * i + 2, :] for i in range(2)]; Xt = [mW[:, 4 * i + 3, :] for i in range(2)]
        Ub = A.alloc("mU", [128, 8, 512], BF16)
        w2k = [("W2", k) for k in range(8)]
        wi = 0; ei = 0
        sclist = []
        acc_ = 0
        for sz in scsz:
            sclist.append((acc_, sz)); acc_ += sz
        assert acc_ == ntl_l

        def load_w1(e_, buf):
            for k in range(0, 8, 2):
                P.dma(W1s[buf][:, k:k + 2, :], w_mlp1()[l, e_, k * 128:(k + 2) * 128, :].rearrange("(a p) n -> p a n", p=128), writes=[(f"W1{buf}", k), (f"W1{buf}", k + 1)], eng="pool")

        def load_w2(e_):
            for k in range(0, 8, 4):
                P.dma(W2[:, k:k + 4, :], w_mlp2()[l, e_, k * 128:(k + 4) * 128, :].rearrange("(a p) n -> p a n", p=128), writes=[("W2", k_) for k_ in range(k, k + 4)], eng="pool")
        load_w1(0, 0)
        load_w2(0)
        for sci, (sc0, nts) in enumerate(sclist):
            tk0 = sc0 * 128; ntk = nts * 128
            P.dma(H2T[:, :, 0:ntk], H2D[:, :, tk0:tk0 + ntk].rearrange("k p n -> p k n"), reads=[("H2D", t) for t in range(sc0, sc0 + nts)], writes=["H2T"])
            P.dma(GTs[:, 0:ntk], GATD[:, tk0:tk0 + ntk], reads=["GATD"], writes=["GTs"])
            cks = [(c0, min(512, ntk - c0)) for c0 in range(0, ntk, 512)]
            for (c0, w_) in cks:
                for dc in range(8):
                    bk = ("ps", 6 + dc % 2)
                    MM(PSb[:, 6 + dc % 2, 0:w_], [(B2f[:, dc * 128:(dc + 1) * 128], GTs[:, c0:c0 + w_])], ["B2f", "GTs"], [bk])
                    CP("act", ACC[:, dc, c0:c0 + w_], PSb[:, 6 + dc % 2, 0:w_], [bk], [("ACC", dc, c0)])
            units = [(e_, ci_, jf) for e_ in range(NE) for ci_ in range(len(cks)) for jf in range(8)]
            pend = None
            pend2 = None

            def tail(pd):
                L__, X__, lk__, xk__, jf__, c0__, w__ = pd
                STT("dve", L__, L__, -6.0, X__, ALU.max, ALU.mult, [lk__, xk__], [lk__])
                TT("dve", Ub[:, jf__, 0:w__], L__, GB[:, c0__:c0__ + w__], ALU.mult, [lk__, "GB"], [("mU", jf__)])

            def mlp2(ci_):
                c0, w_ = cks[ci_]
                for dc in range(8):
                    bk = ("ps", 4 + dc % 2)
                    MM(PSb[:, 4 + dc % 2, 0:w_], [(W2[:, jf, dc * 128:(dc + 1) * 128], Ub[:, jf, 0:w_]) for jf in range(8)], w2k + [("mU", jf) for jf in range(8)], [bk])
                    TT("dve", ACC[:, dc, c0:c0 + w_], ACC[:, dc, c0:c0 + w_], PSb[:, 4 + dc % 2, 0:w_], ALU.add, [bk, ("ACC", dc, c0)], [("ACC", dc, c0)])

            def next_expert(e_):
                return e_ + 1 if e_ + 1 < NE else (0 if sci + 1 < len(sclist) else None)
            for (e_, ci_, jf) in units:
                c0, w_ = cks[ci_]
                if ci_ == 0 and jf == 0:
                    cur = ei % 2; ei += 1
                    W1 = W1s[cur]; w1k = [(f"W1{cur}", k) for k in range(8)]
                    nx = next_expert(e_)
                    if nx is not None:
                        load_w1(nx, 1 - cur)
                bg = jf % 2; bl = 2 + jf % 2; wb = wi % 2; wi += 1
                MM(PSb[:, bg, 0:w_], [(W1[:, k, jf * 128:(jf + 1) * 128], H2T[:, k, c0:c0 + w_]) for k in range(8)], w1k + ["H2T"], [("ps", bg)])
                MM(PSb[:, bl, 0:w_], [(W1[:, k, 1024 + jf * 128:1024 + (jf + 1) * 128], H2T[:, k, c0:c0 + w_]) for k in range(8)], w1k + ["H2T"], [("ps", bl)])
                G_ = Gt[wb][:, 0:w_]; S_ = St[wb][:, 0:w_]; L_ = Lt[wb][:, 0:w_]; X_ = Xt[wb][:, 0:w_]
                gk_ = ("mW", "G", wb); sk_ = ("mW", "S", wb); lk_ = ("mW", "L", wb); xk_ = ("mW", "X", wb)
                TS("dve", G_, PSb[:, bg, 0:w_], B1[:, e_, jf:jf + 1], 7.0, ALU.add, ALU.min, [("ps", bg), "B1"], [gk_])
                ACT(S_, G_, AF.Sigmoid, [gk_], [sk_], scale=1.702)
                TS("dve", L_, PSb[:, bl, 0:w_], B1[:, e_, 8 + jf:9 + jf], 8.0, ALU.add, ALU.min, [("ps", bl), "B1"], [lk_])
                TT("pool", X_, G_, S_, ALU.mult, [gk_, sk_], [xk_])
                if pend is not None:
                    tail(pend)
                if jf == 0 and pend2 is not None:
                    pe_, pc_ = pend2
                    mlp2(pc_)
                    if pc_ == len(cks) - 1:
                        load_w2(e_)
                    pend2 = None
                if ci_ == 0 and jf == 0:
                    P.dma(GB[:, 0:ntk], GATD[e_, tk0:tk0 + ntk].partition_broadcast(128), reads=["GATD"], writes=["GB"])
                pend = (L_, X_, lk_, xk_, jf, c0, w_)
                if jf == 7:
                    pend2 = (e_, ci_)
            tail(pend)
            mlp2(pend2[1])
            nx = next_expert(NE - 1)
            if nx is not None:
                load_w2(nx)
            acck = [("ACC", dc, c0) for dc in range(8) for (c0, _) in cks]
            for tt_ in range(nts):
                t = sc0 + tt_; v = 0 if t < NLT else 1
                eb = tt_ % 2
                ebk = [("mW", r_, eb) for r_ in "GSLX"]
                ebuf = mW[:, 4 * eb:4 * eb + 4, :].rearrange("p a n -> p (a n)")
                xt = ebuf[:, 0:D]; y_ = ebuf[:, D:2 * D]
                P.dma(xt, X1[t * 128:(t + 1) * 128, :], reads=[("X1", t)], writes=ebk)
                pf = PSb[:, 6:8, :].rearrange("p b n -> p (b n)").rearrange("p (k c) -> p k c", k=8)
                TR([(pf[:, k, :], ACC[:, k, tt_ * 128:(tt_ + 1) * 128]) for k in range(8)], identf, acck + ["identf"], [("ps", 6), ("ps", 7)])
                TT("dve", y_, PSb[:, 6:8, :].rearrange("p b n -> p (b n)"), G2[:, v, :], ALU.mult, [("ps", 6), ("ps", 7), ("G2", v)], ebk)
                TT("dve", xt, xt, y_, ALU.add, ebk, ebk)
                if not last:
                    P.dma(XR[t * 128:(t + 1) * 128, :], xt, reads=ebk, writes=[("XR", t)], eng="pool")
                else:
                    sq = GB[:, 0:1]; rs = GB[:, 1:2]
                    ACT(y_, xt, AF.Square, ebk, ebk + ["GB"], accum_out=sq)
                    rstd_from_ssq(sq, rs, 1.0 / D, ["GB"], ["GB"])
                    STT("dve", xt, xt, rs, NFb, ALU.mult, ALU.mult, ebk + ["GB", "NFb"], ebk)
                    P.dma(out[t * 128:(t + 1) * 128, :], xt, reads=ebk, writes=[("out", t)], eng="pool")
        A.free("B2f", "B1", "G2", "NFb", "W10", "W11", "W2", "ACC", "H2T", "GB", "GTs", "mW", "mU")
        if done(f"F{l}"): break

    P.emit()
    P.close()
    es.close()
    return nc, stages, ins


def make_inputs(inputs, b, names=None):
    global _TB
    if _TB is None:
        _TB = _tables()
    f = lambda a: np.ascontiguousarray(a, dtype=np.float32)
    d = {}
    lay = {
        "x": lambda: f(inputs["x"][b]),
        "ctx": lambda: f(inputs["ctx"][b]),
        "c2T": lambda: f(np.stack([inputs["c"][b], inputs["c_ctx"]], 0).reshape(2, 8, 128).transpose(2, 0, 1)),
        "b_inT": lambda: f(inputs["b_in"][:, :768].reshape(2, 6, 128).transpose(2, 0, 1)),
        "convw": lambda: f(inputs["hy_conv_w"].reshape(2, 3, 6, 128).transpose(3, 0, 1, 2)),
        "convb": lambda: f(inputs["hy_conv_b"].reshape(2, 6, 128).transpose(2, 0, 1)),
        "hyw1": lambda: f(inputs["hy_filt_w1"]), "hyw2": lambda: f(inputs["hy_filt_w2"]), "hyw3": lambda: f(inputs["hy_filt_w3"]),
        "hyvec": lambda: f(np.stack([inputs["hy_filt_b1"], inputs["hy_filt_b2"], inputs["hy_filt_b3"], inputs["hy_filt_freq"]], -1).transpose(1, 0, 2)),
        "hyout": lambda: f(inputs["hy_filt_out"]),
        "hyskip": lambda: f(inputs["hy_skip"].reshape(2, 2, 128).transpose(2, 0, 1)),
        "qkg": lambda: f(np.concatenate([np.tile(inputs["ga_q_norm"], (1, 6)), np.tile(inputs["ga_k_norm"], (1, 2))], 1)),
        "sink": lambda: f(inputs["wa_sink"]),
        "bnorm": lambda: f(inputs["branch_norm"]),
        "bnT": lambda: f(inputs["branch_norm"][:, :256].reshape(2, 2, 128).transpose(2, 0, 1)),
        "b1T": lambda: f(inputs["b_mlp1"].reshape(2, NE, 16, 128).transpose(3, 0, 1, 2)),
    }
    for k in ["w_ada", "b_ada", "norm_mix", "norm_ffn", "w_in", "b_in", "w_out", "b_out", "w_router", "b_router",
              "w_mlp1", "w_mlp2", "b_mlp2", "norm_final"]:
        lay[k] = (lambda k=k: f(inputs[k]))
    for k in (names if names is not None else list(lay.keys()) + list(_TB.keys())):
        d[k] = _TB[k] if k in _TB else lay[k]()
    return d


_SHARED = {}


def kernel(**inputs):
    nc, _, ins = build()
    names = list(ins.keys())
    shared = {}
    in_maps = []
    for b in range(8):
        m = make_inputs(inputs, b, [n for n in names if n in ("x", "ctx", "c2T")])
        if not shared:
            shared = make_inputs(inputs, 0, [n for n in names if n not in ("x", "ctx", "c2T")])
        m.update(shared)
        in_maps.append(m)
    res = run_bass_kernel_spmd(nc, in_maps, core_ids=list(range(8)))
    return np.stack([np.asarray(r["out"], dtype=np.float32) for r in res.results], 0)
```
